# Optimizing a Trainium2 kernel written in Bass

```python
import math
import jax
import jax.numpy as jnp
from jax import lax
import numpy as np

D_MODEL = 2048
BATCH = 16
SEQ = 2048
DEPTH = 4

CTX_LEN = 256
GRID_W = 64
NORM_EPS = 1e-6
N_BRANCH = 4
HY_W = 512
HY_ORDER = 2
HY_SHORT = 3
HY_BANDS = 16
HY_FEAT = 1 + 2 * HY_BANDS
HY_HID = 64
HY_IN = (HY_ORDER + 1) * HY_W
LRU_W = 512
LRU_HEADS = 8
LRU_BW = LRU_W // LRU_HEADS
LRU_CONV = 4
LRU_C = 8.0
LRU_IN = 2 * LRU_W
SSD_HEADS = 8
SSD_HEAD_DIM = 64
SSD_W = SSD_HEADS * SSD_HEAD_DIM
SSD_GROUPS = 2
SSD_STATE = 128
SSD_CONV = 4
SSD_CHUNK = 128
SSD_XBC = SSD_W + 2 * SSD_GROUPS * SSD_STATE
SSD_IN = SSD_W + SSD_XBC + 2 * SSD_HEADS
ATT_HEADS = 8
ATT_KV_HEADS = 2
ATT_HEAD_DIM = 128
ATT_BLOCK = 128
ROPE_THETA = 10000.0
ATT_Q = ATT_HEADS * ATT_HEAD_DIM
ATT_KV = ATT_KV_HEADS * ATT_HEAD_DIM
ATT_IN = ATT_Q + 2 * ATT_KV
OFF_HY = 0
OFF_LRU = OFF_HY + HY_IN
OFF_SSD = OFF_LRU + LRU_IN
OFF_ATT = OFF_SSD + SSD_IN
IN_W = OFF_ATT + ATT_IN
MIX_W = HY_W + LRU_W + SSD_W + ATT_Q
FF_DENSE = 5504
N_EXPERTS = 8
TOP_K = 2
FF_EXPERT = 4096

kernel_name = 'hybrid_gated_mixer_dit_block'


def rms_norm(x, gain):
    xf = x.astype(jnp.float32)
    y = xf * lax.rsqrt(jnp.mean(jnp.square(xf), axis=-1, keepdims=True) + NORM_EPS)
    return (y * gain.astype(jnp.float32)).astype(x.dtype)


def dw_conv(u, w, b, pad_left):
    k, ch = w.shape
    y = lax.conv_general_dilated(u, w[:, None, :].astype(u.dtype), window_strides=(1,),
                                 padding=[(pad_left, k - 1 - pad_left)],
                                 dimension_numbers=('NWC', 'WIO', 'NWC'), feature_group_count=ch)
    return y + b


def linear_scan(a, b, h0):
    b = b.at[:, 0].add(a[:, 0] * h0)
    def combine(l, r):
        return l[0] * r[0], r[0] * l[1] + r[1]
    _, h = lax.associative_scan(combine, (a, b), axis=1)
    return h


def flip_seq(t):
    return jnp.flip(t, axis=1)


def same_seq(t):
    return t


def hyena_filters(L, w1, b1, w2, b2, freq, w3, b3, decay):
    f32 = jnp.float32
    t = jnp.arange(L, dtype=f32)
    tn = t / L
    bands = jnp.linspace(1e-4, HY_BANDS - 1, HY_BANDS, dtype=f32)
    ang = (2.0 * math.pi / L) * t[:, None] * bands[None, :]
    feat = jnp.concatenate([tn[:, None], jnp.cos(ang), -jnp.sin(ang)], axis=-1)
    hdn = jnp.sin(freq[0].astype(f32) * (feat @ w1.astype(f32) + b1.astype(f32)))
    hdn = jnp.sin(freq[1].astype(f32) * (hdn @ w2.astype(f32) + b2.astype(f32)))
    filt = (hdn @ w3.astype(f32) + b3.astype(f32)).reshape(L, HY_ORDER, 2, HY_W)
    window = jnp.exp(-tn[:, None, None, None] * jnp.abs(decay.astype(f32))[None])
    return filt * window


def long_conv_bidir(u, h_fwd, h_bwd, bias):
    L = u.shape[1]
    k = jnp.concatenate([h_fwd, jnp.zeros_like(h_fwd[:1]), h_bwd[:0:-1]], axis=0)
    kf = jnp.fft.rfft(k, n=2 * L, axis=0)
    uf32 = u.astype(jnp.float32)
    uf = jnp.fft.rfft(uf32, n=2 * L, axis=1)
    y = jnp.fft.irfft(uf * kf[None], n=2 * L, axis=1)[:, :L]
    return (y + bias.astype(jnp.float32) * uf32).astype(u.dtype)


def hyena_mixer(pc, pl, conv_w, conv_b, w1, b1, w2, b2, freq, w3, b3, decay, bias, with_ctx):
    def run(p):
        filt = hyena_filters(p.shape[1], w1, b1, w2, b2, freq, w3, b3, decay)
        u = dw_conv(p, conv_w, conv_b, (HY_SHORT - 1) // 2)
        v, x1, x2 = jnp.split(u, HY_ORDER + 1, axis=-1)
        z = x1 * long_conv_bidir(v, filt[:, 0, 0], filt[:, 0, 1], bias[0])
        return x2 * long_conv_bidir(z, filt[:, 1, 0], filt[:, 1, 1], bias[1])
    y_c = run(pc) if with_ctx else None
    return y_c, run(pl)


def rglru_coeffs(xc, wa, ba, wx, bx, lam):
    f32 = jnp.float32
    bsz, L, _ = xc.shape
    xf = xc.astype(f32)
    xh = xf.reshape(bsz, L, LRU_HEADS, LRU_BW)
    r = jax.nn.sigmoid(jnp.einsum('blhi,hij->blhj', xh, wa.astype(f32)) + ba.astype(f32)).reshape(bsz, L, LRU_W)
    i = jax.nn.sigmoid(jnp.einsum('blhi,hij->blhj', xh, wx.astype(f32)) + bx.astype(f32)).reshape(bsz, L, LRU_W)
    log_a = -LRU_C * r * jax.nn.softplus(-lam.astype(f32))
    a = jnp.exp(log_a)
    b = jnp.sqrt(-jnp.expm1(2.0 * log_a)) * i * xf
    return a, b


def rglru_mixer(pc, pl, conv_w, conv_b, wa, ba, wx, bx, lam, with_ctx):
    f32 = jnp.float32
    gate_c, xin_c = jnp.split(pc, 2, axis=-1)
    gate_l, xin_l = jnp.split(pl, 2, axis=-1)
    xc_c = dw_conv(xin_c, conv_w, conv_b, LRU_CONV // 2)
    xc_l = dw_conv(xin_l, conv_w, conv_b, LRU_CONV // 2)
    bsz = pl.shape[0]
    h_ctx, h_lat = [], []
    for d, fl in enumerate((same_seq, flip_seq)):
        a, b = rglru_coeffs(fl(xc_c), wa[d], ba[d], wx[d], bx[d], lam[d])
        hc = linear_scan(a, b, jnp.zeros((bsz, LRU_W), f32))
        a, b = rglru_coeffs(fl(xc_l), wa[d], ba[d], wx[d], bx[d], lam[d])
        hl = linear_scan(a, b, hc[:, -1])
        h_ctx.append(fl(hc))
        h_lat.append(fl(hl))
    y_l = (jax.nn.gelu(gate_l.astype(f32)) * (h_lat[0] + h_lat[1])).astype(pl.dtype)
    y_c = (jax.nn.gelu(gate_c.astype(f32)) * (h_ctx[0] + h_ctx[1])).astype(pc.dtype) if with_ctx else None
    return y_c, y_l


def ssd_scan(x, dt, A, bm, cm, h0):
    bsz, L = x.shape[:2]
    nc = L // SSD_CHUNK
    R = SSD_HEADS // SSD_GROUPS
    shp = (bsz, nc, SSD_CHUNK, SSD_GROUPS, R)
    xdt = (x * dt[..., None]).reshape(*shp, SSD_HEAD_DIM)
    bg = bm.reshape(bsz, nc, SSD_CHUNK, SSD_GROUPS, SSD_STATE)
    cg = cm.reshape(bsz, nc, SSD_CHUNK, SSD_GROUPS, SSD_STATE)
    cs = jnp.cumsum((dt * A).reshape(shp), axis=2)
    causal = jnp.tril(jnp.ones((SSD_CHUNK, SSD_CHUNK), dtype=bool))
    seg = cs[:, :, :, None] - cs[:, :, None, :]
    decay = jnp.exp(jnp.where(causal[:, :, None, None], seg, -jnp.inf))
    cb = jnp.einsum('bcign,bcjgn->bcijg', cg, bg)
    y_diag = jnp.einsum('bcijg,bcijgr,bcjgrp->bcigrp', cb, decay, xdt)
    w_end = jnp.exp(cs[:, :, -1:] - cs)
    states = jnp.einsum('bcjgn,bcjgr,bcjgrp->bcgrpn', bg, w_end, xdt)
    chunk_a = jnp.exp(cs[:, :, -1])[..., None, None]
    s_end = linear_scan(chunk_a, states, h0)
    s_prev = jnp.concatenate([h0[:, None], s_end[:, :-1]], axis=1)
    y_off = jnp.einsum('bcign,bcigr,bcgrpn->bcigrp', cg, jnp.exp(cs), s_prev)
    y = (y_diag + y_off).reshape(bsz, L, SSD_HEADS, SSD_HEAD_DIM)
    return y, s_end[:, -1]


def ssd_mixer(pc, pl, conv_w, conv_b, a_log, dt_bias, d_skip, norm_g, with_ctx):
    f32 = jnp.float32
    def prep(p):
        bsz, L, _ = p.shape
        z, xbc, dt_raw = jnp.split(p, [SSD_W, SSD_W + SSD_XBC], axis=-1)
        xbc = jax.nn.silu(dw_conv(xbc, conv_w, conv_b, SSD_CONV // 2)).astype(f32)
        xs, bm, cm = jnp.split(xbc, [SSD_W, SSD_W + SSD_GROUPS * SSD_STATE], axis=-1)
        return (z, xs.reshape(bsz, L, SSD_HEADS, SSD_HEAD_DIM),
                bm.reshape(bsz, L, SSD_GROUPS, SSD_STATE), cm.reshape(bsz, L, SSD_GROUPS, SSD_STATE),
                dt_raw.astype(f32))
    zc, xc_, bc, cc, dtc = prep(pc)
    zl, xl, bl, cl, dtl = prep(pl)
    bsz = pl.shape[0]
    h0 = jnp.zeros((bsz, SSD_GROUPS, SSD_HEADS // SSD_GROUPS, SSD_HEAD_DIM, SSD_STATE), f32)
    ys_c, ys_l = [], []
    for d, fl in enumerate((same_seq, flip_seq)):
        A = -jnp.exp(a_log[d].astype(f32))
        dtb = dt_bias[d].astype(f32)
        dt_c = jax.nn.softplus(dtc[..., d * SSD_HEADS:(d + 1) * SSD_HEADS] + dtb)
        dt_l = jax.nn.softplus(dtl[..., d * SSD_HEADS:(d + 1) * SSD_HEADS] + dtb)
        yc, s_ctx = ssd_scan(fl(xc_), fl(dt_c), A, fl(bc), fl(cc), h0)
        yl, _ = ssd_scan(fl(xl), fl(dt_l), A, fl(bl), fl(cl), s_ctx)
        ys_c.append(fl(yc))
        ys_l.append(fl(yl))
    def finish(ys, xs, z):
        bsz_, L = xs.shape[:2]
        y = ys[0] + ys[1] + d_skip.astype(f32)[:, None] * xs
        y = y.reshape(bsz_, L, SSD_W) * jax.nn.silu(z.astype(f32))
        return rms_norm(y, norm_g).astype(z.dtype)
    y_c = finish(ys_c, xc_, zc) if with_ctx else None
    return y_c, finish(ys_l, xl, zl)


def axial_rope_tables(row_id, col_id):
    axis_dim = ATT_HEAD_DIM // 2
    inv = ROPE_THETA ** (-jnp.arange(0, axis_dim, 2, dtype=jnp.float32) / axis_dim)
    ang = jnp.concatenate([row_id[:, None] * inv, col_id[:, None] * inv], axis=-1)
    return jnp.cos(ang), jnp.sin(ang)


def apply_rope(x, cos, sin):
    xf = x.astype(jnp.float32).reshape(*x.shape[:-1], ATT_HEAD_DIM // 2, 2)
    x1, x2 = xf[..., 0], xf[..., 1]
    c = cos[None, :, None]
    s = sin[None, :, None]
    out = jnp.stack([x1 * c - x2 * s, x1 * s + x2 * c], axis=-1).reshape(x.shape)
    return out.astype(x.dtype)


def attend(q, k, v):
    s = jnp.einsum('bqkgd,bskd->bkgqs', q.astype(jnp.float32), k.astype(jnp.float32)) * (ATT_HEAD_DIM ** -0.5)
    p = jax.nn.softmax(s, axis=-1)
    return jnp.einsum('bkgqs,bskd->bqkgd', p, v.astype(jnp.float32)).astype(v.dtype)


def q_heads(pq, gain):
    bsz, L, _ = pq.shape
    return rms_norm(pq.reshape(bsz, L, ATT_HEADS, ATT_HEAD_DIM), gain)


def kv_heads(pkv, gain):
    bsz, L, _ = pkv.shape
    k, v = jnp.split(pkv, 2, axis=-1)
    k = rms_norm(k.reshape(bsz, L, ATT_KV_HEADS, ATT_HEAD_DIM), gain)
    return k, v.reshape(bsz, L, ATT_KV_HEADS, ATT_HEAD_DIM)


def attention_mixer(pc, pl, q_norm, k_norm, cos, sin, with_ctx):
    G = ATT_HEADS // ATT_KV_HEADS
    bsz, L, _ = pl.shape
    Lc = pc.shape[1]
    kc, vc = kv_heads(pc[..., ATT_Q:], k_norm)
    kl, vl = kv_heads(pl[..., ATT_Q:], k_norm)
    ql = apply_rope(q_heads(pl[..., :ATT_Q], q_norm), cos, sin)
    kl = apply_rope(kl, cos, sin)
    k_all = jnp.concatenate([kc, kl], axis=1)
    v_all = jnp.concatenate([vc, vl], axis=1)
    nb = L // ATT_BLOCK
    qb = ql.reshape(bsz, nb, ATT_BLOCK, ATT_KV_HEADS, G, ATT_HEAD_DIM).swapaxes(0, 1)
    ob = lax.map(lambda qi: attend(qi, k_all, v_all), qb)
    y_l = ob.swapaxes(0, 1).reshape(bsz, L, ATT_Q)
    y_c = None
    if with_ctx:
        qc = q_heads(pc[..., :ATT_Q], q_norm).reshape(bsz, Lc, ATT_KV_HEADS, G, ATT_HEAD_DIM)
        y_c = attend(qc, kc, vc).reshape(bsz, Lc, ATT_Q)
    return y_c, y_l


def merge_branches(h, ys, w_branch, w_gate, b_gate, w_out):
    merged = None
    off = 0
    for i, y in enumerate(ys):
        width = y.shape[-1]
        gate = jax.nn.sigmoid(h @ w_gate[i] + b_gate[i])
        term = gate * (y @ w_branch[off:off + width])
        merged = term if merged is None else merged + term
        off += width
    return merged @ w_out


def swiglu(h, w1, w3, w2):
    return (jax.nn.silu(h @ w1) * (h @ w3)) @ w2


def moe_swiglu(h, w_router, b_router, w1, w3, w2):
    bsz, L, D = h.shape
    t = h.reshape(bsz * L, D)
    logits = (t @ w_router + b_router).astype(jnp.float32)
    top_v, top_i = lax.top_k(logits, TOP_K)
    top_w = jax.nn.softmax(top_v, axis=-1)
    gates = jnp.einsum('tk,tke->te', top_w, jax.nn.one_hot(top_i, N_EXPERTS, dtype=jnp.float32)).astype(h.dtype)
    out = None
    for e in range(N_EXPERTS):
        y_e = gates[:, e:e + 1] * swiglu(t, w1[e], w3[e], w2[e])
        out = y_e if out is None else out + y_e
    return out.reshape(bsz, L, D)


def setup_inputs(seed: int = 0) -> dict:
    key = jax.random.key(seed)
    keys = iter(jax.random.split(key, 64))
    f32 = jnp.float32
    D = D_MODEL
    n_dense = (DEPTH + 1) // 2
    n_moe = DEPTH // 2
    def nrm(shape, scale):
        return jax.random.normal(next(keys), shape, f32) * scale
    def gain(shape, noise=0.05):
        return 1.0 + nrm(shape, noise)
    def unif(shape, lo, hi):
        return jax.random.uniform(next(keys), shape, f32, lo, hi)
    inp = {}
    inp['x'] = nrm((BATCH, SEQ, D), 1.0)
    inp['c'] = nrm((BATCH, D), 1.0)
    inp['ctx'] = nrm((BATCH, CTX_LEN, D), 1.0)
    inp['c_ctx'] = nrm((D,), 1.0)
    inp['norm_mix'] = gain((DEPTH, D))
    inp['norm_ffn'] = gain((DEPTH, D))
    inp['ada_w'] = nrm((DEPTH, D, 6 * D), 0.5 * D ** -0.5)
    inp['ada_b'] = nrm((DEPTH, 6 * D), 0.02)
    inp['w_in'] = nrm((DEPTH, D, IN_W), D ** -0.5)
    inp['hy_conv_w'] = nrm((DEPTH, HY_SHORT, HY_IN), HY_SHORT ** -0.5)
    inp['hy_conv_b'] = nrm((DEPTH, HY_IN), 0.02)
    inp['hy_w1'] = nrm((DEPTH, HY_FEAT, HY_HID), HY_FEAT ** -0.5)
    inp['hy_b1'] = nrm((DEPTH, HY_HID), 0.1)
    inp['hy_w2'] = nrm((DEPTH, HY_HID, HY_HID), HY_HID ** -0.5)
    inp['hy_b2'] = nrm((DEPTH, HY_HID), 0.1)
    inp['hy_freq'] = gain((DEPTH, 2, HY_HID), 0.1)
    inp['hy_w3'] = nrm((DEPTH, HY_HID, HY_ORDER * 2 * HY_W), 0.05 * HY_HID ** -0.5)
    inp['hy_b3'] = nrm((DEPTH, HY_ORDER * 2 * HY_W), 0.001)
    base_decay = jnp.linspace(math.log(1e-2) / 0.3, math.log(1e-2) / 1.5, HY_W, dtype=f32)
    inp['hy_decay'] = base_decay + nrm((DEPTH, HY_ORDER, 2, HY_W), 0.1)
    inp['hy_bias'] = nrm((DEPTH, HY_ORDER, HY_W), 0.5)
    inp['lru_conv_w'] = nrm((DEPTH, LRU_CONV, LRU_W), 0.5)
    inp['lru_conv_b'] = nrm((DEPTH, LRU_W), 0.02)
    inp['lru_wa'] = nrm((DEPTH, 2, LRU_HEADS, LRU_BW, LRU_BW), LRU_BW ** -0.5)
    inp['lru_ba'] = nrm((DEPTH, 2, LRU_HEADS, LRU_BW), 0.1)
    inp['lru_wx'] = nrm((DEPTH, 2, LRU_HEADS, LRU_BW, LRU_BW), LRU_BW ** -0.5)
    inp['lru_bx'] = nrm((DEPTH, 2, LRU_HEADS, LRU_BW), 0.1)
    a_root = unif((DEPTH, 2, LRU_W), 0.9, 0.999) ** (1.0 / LRU_C)
    inp['lru_lambda'] = jnp.log(a_root) - jnp.log1p(-a_root)
    inp['ssd_conv_w'] = nrm((DEPTH, SSD_CONV, SSD_XBC), 0.5)
    inp['ssd_conv_b'] = nrm((DEPTH, SSD_XBC), 0.02)
    inp['ssd_a_log'] = jnp.log(unif((DEPTH, 2, SSD_HEADS), 1.0, 16.0))
    dt0 = jnp.exp(unif((DEPTH, 2, SSD_HEADS), math.log(1e-3), math.log(1e-1)))
    inp['ssd_dt_bias'] = dt0 + jnp.log(-jnp.expm1(-dt0))
    inp['ssd_d'] = gain((DEPTH, SSD_HEADS), 0.1)
    inp['ssd_norm'] = gain((DEPTH, SSD_W))
    inp['att_q_norm'] = gain((DEPTH, ATT_HEAD_DIM))
    inp['att_k_norm'] = gain((DEPTH, ATT_HEAD_DIM))
    inp['w_branch'] = nrm((DEPTH, MIX_W, D), (MIX_W // N_BRANCH) ** -0.5)
    inp['w_gate'] = nrm((DEPTH, N_BRANCH, D, D), D ** -0.5)
    inp['b_gate'] = nrm((DEPTH, N_BRANCH, D), 0.1)
    inp['w_out'] = nrm((DEPTH, D, D), D ** -0.5)
    inp['ffn_w1'] = nrm((n_dense, D, FF_DENSE), D ** -0.5)
    inp['ffn_w3'] = nrm((n_dense, D, FF_DENSE), D ** -0.5)
    inp['ffn_w2'] = nrm((n_dense, FF_DENSE, D), FF_DENSE ** -0.5)
    inp['moe_router'] = nrm((n_moe, D, N_EXPERTS), D ** -0.5)
    inp['moe_router_b'] = nrm((n_moe, N_EXPERTS), 0.01)
    inp['moe_w1'] = nrm((n_moe, N_EXPERTS, D, FF_EXPERT), D ** -0.5)
    inp['moe_w3'] = nrm((n_moe, N_EXPERTS, D, FF_EXPERT), D ** -0.5)
    inp['moe_w2'] = nrm((n_moe, N_EXPERTS, FF_EXPERT, D), FF_EXPERT ** -0.5)
    inp['norm_final'] = gain((D,))
    return inp


def reference(x, c, ctx, c_ctx, norm_mix, norm_ffn, ada_w, ada_b, w_in,
              hy_conv_w, hy_conv_b, hy_w1, hy_b1, hy_w2, hy_b2, hy_freq, hy_w3, hy_b3, hy_decay, hy_bias,
              lru_conv_w, lru_conv_b, lru_wa, lru_ba, lru_wx, lru_bx, lru_lambda,
              ssd_conv_w, ssd_conv_b, ssd_a_log, ssd_dt_bias, ssd_d, ssd_norm,
              att_q_norm, att_k_norm, w_branch, w_gate, b_gate, w_out,
              ffn_w1, ffn_w3, ffn_w2, moe_router, moe_router_b, moe_w1, moe_w3, moe_w2, norm_final):
    Lc = ctx.shape[1]
    L = x.shape[1]
    ROWS = L // GRID_W
    row_id = jnp.repeat(jnp.arange(ROWS, dtype=jnp.float32), GRID_W)
    col_id = jnp.tile(jnp.arange(GRID_W, dtype=jnp.float32), ROWS)
    cos, sin = axial_rope_tables(row_id, col_id)
    s_c = jax.nn.silu(c)
    s_ctx = jax.nn.silu(c_ctx)
    xc = ctx
    for l in range(DEPTH):
        with_ctx = l < DEPTH - 1
        mod_l = jnp.split((s_c @ ada_w[l] + ada_b[l])[:, None, :], 6, axis=-1)
        mod_c = jnp.split((s_ctx @ ada_w[l] + ada_b[l])[None, None, :], 6, axis=-1)
        h_l = rms_norm(x, norm_mix[l]) * (1.0 + mod_l[1]) + mod_l[0]
        h_c = rms_norm(xc, norm_mix[l]) * (1.0 + mod_c[1]) + mod_c[0]
        h_all = jnp.concatenate([h_c, h_l], axis=1)
        proj = h_all @ w_in[l]
        pc, pl = proj[:, :Lc], proj[:, Lc:]
        hy_c, hy_l = hyena_mixer(pc[..., OFF_HY:OFF_LRU], pl[..., OFF_HY:OFF_LRU], hy_conv_w[l], hy_conv_b[l],
                                 hy_w1[l], hy_b1[l], hy_w2[l], hy_b2[l], hy_freq[l], hy_w3[l], hy_b3[l],
                                 hy_decay[l], hy_bias[l], with_ctx)
        lru_c, lru_l = rglru_mixer(pc[..., OFF_LRU:OFF_SSD], pl[..., OFF_LRU:OFF_SSD], lru_conv_w[l], lru_conv_b[l],
                                   lru_wa[l], lru_ba[l], lru_wx[l], lru_bx[l], lru_lambda[l], with_ctx)
        ssd_c, ssd_l = ssd_mixer(pc[..., OFF_SSD:OFF_ATT], pl[..., OFF_SSD:OFF_ATT], ssd_conv_w[l], ssd_conv_b[l],
                                 ssd_a_log[l], ssd_dt_bias[l], ssd_d[l], ssd_norm[l], with_ctx)
        att_c, att_l = attention_mixer(pc[..., OFF_ATT:IN_W], pl[..., OFF_ATT:IN_W], att_q_norm[l], att_k_norm[l],
                                       cos, sin, with_ctx)
        ys_l = [hy_l, lru_l, ssd_l, att_l]
        if with_ctx:
            ys_c = [hy_c, lru_c, ssd_c, att_c]
            ys = [jnp.concatenate([yc_, yl_], axis=1) for yc_, yl_ in zip(ys_c, ys_l)]
            m = merge_branches(h_all, ys, w_branch[l], w_gate[l], b_gate[l], w_out[l])
            xc = xc + mod_c[2] * m[:, :Lc]
            x = x + mod_l[2] * m[:, Lc:]
        else:
            x = x + mod_l[2] * merge_branches(h_l, ys_l, w_branch[l], w_gate[l], b_gate[l], w_out[l])
        f_l = rms_norm(x, norm_ffn[l]) * (1.0 + mod_l[4]) + mod_l[3]
        if with_ctx:
            f_c = rms_norm(xc, norm_ffn[l]) * (1.0 + mod_c[4]) + mod_c[3]
            f_in = jnp.concatenate([f_c, f_l], axis=1)
        else:
            f_in = f_l
        j = l // 2
        if l % 2 == 0:
            f_out = swiglu(f_in, ffn_w1[j], ffn_w3[j], ffn_w2[j])
        else:
            f_out = moe_swiglu(f_in, moe_router[j], moe_router_b[j], moe_w1[j], moe_w3[j], moe_w2[j])
        if with_ctx:
            xc = xc + mod_c[5] * f_out[:, :Lc]
            x = x + mod_l[5] * f_out[:, Lc:]
        else:
            x = x + mod_l[5] * f_out
    return rms_norm(x, norm_final)
```

```python
import math
from contextlib import ExitStack

import numpy as np
import ml_dtypes

import concourse.bass as bass
import concourse.mybir as mybir
from concourse.bass_utils import run_bass_kernel_spmd

F32 = mybir.dt.float32
BF16 = mybir.dt.bfloat16
U8 = mybir.dt.uint8
ALU = mybir.AluOpType
AF = mybir.ActivationFunctionType
AX = mybir.AxisListType

D = 2048
KC = 16
DEPTH = 4
LC = 256
LL = 2048
T = LC + LL
NSEQ = 2
NCORE = 8
EPS = 1e-6
IN_W = 5648
FF = 5504
NEXP = 8
FFE = 4096
PI = math.pi

OFF_HY, OFF_LRU, OFF_SSD, OFF_ATT = 0, 1536, 2560, 4112
FM_COLS = ([OFF_HY + 128 * i for i in range(12)] + [OFF_LRU + 128 * i for i in range(8)]
           + [OFF_SSD + 512 + 128 * i for i in range(8)] + [OFF_ATT + 128 * i for i in range(10)])
PF_HY, PF_LRU, PF_SSD, PF_ATT = 0, 12, 20, 28
NFM = len(FM_COLS)
TM_Z, TM_DT, TM_V, NTM = 0, 512, 528, 784

SB_BYTES = 206 * 1024


class Buf:
    __slots__ = ("w", "r")

    def __init__(self):
        self.w = None
        self.r = []


def bufs(n):
    return [Buf() for _ in range(n)]


class Prog:
    ENG = ("pe", "dve", "act", "pool", "sp")
    NSLOT = {"sp": 24, "pool": 12, "act": 4}

    def __init__(self, nc):
        self.nc = nc
        self.ops = {e: [] for e in self.ENG}
        self.cnt = {e: 0 for e in self.ENG}
        self.seen = {e: {} for e in self.ENG}
        self.slot_uses = {q: [0] * n for q, n in self.NSLOT.items()}
        self.slot_next = {q: 0 for q in self.NSLOT}
        self.extra_sems = {}
        self.ncoll = 0
        self.sb_off = 0
        self.arena = None
        self.psum = None

    def _deps(self, eng, reads, writes):
        need = {}
        for b in reads:
            if b.w is not None:
                k, v = b.w
                if need.get(k, 0) < v:
                    need[k] = v
        for b in writes:
            if b.w is not None:
                k, v = b.w
                if need.get(k, 0) < v:
                    need[k] = v
            for k, v in b.r:
                if need.get(k, 0) < v:
                    need[k] = v
        waits = []
        seen = self.seen[eng]
        for k, v in need.items():
            if k == "c_pe" and eng == "pe":
                continue
            if seen.get(k, 0) < v:
                waits.append((k, v))
                seen[k] = v
        return waits

    def _mark(self, tok, reads, writes):
        for b in reads:
            b.r.append(tok)
            if len(b.r) > 64:
                m = {}
                for k, v in b.r:
                    if m.get(k, 0) < v:
                        m[k] = v
                b.r = list(m.items())
        for b in writes:
            b.w = tok
            b.r = []

    def op(self, eng, fn, reads=(), writes=(), inc=True):
        waits = self._deps(eng, reads, writes)
        key = "c_" + eng
        if inc:
            self.cnt[eng] += 1
            tok = (key, self.cnt[eng])
            self.ops[eng].append((waits, fn, (key, 1)))
        else:
            tok = (key, self.cnt[eng] + 1)
            self.ops[eng].append((waits, fn, None))
        self._mark(tok, reads, writes)

    def dma(self, out, in_, reads=(), writes=(), q="sp", **kw):
        waits = self._deps(q, reads, writes)
        s = self.slot_next[q]
        self.slot_next[q] = (s + 1) % self.NSLOT[q]
        key = "d_%s_%d" % (q, s)
        prev = self.slot_uses[q][s] * 16
        if prev and self.seen[q].get(key, 0) < prev:
            waits.append((key, prev))
            self.seen[q][key] = prev
        self.slot_uses[q][s] += 1
        tok = (key, prev + 16)
        self.ops[q].append((waits, (lambda e: e.dma_start(out=out, in_=in_, **kw)), (key, 16)))
        self._mark(tok, reads, writes)

    def collective(self, kind, ins, outs, reads, writes):
        waits = self._deps("pool", reads, writes)
        i = self.ncoll
        self.ncoll += 1
        key = "cc"
        prev = i
        if prev and self.seen["pool"].get(key, 0) < prev:
            waits.append((key, prev))
            self.seen["pool"][key] = prev
        self.extra_sems[key] = prev + 1
        tok = (key, prev + 1)
        fn = lambda e: e.collective_compute(kind, ALU.bypass, replica_groups=[list(range(NCORE))],
                                            ins=[ins.opt()], outs=[outs.opt()])
        self.ops["pool"].append((waits, fn, (key, None)))
        self._mark(tok, reads, writes)

    def barrier(self):
        toks = [("c_" + e, self.cnt[e]) for e in self.ENG if self.cnt[e]]
        for q, uses in self.slot_uses.items():
            for s, u in enumerate(uses):
                if u:
                    toks.append(("d_%s_%d" % (q, s), u * 16))
        for k, v in self.extra_sems.items():
            toks.append((k, v))
        for e in self.ENG:
            waits = []
            for k, v in toks:
                if k == "c_" + e:
                    continue
                if self.seen[e].get(k, 0) < v:
                    waits.append((k, v))
                    self.seen[e][k] = v
            if waits:
                self.ops[e].append((waits, None, None))

    def reset_sbuf(self, keep=0):
        self.sb_off = keep

    def tile(self, shape, dtype):
        esz = 4 if dtype == F32 else (2 if dtype == BF16 else 1)
        n = 1
        for s in shape[1:]:
            n *= s
        nbytes = (n * esz + 63) // 64 * 64
        off = self.sb_off
        assert off + nbytes <= SB_BYTES, ("SBUF overflow", off, nbytes)
        self.sb_off = off + nbytes
        ap = self.arena[0:shape[0], off:off + n * esz]
        if dtype != U8:
            ap = ap.bitcast(dtype)
        if len(shape) == 3:
            ap = ap.rearrange("p (a b) -> p a b", a=shape[1])
        elif len(shape) == 4:
            ap = ap.rearrange("p (a b c) -> p a b c", a=shape[1], b=shape[2])
        return ap

    def mm(self, out, lhsT, rhs, start, stop, R, W, inc=None):
        inc = stop if inc is None else inc
        self.op("pe", lambda e: e.matmul(out, lhsT, rhs, start=start, stop=stop), R, W, inc=inc)

    def tr(self, out, in_, ident, R, W):
        self.op("pe", lambda e: e.transpose(out, in_, ident), R, W)

    def act(self, out, in_, func, R, W, bias=None, scale=None, accum_out=None):
        kw = {}
        if bias is not None:
            kw["bias"] = bias
        if scale is not None:
            kw["scale"] = scale
        if accum_out is not None:
            kw["accum_out"] = accum_out
        self.op("act", lambda e: e.activation(out, in_, func, **kw), R, W)

    def ts(self, out, in0, s1, s2, op0, op1, R, W, eng="dve"):
        if s2 is None:
            self.op(eng, lambda e: e.tensor_scalar(out, in0, s1, None, op0), R, W)
        else:
            self.op(eng, lambda e: e.tensor_scalar(out, in0, s1, s2, op0, op1), R, W)

    def tt(self, out, in0, in1, op, R, W, eng="dve"):
        self.op(eng, lambda e: e.tensor_tensor(out, in0, in1, op), R, W)

    def stt(self, out, in0, scalar, in1, op0, op1, R, W):
        self.op("dve", lambda e: e.scalar_tensor_tensor(out, in0, scalar, in1, op0, op1), R, W)

    def copy(self, out, in_, R, W, eng="dve"):
        if eng == "act":
            self.op("act", lambda e: e.activation(out, in_, AF.Copy), R, W)
        else:
            self.op(eng, lambda e: e.tensor_copy(out, in_), R, W)

    def memset(self, ap, val, W, eng="dve"):
        self.op(eng, lambda e: e.memset(ap, val), (), W)

    def recip(self, out, in_, R, W):
        self.op("dve", lambda e: e.reciprocal(out, in_), R, W)

    def emit(self, es):
        nc = self.nc
        keys = ["c_" + e for e in self.ENG]
        for q, uses in self.slot_uses.items():
            for s, u in enumerate(uses):
                if u:
                    keys.append("d_%s_%d" % (q, s))
        keys += list(self.extra_sems)
        sems = {k: es.enter_context(nc.semaphore(k)) for k in keys}
        block = es.enter_context(nc.Block())
        slot_uses = self.slot_uses

        def run(e, name):
            for waits, fn, inc in self.ops[name]:
                for k, v in waits:
                    e.wait_ge(sems[k], v)
                if fn is None:
                    continue
                ins = fn(e)
                if inc is not None:
                    if inc[1] is None:
                        ins.then_inc(sems[inc[0]])
                    else:
                        ins.then_inc(sems[inc[0]], inc[1])
            if name in slot_uses:
                for s, u in enumerate(slot_uses[name]):
                    if u:
                        e.wait_ge(sems["d_%s_%d" % (name, s)], u * 16)

        @block.tensor
        def _(e):
            run(e, "pe")

        @block.vector
        def _(e):
            run(e, "dve")

        @block.scalar
        def _(e):
            run(e, "act")

        @block.gpsimd
        def _(e):
            run(e, "pool")

        @block.sync
        def _(e):
            run(e, "sp")


class K:
    def __init__(self, n_layers=DEPTH, n_shards=NCORE, debug=(), lazy=False, layers=None):
        self.lazy = lazy
        self.layers = list(range(n_layers)) if layers is None else list(layers)
        self.n_layers = n_layers
        self.n_shards = n_shards
        self.debug = set(debug)
        self.nc = bass.Bass("TRN2", target_bir_lowering=False)
        self.p = Prog(self.nc)
        self.inputs = {}
        self.dbuf = {}
        self.es = ExitStack()

    def ext_in(self, name, shape, dtype=F32):
        t = self.nc.dram_tensor(name, list(shape), dtype, kind="ExternalInput").ap()
        self.inputs[name] = (tuple(shape), dtype)
        return t

    def ext_out(self, name, shape, dtype=F32):
        return self.nc.dram_tensor(name, list(shape), dtype, kind="ExternalOutput").ap()

    def scratch(self, name, shape, dtype):
        return self.nc.dram_tensor(name, list(shape), dtype).ap()

    def weight(self, name, rows, cols, cast=True):
        p = self.p
        ns = self.n_shards
        if ns == 0:
            if not hasattr(self, "wspec"):
                self.wspec = {}
            self.wspec[name] = (rows, cols, cast)
            return self.ext_in(name, (rows, cols), BF16), Buf()
        rs = rows // ns
        src = self.ext_in(name, (rs, cols), F32 if cast else BF16)
        full = self.scratch(name + "_g", (rows, cols), BF16)
        b = Buf()
        if ns == 1:
            dst = full
        else:
            dst = self.scratch(name + "_s", (rs, cols), BF16)
        bs = Buf()
        step = 512
        for r0 in range(0, rs, step):
            r1 = min(rs, r0 + step)
            p.dma(dst[r0:r1, :], src[r0:r1, :], (), (bs if ns > 1 else b,), q="pool")
        if ns > 1:
            p.collective("AllGather", dst, full, (bs,), (b,))
        return full, b

    def B(self, name, *idx):
        key = (name,) + idx
        b = self.dbuf.get(key)
        if b is None:
            b = self.dbuf[key] = Buf()
        return b

    def tokbufs(self, name, s, t0, t1):
        return [self.B(name, s, i) for i in range(t0 // 256, (t1 + 255) // 256)]

    def setup(self):
        nc, p, es = self.nc, self.p, self.es
        p.arena = es.enter_context(nc.sbuf_tensor("arena", [128, SB_BYTES], U8))
        ps = es.enter_context(nc.psum_tensor("ps", [128, 8, 512], F32))
        self.ps = ps
        self.psb = bufs(8)
        self.x_in = self.ext_in("x", (NSEQ * LL, D))
        self.ctx_in = self.ext_in("ctx", (NSEQ * LC, D))
        self.cT_in = self.ext_in("cT", (128, KC * 3))
        self.consts_in = self.ext_in("consts", (128, 128))
        self.X = self.scratch("X", (NSEQ * D, T), F32)
        self.H = self.scratch("H", (NSEQ * D, T), BF16)
        self.PF = self.scratch("PF", (NSEQ * NFM * 128, T), F32)
        self.PT = self.scratch("PT", (NSEQ * T, NTM), F32)
        self.Y = self.scratch("Y", (NSEQ * 2560, T), BF16)
        self.G = self.scratch("G", (NSEQ * 4 * D, T), BF16)
        self.M = self.scratch("M", (NSEQ * D, T), BF16)
        self.ident_f = p.tile([128, 128], F32)
        self.ident_b = p.tile([128, 128], BF16)
        self.ones_f = p.tile([128, 128], F32)
        self.ones_b = p.tile([128, 128], BF16)
        self.eps_t = p.tile([128, 1], F32)
        self.one_t = p.tile([128, 1], F32)
        self.negpi_t = p.tile([128, 1], F32)
        self.cb = Buf()
        p.dma(self.ident_f, self.consts_in[:, :], (), (self.cb,))
        p.copy(self.ident_b, self.ident_f, (self.cb,), (self.cb,))
        p.memset(self.ones_f, 1.0, (self.cb,))
        p.memset(self.ones_b, 1.0, (self.cb,))
        p.memset(self.eps_t, EPS, (self.cb,))
        p.memset(self.one_t, 1.0, (self.cb,))
        p.memset(self.negpi_t, -PI, (self.cb,))
        self.sp_in = {}
        self.sT = p.tile([128, KC, 3], BF16)
        self.normg = p.tile([128, 2 * DEPTH * KC + KC], F32)
        self.adab = p.tile([128, DEPTH, 96], F32)
        self.modv = p.tile([128, 96, 3], F32)
        self.A1 = p.tile([128, KC, 3], F32)
        self.A2 = p.tile([128, KC, 3], F32)
        self.modb = Buf()
        normg_in = self.ext_in("normg", (128, 2 * DEPTH * KC + KC))
        adab_in = self.ext_in("adab", (128, DEPTH * 96))
        p.dma(self.normg, normg_in[:, :], (), (self.cb,))
        p.dma(self.adab, adab_in.rearrange("p (l j) -> p l j", l=DEPTH), (), (self.cb,))
        ct = p.tile([128, KC, 3], F32)
        p.dma(ct, self.cT_in.rearrange("p (j b) -> p j b", b=3), (), (self.cb,))
        p.act(self.sT, ct, AF.Silu, (self.cb,), (self.cb,))
        self.keep = p.sb_off

    def new_stage(self):
        self.p.barrier()
        self.p.reset_sbuf(self.keep)

    def stage_input(self):
        p = self.p
        self.new_stage()
        ps = self.ps
        xin = [p.tile([128, D], F32) for _ in range(2)]
        xin_b = bufs(2)
        xT = [p.tile([128, KC, 512], F32) for _ in range(2)]
        xT_b = bufs(2)
        it = 0
        ig = 0
        for s in range(NSEQ):
            groups = [(self.ctx_in, s * LC, 0, 2)] + [(self.x_in, s * LL + 512 * g, LC + 512 * g, 4) for g in range(4)]
            for (src, r0, t0, ntile) in groups:
                xt, xtb = xT[ig % 2], xT_b[ig % 2]
                ig += 1
                for tt in range(ntile):
                    xi, xib = xin[it % 2], xin_b[it % 2]
                    it += 1
                    p.dma(xi, src[r0 + tt * 128:r0 + (tt + 1) * 128, :], (), (xib,))
                    for bq in range(4):
                        bank = (it % 2) * 4 + bq
                        for c in range(4):
                            j = bq * 4 + c
                            p.tr(ps[:, bank, c * 128:(c + 1) * 128], xi[:, j * 128:(j + 1) * 128], self.ident_f,
                                 (xib, self.cb), (self.psb[bank],))
                        dst = xt[:, bq * 4:(bq + 1) * 4, tt * 128:(tt + 1) * 128]
                        srcp = ps[:, bank, :].rearrange("p (c t) -> p c t", c=4)
                        p.copy(dst, srcp, (self.psb[bank],), (xtb,), eng=("act" if bq % 2 else "dve"))
                n = ntile * 128
                Xs = self.X[s * D:(s + 1) * D, :].rearrange("(j p) t -> p j t", p=128)
                p.dma(Xs[:, :, t0:t0 + n], xt[:, :, 0:n], (xtb,), self.tokbufs("X", s, t0, t0 + n))

    def stage_mod(self, l):
        p = self.p
        self.new_stage()
        ps = self.ps
        W, wb = self.Wt("ada", l)
        Wv = W.rearrange("(kc p) n -> p kc n", p=128)
        wt = [p.tile([128, KC, 512], BF16) for _ in range(2)]
        wtb = bufs(2)
        for ti in range(24):
            w_sb, w_b = wt[ti % 2], wtb[ti % 2]
            p.dma(w_sb, Wv[:, :, ti * 512:(ti + 1) * 512], (wb,), (w_b,))
            for c in range(4):
                j = ti * 4 + c
                for k in range(KC):
                    p.mm(ps[:, 0, j * 3:(j + 1) * 3], w_sb[:, k, c * 128:(c + 1) * 128], self.sT[:, k, :],
                         k == 0, k == KC - 1, (w_b, self.cb), (self.psb[0],))
        pv = ps[:, 0, 0:288].rearrange("p (j b) -> p j b", b=3)
        p.tt(self.modv, pv, self.adab[:, l, :].unsqueeze(2).broadcast_to([128, 96, 3]), ALU.add,
             (self.psb[0], self.cb), (self.modb,))
        for (A, g0, m0) in ((self.A1, l * KC, 16), (self.A2, (DEPTH + l) * KC, 64)):
            p.ts(A, self.modv[:, m0:m0 + KC, :], 1.0, None, ALU.add, None, (self.modb,), (self.modb,))
            p.tt(A, A, self.normg[:, g0:g0 + KC].unsqueeze(2).broadcast_to([128, KC, 3]), ALU.mult,
                 (self.modb, self.cb), (self.modb,))

    def seq_blocks(self, s):
        return [(0, LC, 2)] + [(LC + 512 * i, 512, s) for i in range(4)]

    def stage_norm(self, l, which, router=None):
        p = self.p
        self.new_stage()
        ps = self.ps
        A = self.A1 if which == 0 else self.A2
        m0 = 0 if which == 0 else 48
        x_sb = p.tile([128, KC, 512], F32)
        sq_sb = p.tile([128, KC, 512], F32)
        h_sb = p.tile([128, KC, 512], BF16)
        rs_sb = p.tile([128, 512], F32)
        xb, sqb, hb, rsb = bufs(4)
        if router is not None:
            f_sb = p.tile([128, KC, 512], F32)
            fb = Buf()
            self.router_setup(router)
        for s in range(NSEQ):
            Xs = self.X[s * D:(s + 1) * D, :].rearrange("(j p) t -> p j t", p=128)
            Hs = self.H[s * D:(s + 1) * D, :].rearrange("(j p) t -> p j t", p=128)
            for (t0, n, col) in self.seq_blocks(s):
                p.dma(x_sb[:, :, 0:n], Xs[:, :, t0:t0 + n], self.tokbufs("X", s, t0, t0 + n), (xb,))
                p.act(sq_sb[:, :, 0:n], x_sb[:, :, 0:n], AF.Square, (xb,), (sqb,))
                for j in range(KC):
                    p.mm(ps[:, 0, 0:n], self.ones_f, sq_sb[:, j, 0:n], j == 0, j == KC - 1, (sqb, self.cb), (self.psb[0],))
                p.act(rs_sb[:, 0:n], ps[:, 0, 0:n], AF.Sqrt, (self.psb[0], self.cb), (rsb,), bias=self.eps_t, scale=1.0 / D)
                p.recip(rs_sb[:, 0:n], rs_sb[:, 0:n], (rsb,), (rsb,))
                p.tt(sq_sb[:, :, 0:n], x_sb[:, :, 0:n], rs_sb[:, 0:n].unsqueeze(1).broadcast_to([128, KC, n]), ALU.mult,
                     (xb, rsb), (sqb,))
                for j in range(KC):
                    if router is None:
                        p.act(h_sb[:, j, 0:n], sq_sb[:, j, 0:n], AF.Identity, (sqb, self.modb), (hb,),
                              bias=self.modv[:, m0 + j, col:col + 1], scale=A[:, j, col:col + 1])
                    else:
                        p.act(f_sb[:, j, 0:n], sq_sb[:, j, 0:n], AF.Identity, (sqb, self.modb), (fb,),
                              bias=self.modv[:, m0 + j, col:col + 1], scale=A[:, j, col:col + 1])
                if router is not None:
                    p.copy(h_sb[:, :, 0:n], f_sb[:, :, 0:n], (fb,), (hb,))
                    self.router_block(router, s, t0, n, f_sb, fb)
                p.dma(Hs[:, :, t0:t0 + n], h_sb[:, :, 0:n], (hb,), self.tokbufs("H", s, t0, t0 + n))

    WSPEC = {"ada": ("ada_w%d", D, 6 * D), "in": ("w_in%d", D, IN_W), "gate": ("w_gate%d", 4 * D, D),
             "branch": ("w_branch%d", 2560, D), "out": ("w_out%d", D, D)}

    def Wt(self, kind, l=0, e=0):
        key = (kind, l, e)
        if not hasattr(self, "_w"):
            self._w = {}
        if key in self._w:
            return self._w[key]
        j = l // 2
        if kind in self.WSPEC:
            nm, r, c = self.WSPEC[kind]
            v = self.weight(nm % l, r, c)
        elif kind in ("f1", "f3"):
            v = self.weight("ffn_w%s_%d" % (kind[1], j), D, FF)
        elif kind == "f2":
            v = self.weight("ffn_w2_%d" % j, FF, D)
        elif kind in ("m1", "m3"):
            v = self.weight("moe_w%s_%d_%d" % (kind[1], j, e), D, FFE)
        elif kind == "m2":
            v = self.weight("moe_w2_%d_%d" % (j, e), FFE, D)
        elif kind.startswith("dft"):
            v = self.weight("dft_" + kind[3:], LL, LL, cast=False)
        else:
            raise KeyError(kind)
        self._w[key] = v
        return v

    def declare_weights(self):
        for l in self.layers:
            self.Wt("ada", l)
            self.Wt("in", l)
            if l == self.layers[0]:
                for nm in ("cf", "sf", "ci", "si"):
                    self.Wt("dft" + nm)
            self.Wt("gate", l)
            self.Wt("branch", l)
            self.Wt("out", l)
            if l % 2 == 0:
                for kd in ("f1", "f3", "f2"):
                    self.Wt(kd, l)
            else:
                for e in range(NEXP):
                    for kd in ("m1", "m3", "m2"):
                        self.Wt(kd, l, e)

    def dump(self, name, ap, shape, dtype):
        o = self.ext_out("dbg_" + name, shape, dtype)
        self.p.barrier()
        self.p.dma(o, ap, (), ())

    def small(self, name, ncols, dtype=F32):
        if name not in self.sp_in:
            self.sp_in[name] = self.ext_in(name, (128, ncols), dtype)
        return self.sp_in[name]

    def load_small(self, name, ncols, shape=None):
        p = self.p
        t = p.tile([128, ncols], F32)
        b = Buf()
        p.dma(t, self.small(name, ncols)[:, :], (), (b,))
        return t, b

    def load_hseq(self, src, s, nch, t_sb, blkbufs, name):
        p = self.p
        v = src.rearrange("(j p) t -> p j t", p=128)
        for bi, (t0, n, col) in enumerate(self.seq_blocks(s)):
            p.dma(t_sb[:, :, t0:t0 + n], v[:, :, t0:t0 + n], self.tokbufs(name, s, t0, t0 + n), (blkbufs[bi],))

    PROJ_TILES = [(0, 512, 0), (512, 512, 4), (1024, 512, 8), (1536, 512, 12), (2048, 512, 16),
                  (3072, 512, 20), (3584, 512, 24), (4112, 512, 28), (4624, 512, 32), (5136, 256, 36)]

    def run_proj(self, l):
        p = self.p
        self.new_stage()
        ps, psb = self.ps, self.psb
        Win, winb = self.Wt("in", l)
        Wg, wgb = self.Wt("gate", l)
        Winv = Win.rearrange("(kc p) n -> p kc n", p=128)
        Wgv = Wg.rearrange("(i kc p) n -> p i kc n", i=4, p=128)
        bg, bgb = self.load_small("bgate", DEPTH * 4 * KC)
        h_sb = p.tile([128, KC, T], BF16)
        hblk = bufs(5)
        wt = [p.tile([128, KC, 512], BF16) for _ in range(2)]
        wtb = bufs(2)
        st32 = [p.tile([128, T], F32) for _ in range(2)]
        st32b = bufs(2)
        st16 = [p.tile([128, T], BF16) for _ in range(2)]
        st16b = bufs(2)
        wz = p.tile([128, KC, 512], BF16)
        wdt = p.tile([128, KC, 16], BF16)
        wv = p.tile([128, KC, 256], BF16)
        wtmb = Buf()
        pt = [p.tile([128, NTM], F32) for _ in range(2)]
        ptb = bufs(2)
        p.dma(wz, Winv[:, :, OFF_SSD:OFF_SSD + 512], (winb,), (wtmb,))
        p.dma(wdt, Winv[:, :, OFF_ATT - 16:OFF_ATT], (winb,), (wtmb,))
        p.dma(wv, Winv[:, :, IN_W - 256:IN_W], (winb,), (wtmb,))
        tiles = [("in",) + t for t in self.PROJ_TILES] + [("gate", i, c0) for i in range(4) for c0 in range(0, D, 512)]
        bank = [0]

        def load(ti):
            tl = tiles[ti]
            w_sb, w_b = wt[ti % 2], wtb[ti % 2]
            if tl[0] == "in":
                p.dma(w_sb[:, :, 0:tl[2]], Winv[:, :, tl[1]:tl[1] + tl[2]], (winb,), (w_b,))
            else:
                p.dma(w_sb, Wgv[:, tl[1], :, tl[2]:tl[2] + 512], (wgb,), (w_b,))

        nst = [0]
        for s in range(NSEQ):
            self.load_hseq(self.H[s * D:(s + 1) * D, :], s, KC, h_sb, hblk, "H")
            load(0)
            for ti, tl in enumerate(tiles):
                if ti + 1 < len(tiles):
                    load(ti + 1)
                w_sb, w_b = wt[ti % 2], wtb[ti % 2]
                nch = (tl[2] if tl[0] == "in" else 512) // 128
                for c in range(nch):
                    si = nst[0] % 2
                    nst[0] += 1
                    for bi, (t0, n, col) in enumerate(self.seq_blocks(s)):
                        bk = bank[0] % 6
                        bank[0] += 1
                        for k in range(KC):
                            p.mm(ps[:, bk, 0:n], w_sb[:, k, c * 128:(c + 1) * 128], h_sb[:, k, t0:t0 + n],
                                 k == 0, k == KC - 1, (w_b, hblk[bi]), (psb[bk],))
                        if tl[0] == "in":
                            p.copy(st32[si][:, t0:t0 + n], ps[:, bk, 0:n], (psb[bk],), (st32b[si],),
                                   eng=("act" if bi % 2 else "dve"))
                        else:
                            dc = tl[2] // 128 + c
                            o = ((l * 4 + tl[1]) * KC + dc)
                            p.act(st16[si][:, t0:t0 + n], ps[:, bk, 0:n], AF.Sigmoid, (psb[bk], bgb), (st16b[si],),
                                  bias=bg[:, o:o + 1])
                    if tl[0] == "in":
                        ch = tl[3] + c
                        r0 = (s * NFM + ch) * 128
                        p.dma(self.PF[r0:r0 + 128, :], st32[si], (st32b[si],), (self.B("PF", s, ch),))
                    else:
                        dc = tl[2] // 128 + c
                        r0 = ((s * 4 + tl[1]) * KC + dc) * 128
                        p.dma(self.G[r0:r0 + 128, :], st16[si], (st16b[si],), (self.B("G", s, tl[1], dc),))
            for tt in range(T // 128):
                bi = 0 if tt < 2 else 1 + (tt - 2) // 4
                pi = tt % 2
                for k in range(KC):
                    p.mm(ps[:, 6, :], h_sb[:, k, tt * 128:(tt + 1) * 128], wz[:, k, :], k == 0, k == KC - 1,
                         (hblk[bi], wtmb), (psb[6],))
                for k in range(KC):
                    p.mm(ps[:, 7, 0:16], h_sb[:, k, tt * 128:(tt + 1) * 128], wdt[:, k, :], k == 0, k == KC - 1,
                         (hblk[bi], wtmb), (psb[7],))
                for k in range(KC):
                    p.mm(ps[:, 7, 16:272], h_sb[:, k, tt * 128:(tt + 1) * 128], wv[:, k, :], k == 0, k == KC - 1,
                         (hblk[bi], wtmb), (psb[7],))
                p.copy(pt[pi][:, 0:512], ps[:, 6, :], (psb[6],), (ptb[pi],), eng="act")
                p.copy(pt[pi][:, 512:NTM], ps[:, 7, 0:272], (psb[7],), (ptb[pi],))
                r0 = s * T + tt * 128
                p.dma(self.PT[r0:r0 + 128, :], pt[pi], (ptb[pi],), (self.B("PT", s, tt),))

    BR_K = [(0, 4), (4, 8), (8, 12), (12, 20)]

    def run_merge(self, l):
        p = self.p
        self.new_stage()
        ps, psb = self.ps, self.psb
        Wb, wbb = self.Wt("branch", l)
        Wbv = Wb.rearrange("(kc p) n -> p kc n", p=128)
        y_sb = p.tile([128, 20, T], BF16)
        yblk = bufs(5)
        wt = [p.tile([128, 20, 512], BF16) for _ in range(2)]
        wtb = bufs(2)
        g_sb = [p.tile([128, 4, 512], BF16) for _ in range(2)]
        gb = bufs(2)
        m_sb = [p.tile([128, 512], F32) for _ in range(2)]
        mb = bufs(2)
        tmp = [p.tile([128, 512], F32) for _ in range(3)]
        tmpb = bufs(3)
        st16 = [p.tile([128, T], BF16) for _ in range(2)]
        st16b = bufs(2)
        it = 0
        for s in range(NSEQ):
            self.load_hseq(self.Y[s * 2560:(s + 1) * 2560, :], s, 20, y_sb, yblk, "Y")
            Gs = self.G[s * 4 * D:(s + 1) * 4 * D, :].rearrange("(i c p) t -> p i c t", i=4, c=KC)
            p.dma(wt[0], Wbv[:, :, 0:512], (wbb,), (wtb[0],))
            for ti in range(4):
                if ti + 1 < 4:
                    p.dma(wt[(ti + 1) % 2], Wbv[:, :, (ti + 1) * 512:(ti + 2) * 512], (wbb,), (wtb[(ti + 1) % 2],))
                w_sb, w_b = wt[ti % 2], wtb[ti % 2]
                for c in range(4):
                    dc = ti * 4 + c
                    si = dc % 2
                    for bi, (t0, n, col) in enumerate(self.seq_blocks(s)):
                        gi = it % 2
                        it += 1
                        p.dma(g_sb[gi][:, :, 0:n], Gs[:, :, dc, t0:t0 + n], [self.B("G", s, i, dc) for i in range(4)], (gb[gi],))
                        for i, (k0, k1) in enumerate(self.BR_K):
                            bk = (it % 2) * 4 + i
                            for k in range(k0, k1):
                                p.mm(ps[:, bk, 0:n], w_sb[:, k, c * 128:(c + 1) * 128], y_sb[:, k, t0:t0 + n],
                                     k == k0, k == k1 - 1, (w_b, yblk[bi]), (psb[bk],))
                        m = m_sb[gi]
                        bk0 = (it % 2) * 4
                        p.tt(m[:, 0:n], g_sb[gi][:, 0, 0:n], ps[:, bk0, 0:n], ALU.mult, (gb[gi], psb[bk0]), (mb[gi],))
                        for i in range(1, 4):
                            p.tt(tmp[i - 1][:, 0:n], g_sb[gi][:, i, 0:n], ps[:, bk0 + i, 0:n], ALU.mult,
                                 (gb[gi], psb[bk0 + i]), (tmpb[i - 1],))
                        p.tt(m[:, 0:n], m[:, 0:n], tmp[0][:, 0:n], ALU.add, (mb[gi], tmpb[0]), (mb[gi],), eng="pool")
                        p.tt(tmp[1][:, 0:n], tmp[1][:, 0:n], tmp[2][:, 0:n], ALU.add, (tmpb[1], tmpb[2]), (tmpb[1],), eng="pool")
                        p.tt(st16[si][:, t0:t0 + n], m[:, 0:n], tmp[1][:, 0:n], ALU.add, (mb[gi], tmpb[1]), (st16b[si],), eng="pool")
                    r0 = s * D + dc * 128
                    p.dma(self.M[r0:r0 + 128, :], st16[si], (st16b[si],), (self.B("M", s, dc),))

    def run_out(self, l):
        p = self.p
        self.new_stage()
        ps, psb = self.ps, self.psb
        Wo, wob = self.Wt("out", l)
        Wov = Wo.rearrange("(kc p) n -> p kc n", p=128)
        m_sb = p.tile([128, KC, T], BF16)
        mblk = bufs(5)
        wt = [p.tile([128, KC, 512], BF16) for _ in range(2)]
        wtb = bufs(2)
        xr = [p.tile([128, T], F32) for _ in range(2)]
        xrb = bufs(2)
        bank = 0
        for s in range(NSEQ):
            v = self.M[s * D:(s + 1) * D, :].rearrange("(j p) t -> p j t", p=128)
            for bi, (t0, n, col) in enumerate(self.seq_blocks(s)):
                p.dma(m_sb[:, :, t0:t0 + n], v[:, :, t0:t0 + n], [self.B("M", s, dc) for dc in range(KC)], (mblk[bi],))
            allx = self.tokbufs("X", s, 0, T)
            p.dma(wt[0], Wov[:, :, 0:512], (wob,), (wtb[0],))
            for ti in range(4):
                if ti + 1 < 4:
                    p.dma(wt[(ti + 1) % 2], Wov[:, :, (ti + 1) * 512:(ti + 2) * 512], (wob,), (wtb[(ti + 1) % 2],))
                w_sb, w_b = wt[ti % 2], wtb[ti % 2]
                for c in range(4):
                    dc = ti * 4 + c
                    xi = dc % 2
                    r0 = s * D + dc * 128
                    p.dma(xr[xi], self.X[r0:r0 + 128, :], allx, (xrb[xi],))
                    for bi, (t0, n, col) in enumerate(self.seq_blocks(s)):
                        bk = bank % 8
                        bank += 1
                        for k in range(KC):
                            p.mm(ps[:, bk, 0:n], w_sb[:, k, c * 128:(c + 1) * 128], m_sb[:, k, t0:t0 + n],
                                 k == 0, k == KC - 1, (w_b, mblk[bi]), (psb[bk],))
                        p.stt(xr[xi][:, t0:t0 + n], ps[:, bk, 0:n], self.modv[:, 32 + dc, col:col + 1], xr[xi][:, t0:t0 + n],
                              ALU.mult, ALU.add, (psb[bk], self.modb, xrb[xi]), (xrb[xi],))
                    p.dma(self.X[r0:r0 + 128, :], xr[xi], (xrb[xi],), allx)

    def ffn_groups(self, s):
        out = []
        for g0, n in ((0, 512), (512, 512), (1024, 512), (1536, 512), (2048, 256)):
            subs = [(0, 256, 2), (256, 256, s)] if g0 == 0 else [(0, n, s)]
            out.append((g0, n, subs))
        return out

    def run_ffn(self, l):
        if l % 2 == 1:
            return self.run_moe(l)
        p = self.p
        self.new_stage()
        ps, psb = self.ps, self.psb
        W1, w1b = self.Wt("f1", l)
        W3, w3b = self.Wt("f3", l)
        W2, w2b = self.Wt("f2", l)
        W1v = W1.rearrange("(kc p) n -> p kc n", p=128)
        W3v = W3.rearrange("(kc p) n -> p kc n", p=128)
        W2v = W2.rearrange("(kc p) n -> p kc n", p=128)
        NFC = FF // 128
        f_sb = p.tile([128, KC, 512], BF16)
        fbuf = Buf()
        w1t = [p.tile([128, KC, 256], BF16) for _ in range(2)]
        w3t = [p.tile([128, KC, 256], BF16) for _ in range(2)]
        w13b = bufs(2)
        a_sb = p.tile([128, NFC, 512], BF16)
        ab = bufs(NFC)
        w2t = [p.tile([128, NFC, 256], BF16) for _ in range(2)]
        w2tb = bufs(2)
        tmp = [p.tile([128, 512], F32) for _ in range(2)]
        tmpb = bufs(2)
        xt = [p.tile([128, 512], F32) for _ in range(2)]
        xtb = bufs(2)
        t1 = [(c0, min(256, FF - c0)) for c0 in range(0, FF, 256)]
        it = 0
        for s in range(NSEQ):
            Hs = self.H[s * D:(s + 1) * D, :].rearrange("(j p) t -> p j t", p=128)
            for (g0, n, subs) in self.ffn_groups(s):
                p.dma(f_sb[:, :, 0:n], Hs[:, :, g0:g0 + n], self.tokbufs("H", s, g0, g0 + n), (fbuf,))

                def ld1(ti):
                    c0, nc_ = t1[ti]
                    p.dma(w1t[ti % 2][:, :, 0:nc_], W1v[:, :, c0:c0 + nc_], (w1b,), (w13b[ti % 2],))
                    p.dma(w3t[ti % 2][:, :, 0:nc_], W3v[:, :, c0:c0 + nc_], (w3b,), (w13b[ti % 2],))

                def ld2(ti):
                    p.dma(w2t[ti % 2], W2v[:, :, ti * 256:(ti + 1) * 256], (w2b,), (w2tb[ti % 2],))

                ld1(0)
                for ti, (c0, nc_) in enumerate(t1):
                    if ti + 1 < len(t1):
                        ld1(ti + 1)
                    else:
                        ld2(0)
                    for c in range(nc_ // 128):
                        ffc = c0 // 128 + c
                        ba, bb = (it % 2) * 2, (it % 2) * 2 + 1
                        it += 1
                        for k in range(KC):
                            p.mm(ps[:, ba, 0:n], w1t[ti % 2][:, k, c * 128:(c + 1) * 128], f_sb[:, k, 0:n], k == 0, k == KC - 1,
                                 (w13b[ti % 2], fbuf), (psb[ba],))
                        for k in range(KC):
                            p.mm(ps[:, bb, 0:n], w3t[ti % 2][:, k, c * 128:(c + 1) * 128], f_sb[:, k, 0:n], k == 0, k == KC - 1,
                                 (w13b[ti % 2], fbuf), (psb[bb],))
                        tb = it % 2
                        p.act(tmp[tb][:, 0:n], ps[:, ba, 0:n], AF.Silu, (psb[ba],), (tmpb[tb],))
                        p.tt(a_sb[:, ffc, 0:n], tmp[tb][:, 0:n], ps[:, bb, 0:n], ALU.mult, (tmpb[tb], psb[bb]), (ab[ffc],))
                for ti in range(8):
                    if ti + 1 < 8:
                        ld2(ti + 1)
                    for c in range(2):
                        dc = ti * 2 + c
                        bk = 4 + dc % 4
                        xi = dc % 2
                        r0 = s * D + dc * 128
                        xbs = self.tokbufs("X", s, g0, g0 + n)
                        p.dma(xt[xi][:, 0:n], self.X[r0:r0 + 128, g0:g0 + n], xbs, (xtb[xi],))
                        for k in range(NFC):
                            p.mm(ps[:, bk, 0:n], w2t[ti % 2][:, k, c * 128:(c + 1) * 128], a_sb[:, k, 0:n], k == 0, k == NFC - 1,
                                 (w2tb[ti % 2], ab[k]), (psb[bk],))
                        for (o, m, col) in subs:
                            p.stt(xt[xi][:, o:o + m], ps[:, bk, o:o + m], self.modv[:, 80 + dc, col:col + 1], xt[xi][:, o:o + m],
                                  ALU.mult, ALU.add, (psb[bk], self.modb, xtb[xi]), (xtb[xi],))
                        p.dma(self.X[r0:r0 + 128, g0:g0 + n], xt[xi][:, 0:n], (xtb[xi],), xbs)

    def run_moe(self, l):
        p = self.p
        self.new_stage()
        ps, psb = self.ps, self.psb
        NFC = FFE // 128
        f_sb = p.tile([128, KC, 512], BF16)
        fbuf = Buf()
        w1t = [p.tile([128, KC, 256], BF16) for _ in range(2)]
        w3t = [p.tile([128, KC, 256], BF16) for _ in range(2)]
        w13b = bufs(2)
        a_sb = p.tile([128, NFC, 512], BF16)
        ab = bufs(NFC)
        w2t = [p.tile([128, NFC, 256], BF16) for _ in range(2)]
        w2tb = bufs(2)
        acc = p.tile([128, KC, 512], F32)
        accb = bufs(KC)
        tmp = [p.tile([128, 512], F32) for _ in range(2)]
        tmpb = bufs(2)
        tmp2 = [p.tile([128, 512], F32) for _ in range(2)]
        tmp2b = bufs(2)
        gt = [p.tile([128, 512], F32) for _ in range(2)]
        gtb = bufs(2)
        xt = [p.tile([128, 512], F32) for _ in range(2)]
        xtb = bufs(2)
        GF = self.GATES[l]
        it = 0
        for s in range(NSEQ):
            Hs = self.H[s * D:(s + 1) * D, :].rearrange("(j p) t -> p j t", p=128)
            for (g0, n, subs) in self.ffn_groups(s):
                p.dma(f_sb[:, :, 0:n], Hs[:, :, g0:g0 + n], self.tokbufs("H", s, g0, g0 + n), (fbuf,))
                for e in range(NEXP):
                    W1, w1b = self.Wt("m1", l, e)
                    W3, w3b = self.Wt("m3", l, e)
                    W2, w2b = self.Wt("m2", l, e)
                    W1v = W1.rearrange("(kc p) n -> p kc n", p=128)
                    W3v = W3.rearrange("(kc p) n -> p kc n", p=128)
                    W2v = W2.rearrange("(kc p) n -> p kc n", p=128)
                    gi = e % 2
                    p.dma(gt[gi][:, 0:n], GF[e, s * T + g0:s * T + g0 + n].partition_broadcast(128),
                          self.tokbufs("GATES%d" % l, s, g0, g0 + n), (gtb[gi],))

                    def ld1(ti):
                        p.dma(w1t[ti % 2], W1v[:, :, ti * 256:(ti + 1) * 256], (w1b,), (w13b[ti % 2],))
                        p.dma(w3t[ti % 2], W3v[:, :, ti * 256:(ti + 1) * 256], (w3b,), (w13b[ti % 2],))

                    def ld2(ti):
                        p.dma(w2t[ti % 2], W2v[:, :, ti * 256:(ti + 1) * 256], (w2b,), (w2tb[ti % 2],))

                    ld1(0)
                    for ti in range(16):
                        if ti + 1 < 16:
                            ld1(ti + 1)
                        else:
                            ld2(0)
                        for c in range(2):
                            ffc = ti * 2 + c
                            ba, bb = (it % 2) * 2, (it % 2) * 2 + 1
                            it += 1
                            for k in range(KC):
                                p.mm(ps[:, ba, 0:n], w1t[ti % 2][:, k, c * 128:(c + 1) * 128], f_sb[:, k, 0:n], k == 0, k == KC - 1,
                                     (w13b[ti % 2], fbuf), (psb[ba],))
                            for k in range(KC):
                                p.mm(ps[:, bb, 0:n], w3t[ti % 2][:, k, c * 128:(c + 1) * 128], f_sb[:, k, 0:n], k == 0, k == KC - 1,
                                     (w13b[ti % 2], fbuf), (psb[bb],))
                            tb = it % 2
                            p.act(tmp[tb][:, 0:n], ps[:, ba, 0:n], AF.Silu, (psb[ba],), (tmpb[tb],))
                            p.tt(tmp2[tb][:, 0:n], tmp[tb][:, 0:n], ps[:, bb, 0:n], ALU.mult, (tmpb[tb], psb[bb]), (tmp2b[tb],))
                            p.tt(a_sb[:, ffc, 0:n], tmp2[tb][:, 0:n], gt[gi][:, 0:n], ALU.mult, (tmp2b[tb], gtb[gi]), (ab[ffc],), eng="pool")
                    for ti in range(8):
                        if ti + 1 < 8:
                            ld2(ti + 1)
                        for c in range(2):
                            dc = ti * 2 + c
                            bk = 4 + dc % 4
                            for k in range(NFC):
                                p.mm(ps[:, bk, 0:n], w2t[ti % 2][:, k, c * 128:(c + 1) * 128], a_sb[:, k, 0:n], k == 0, k == NFC - 1,
                                     (w2tb[ti % 2], ab[k]), (psb[bk],))
                            if e == 0:
                                p.copy(acc[:, dc, 0:n], ps[:, bk, 0:n], (psb[bk],), (accb[dc],))
                            else:
                                p.tt(acc[:, dc, 0:n], acc[:, dc, 0:n], ps[:, bk, 0:n], ALU.add, (accb[dc], psb[bk]), (accb[dc],))
                xbs = self.tokbufs("X", s, g0, g0 + n)
                for dc in range(KC):
                    xi = dc % 2
                    r0 = s * D + dc * 128
                    p.dma(xt[xi][:, 0:n], self.X[r0:r0 + 128, g0:g0 + n], xbs, (xtb[xi],))
                    for (o, m, col) in subs:
                        p.stt(xt[xi][:, o:o + m], acc[:, dc, o:o + m], self.modv[:, 80 + dc, col:col + 1], xt[xi][:, o:o + m],
                              ALU.mult, ALU.add, (accb[dc], self.modb, xtb[xi]), (xtb[xi],))
                    p.dma(self.X[r0:r0 + 128, g0:g0 + n], xt[xi][:, 0:n], (xtb[xi],), xbs)

    def router_setup(self, l):
        p = self.p
        j = l // 2
        if not hasattr(self, "GATES"):
            self.GATES = {}
        self.GATES[l] = self.scratch("GATES%d" % l, (NEXP, NSEQ * T), F32)
        self.wr, self.wrb = self.load_small("router%d" % j, KC * NEXP)
        self.rb = p.tile([128, NEXP], F32)
        rb_in = self.ext_in("router_b%d" % j, (1, NEXP))
        p.dma(self.rb, rb_in[0, :].partition_broadcast(128), (), (self.wrb,))
        self.r_t = [p.tile([128, NEXP], F32) for _ in range(6)]
        self.r_s = [p.tile([128, 1], F32) for _ in range(5)]
        self.r_b = Buf()
        self.gT = p.tile([NEXP, 512], F32)
        self.gTb = Buf()

    def router_block(self, l, s, t0, n, f_sb, fb):
        p = self.p
        ps, psb = self.ps, self.psb
        wr = self.wr.rearrange("p (k e) -> p k e", e=NEXP)
        lg, eq, l2, sel, ex, w = self.r_t
        m1, m2, nm1, sm, rs = self.r_s
        rb = self.r_b
        for tt in range(n // 128):
            for k in range(KC):
                p.mm(ps[:, 1, 0:NEXP], f_sb[:, k, tt * 128:(tt + 1) * 128], wr[:, k, :], k == 0, k == KC - 1,
                     (fb, self.wrb), (psb[1],))
            p.tt(lg, ps[:, 1, 0:NEXP], self.rb, ALU.add, (psb[1], self.wrb), (rb,))
            p.op("dve", lambda e: e.tensor_reduce(m1, lg, AX.X, ALU.max), (rb,), (rb,))
            p.ts(eq, lg, m1[:, 0:1], None, ALU.is_equal, None, (rb,), (rb,))
            p.stt(l2, eq, -1e30, lg, ALU.mult, ALU.add, (rb,), (rb,))
            p.op("dve", lambda e: e.tensor_reduce(m2, l2, AX.X, ALU.max), (rb,), (rb,))
            p.ts(sel, lg, m2[:, 0:1], None, ALU.is_ge, None, (rb,), (rb,))
            p.ts(nm1, m1, -1.0, None, ALU.mult, None, (rb,), (rb,))
            p.act(ex, lg, AF.Exp, (rb,), (rb,), bias=nm1[:, 0:1])
            p.tt(w, ex, sel, ALU.mult, (rb,), (rb,))
            p.op("dve", lambda e: e.tensor_reduce(sm, w, AX.X, ALU.add), (rb,), (rb,))
            p.recip(rs, sm, (rb,), (rb,))
            p.ts(w, w, rs[:, 0:1], None, ALU.mult, None, (rb,), (rb,))
            p.tr(ps[0:NEXP, 2, 0:128], w, self.ident_f, (rb, self.cb), (psb[2],))
            p.copy(self.gT[:, tt * 128:(tt + 1) * 128], ps[0:NEXP, 2, 0:128], (psb[2],), (self.gTb,))
        p.dma(self.GATES[l][:, s * T + t0:s * T + t0 + n], self.gT[:, 0:n], (self.gTb,),
              self.tokbufs("GATES%d" % l, s, t0, t0 + n))

    def stage_final(self):
        p = self.p
        self.new_stage()
        ps, psb = self.ps, self.psb
        out = self.ext_out("out", (NSEQ * LL, D))
        x_sb = p.tile([128, KC, 512], F32)
        sq_sb = p.tile([128, KC, 512], F32)
        rs_sb = p.tile([128, 512], F32)
        o_sb = [p.tile([128, D], F32) for _ in range(2)]
        xb, sqb, rsb = bufs(3)
        ob = bufs(2)
        gf0 = 2 * DEPTH * KC
        it = 0
        for s in range(NSEQ):
            Xs = self.X[s * D:(s + 1) * D, :].rearrange("(j p) t -> p j t", p=128)
            for g in range(4):
                t0 = LC + g * 512
                p.dma(x_sb, Xs[:, :, t0:t0 + 512], self.tokbufs("X", s, t0, t0 + 512), (xb,))
                p.act(sq_sb, x_sb, AF.Square, (xb,), (sqb,))
                for j in range(KC):
                    p.mm(ps[:, 0, :], self.ones_f, sq_sb[:, j, :], j == 0, j == KC - 1, (sqb, self.cb), (psb[0],))
                p.act(rs_sb, ps[:, 0, :], AF.Sqrt, (psb[0], self.cb), (rsb,), bias=self.eps_t, scale=1.0 / D)
                p.recip(rs_sb, rs_sb, (rsb,), (rsb,))
                for j in range(KC):
                    p.stt(sq_sb[:, j, :], x_sb[:, j, :], self.normg[:, gf0 + j:gf0 + j + 1], rs_sb, ALU.mult, ALU.mult,
                          (xb, rsb, self.cb), (sqb,))
                for tt in range(4):
                    oi = it % 2
                    it += 1
                    for bq in range(4):
                        bank = 1 + (it % 2) * 3 + (bq % 3) if False else 4 + bq
                        for c in range(4):
                            j = bq * 4 + c
                            p.tr(ps[:, bank, c * 128:(c + 1) * 128], sq_sb[:, j, tt * 128:(tt + 1) * 128], self.ident_f,
                                 (sqb, self.cb), (psb[bank],))
                        p.copy(o_sb[oi][:, bq * 512:(bq + 1) * 512], ps[:, bank, :], (psb[bank],), (ob[oi],),
                               eng=("act" if bq % 2 else "dve"))
                    r0 = s * LL + g * 512 + tt * 128
                    p.dma(out[r0:r0 + 128, :], o_sb[oi], (ob[oi],), ())

    def run_lru(self, l):
        p = self.p
        self.new_stage()
        ps, psb = self.ps, self.psb
        cw, cwb = self.load_small("lru_cw", DEPTH * 4 * 4)
        cbias, cbb = self.load_small("lru_cb", DEPTH * 4)
        ba, bab = self.load_small("lru_ba", DEPTH * 2 * 4)
        bx, bxb = self.load_small("lru_bx", DEPTH * 2 * 4)
        lam, lamb = self.load_small("lru_lam", DEPTH * 2 * 4)
        if "lru_wa" not in self.sp_in:
            self.sp_in["lru_wa"] = self.ext_in("lru_wa", (DEPTH * 2 * 8 * 64, 64))
            self.sp_in["lru_wx"] = self.ext_in("lru_wx", (DEPTH * 2 * 8 * 64, 64))
        stg = p.tile([128, 16, 128], F32)
        bd = p.tile([128, 16, 128], BF16)
        bdb = Buf()
        sgb = bufs(16)
        for i in range(16):
            p.memset(stg[:, i, :], 0.0, (sgb[i],), eng="pool")
        for d in range(2):
            for mi, nm in enumerate(("lru_wa", "lru_wx")):
                for j in range(4):
                    idx = (d * 2 + mi) * 4 + j
                    for half in range(2):
                        r0 = ((l * 2 + d) * 8 + 2 * j + half) * 64
                        p.dma(stg[half * 64:(half + 1) * 64, idx, half * 64:(half + 1) * 64], self.sp_in[nm][r0:r0 + 64, :],
                              (), (sgb[idx],))
        p.copy(bd, stg, sgb, (bdb,))
        nl = p.tile([128, 8], F32)
        nl2 = p.tile([128, 8], F32)
        p.act(nl, lam[:, l * 8:(l + 1) * 8], AF.Exp, (lamb,), (bdb,), scale=-1.0)
        p.act(nl, nl, AF.Ln, (bdb,), (bdb,), bias=self.one_t)
        p.ts(nl2, nl, -16.0, None, ALU.mult, None, (bdb,), (bdb,))
        p.ts(nl, nl, -8.0, None, ALU.mult, None, (bdb,), (bdb,))
        XP = T + 6
        gate = p.tile([128, T], F32)
        xp = p.tile([128, XP], F32)
        xc = p.tile([128, T], F32)
        xcb = p.tile([128, T], BF16)
        r_sb = p.tile([128, T], F32)
        i_sb = p.tile([128, T], F32)
        a_sb = p.tile([128, T], F32)
        t_sb = p.tile([128, T], F32)
        hf = p.tile([128, T], F32)
        hb = p.tile([128, T], F32)
        u_sb = p.tile([128, T], F32)
        y_sb = p.tile([128, T], BF16)
        gb_, xpb, xcbuf, xcbb, rb_, ib_, ab_, tb_, hfb, hbb, ub, yb = bufs(12)
        p.memset(xp, 0.0, (xpb,))
        bank = 0
        for s in range(NSEQ):
            for j in range(4):
                rg = (s * NFM + PF_LRU + j) * 128
                rx = (s * NFM + PF_LRU + 4 + j) * 128
                p.dma(gate, self.PF[rg:rg + 128, :], (self.B("PF", s, PF_LRU + j),), (gb_,))
                p.dma(xp[:, 2:2 + LC], self.PF[rx:rx + 128, 0:LC], (self.B("PF", s, PF_LRU + 4 + j),), (xpb,))
                p.dma(xp[:, 5 + LC:5 + T], self.PF[rx:rx + 128, LC:T], (self.B("PF", s, PF_LRU + 4 + j),), (xpb,))
                for (o0, n, base) in ((0, LC, 0), (LC, LL, LC + 3)):
                    for k in range(4):
                        w = cw[:, (l * 4 + k) * 4 + j:(l * 4 + k) * 4 + j + 1]
                        src = xp[:, base + k:base + k + n]
                        if k == 0:
                            p.ts(xc[:, o0:o0 + n], src, w, cbias[:, l * 4 + j:l * 4 + j + 1], ALU.mult, ALU.add,
                                 (xpb, cwb, cbb), (xcbuf,))
                        else:
                            p.stt(xc[:, o0:o0 + n], src, w, xc[:, o0:o0 + n], ALU.mult, ALU.add, (xpb, cwb, xcbuf), (xcbuf,))
                p.copy(xcb, xc, (xcbuf,), (xcbb,), eng="act")
                for d in range(2):
                    for (t0, n, col) in self.seq_blocks(s):
                        for mi, (dst, dstb, bias_t, bias_b) in enumerate(((r_sb, rb_, ba, bab), (i_sb, ib_, bx, bxb))):
                            bk = bank % 8
                            bank += 1
                            idx = (d * 2 + mi) * 4 + j
                            p.mm(ps[:, bk, 0:n], bd[:, idx, :], xcb[:, t0:t0 + n], True, True, (bdb, xcbb), (psb[bk],))
                            o = (l * 2 + d) * 4 + j
                            p.act(dst[:, t0:t0 + n], ps[:, bk, 0:n], AF.Sigmoid, (psb[bk], bias_b), (dstb,), bias=bias_t[:, o:o + 1])
                    p.act(a_sb, r_sb, AF.Exp, (rb_, bdb), (ab_,), scale=nl[:, d * 4 + j:d * 4 + j + 1])
                    p.act(t_sb, r_sb, AF.Exp, (rb_, bdb), (tb_,), scale=nl2[:, d * 4 + j:d * 4 + j + 1])
                    p.act(t_sb, t_sb, AF.Sqrt, (tb_, self.cb), (tb_,), bias=self.one_t, scale=-1.0)
                    p.tt(t_sb, t_sb, i_sb, ALU.mult, (tb_, ib_), (tb_,))
                    p.tt(t_sb, t_sb, xc, ALU.mult, (tb_, xcbuf), (tb_,), eng="pool")
                    if d == 0:
                        p.op("dve", lambda e: e.tensor_tensor_scan(hf, a_sb, t_sb, 0.0, ALU.mult, ALU.add), (ab_, tb_), (hfb,))
                    else:
                        p.op("dve", lambda e: e.tensor_tensor_scan(hb[:, 0:LC][:, ::-1], a_sb[:, 0:LC][:, ::-1], t_sb[:, 0:LC][:, ::-1],
                                                                  0.0, ALU.mult, ALU.add), (ab_, tb_), (hbb,))
                        p.op("dve", lambda e: e.tensor_tensor_scan(hb[:, LC:T][:, ::-1], a_sb[:, LC:T][:, ::-1], t_sb[:, LC:T][:, ::-1],
                                                                  hb[:, 0:1], ALU.mult, ALU.add), (ab_, tb_, hbb), (hbb,))
                p.tt(u_sb, gate, gate, ALU.mult, (gb_,), (ub,), eng="pool")
                p.ts(u_sb, u_sb, 0.044715, 1.0, ALU.mult, ALU.add, (ub,), (ub,), eng="pool")
                p.tt(u_sb, u_sb, gate, ALU.mult, (ub, gb_), (ub,), eng="pool")
                p.act(u_sb, u_sb, AF.Sigmoid, (ub,), (ub,), scale=1.5957691216057308)
                p.tt(u_sb, u_sb, gate, ALU.mult, (ub, gb_), (ub,), eng="pool")
                p.tt(hf, hf, hb, ALU.add, (hfb, hbb), (hfb,))
                p.tt(y_sb, u_sb, hf, ALU.mult, (ub, hfb), (yb,))
                r0 = s * 2560 + 512 + j * 128
                p.dma(self.Y[r0:r0 + 128, :], y_sb, (yb,), self.tokbufs("Y", s, 0, T))

    def run_att(self, l):
        p = self.p
        self.new_stage()
        ps, psb = self.ps, self.psb
        SC = 128.0 ** -0.5
        qn, qnb_ = self.load_small("att_qn", DEPTH)
        kn, knb_ = self.load_small("att_kn", DEPTH)
        cos_t, cosb = self.load_small("rope_cos", LL)
        sin_t, sinb = self.load_small("rope_sin", LL)
        rot32, rotb = self.load_small("rotm", 128)
        if "att_rows" not in self.sp_in:
            self.sp_in["att_rows"] = self.ext_in("att_rows", (1, DEPTH * 256))
        rot = p.tile([128, 128], BF16)
        p.copy(rot, rot32, (rotb,), (rotb,))
        gqs = p.tile([128, 1], F32)
        p.ts(gqs, qn[:, l:l + 1], SC, None, ALU.mult, None, (qnb_,), (qnb_,))
        rows = p.tile([1, 256], F32)
        mx = p.tile([1, 4], F32)
        nb = p.tile([128, 1], F32)
        nbb = Buf()
        p.dma(rows, self.sp_in["att_rows"][0:1, l * 256:(l + 1) * 256], (), (nbb,))
        p.op("dve", lambda e: e.tensor_reduce(mx[:, 0:2], rows.rearrange("p (a b) -> p a b", a=2), AX.X, ALU.max,
                                              apply_absolute_value=True), (nbb,), (nbb,))
        p.tt(mx[:, 2:3], mx[:, 0:1], mx[:, 1:2], ALU.mult, (nbb,), (nbb,))
        p.ts(mx[:, 3:4], mx[:, 2:3], -(128.0 ** 0.5), None, ALU.mult, None, (nbb,), (nbb,))
        p.mm(ps[:, 0, 0:1], self.ones_f[0:1, :], mx[:, 3:4], True, True, (nbb, self.cb), (psb[0],))
        p.copy(nb, ps[:, 0, 0:1], (psb[0],), (nbb,))
        x32 = p.tile([128, T], F32)
        sq = p.tile([128, T], F32)
        rs = p.tile([128, 512], F32)
        qn32 = p.tile([128, T], F32)
        qnb = p.tile([128, LL], BF16)
        t1 = p.tile([128, 512], F32)
        t2 = p.tile([128, 512], F32)
        qk = p.tile([128, 10, T], BF16)
        qkb = bufs(10)
        v32 = p.tile([128, 18, 256], F32)
        v_sb = p.tile([128, 18, 256], BF16)
        pT = [p.tile([128, 512], BF16) for _ in range(3)]
        pTb = bufs(3)
        rc = p.tile([128, 512], F32)
        ost = [p.tile([128, 512], BF16) for _ in range(2)]
        ostb = bufs(2)
        x32b, sqb, rsb, qn32b, qnbb, t1b, t2b, v32b, vb, rcb = bufs(10)
        bank = 0
        io = 0
        for s in range(NSEQ):
            for hh in range(10):
                r0 = (s * NFM + PF_ATT + hh) * 128
                g = gqs if hh < 8 else kn[:, l:l + 1]
                p.dma(x32, self.PF[r0:r0 + 128, :], (self.B("PF", s, PF_ATT + hh),), (x32b,))
                p.act(sq, x32, AF.Square, (x32b,), (sqb,))
                for (t0, n, col) in self.seq_blocks(s):
                    bk = bank % 4
                    bank += 1
                    p.mm(ps[:, bk, 0:n], self.ones_f, sq[:, t0:t0 + n], True, True, (sqb, self.cb), (psb[bk],))
                    p.act(rs[:, 0:n], ps[:, bk, 0:n], AF.Sqrt, (psb[bk], self.cb), (rsb,), bias=self.eps_t, scale=1.0 / 128)
                    p.recip(rs[:, 0:n], rs[:, 0:n], (rsb,), (rsb,))
                    p.stt(qn32[:, t0:t0 + n], x32[:, t0:t0 + n], g, rs[:, 0:n], ALU.mult, ALU.mult,
                          (x32b, rsb, qnb_, knb_), (qn32b,))
                p.copy(qk[:, hh, 0:LC], qn32[:, 0:LC], (qn32b,), (qkb[hh],), eng="act")
                p.copy(qnb, qn32[:, LC:T], (qn32b,), (qnbb,), eng="act")
                for gblk in range(4):
                    c0 = gblk * 512
                    bk = bank % 4
                    bank += 1
                    p.mm(ps[:, bk, :], rot, qnb[:, c0:c0 + 512], True, True, (rotb, qnbb), (psb[bk],))
                    p.tt(t1, qn32[:, LC + c0:LC + c0 + 512], cos_t[:, c0:c0 + 512], ALU.mult, (qn32b, cosb), (t1b,), eng="pool")
                    p.tt(t2, ps[:, bk, :], sin_t[:, c0:c0 + 512], ALU.mult, (psb[bk], sinb), (t2b,))
                    p.tt(qk[:, hh, LC + c0:LC + c0 + 512], t1, t2, ALU.add, (t1b, t2b), (qkb[hh],))
            PTs = self.PT[s * T:(s + 1) * T, :].rearrange("(c p) f -> p c f", p=128)
            p.dma(v32, PTs[:, :, TM_V:TM_V + 256], [self.B("PT", s, tt) for tt in range(18)], (v32b,))
            p.copy(v_sb, v32, (v32b,), (vb,))
            for h in range(8):
                g = h // 4
                for (q0, nq, nsc) in ((0, LC, 2), (LC, 512, 18), (LC + 512, 512, 18), (LC + 1024, 512, 18), (LC + 1536, 512, 18)):
                    bo, br = 4 + (io % 2) * 2, 5 + (io % 2) * 2
                    for sc in range(nsc):
                        bk = bank % 4
                        bank += 1
                        pi = bank % 3
                        p.mm(ps[:, bk, 0:nq], qk[:, 8 + g, sc * 128:(sc + 1) * 128], qk[:, h, q0:q0 + nq], True, True,
                             (qkb[8 + g], qkb[h]), (psb[bk],))
                        p.act(pT[pi][:, 0:nq], ps[:, bk, 0:nq], AF.Exp, (psb[bk], nbb), (pTb[pi],), bias=nb[:, 0:1])
                        p.mm(ps[:, bo, 0:nq], v_sb[:, sc, g * 128:(g + 1) * 128], pT[pi][:, 0:nq], sc == 0, sc == nsc - 1,
                             (vb, pTb[pi]), (psb[bo],))
                        p.mm(ps[:, br, 0:nq], self.ones_b, pT[pi][:, 0:nq], sc == 0, sc == nsc - 1,
                             (self.cb, pTb[pi]), (psb[br],))
                    p.recip(rc[:, 0:nq], ps[:, br, 0:nq], (psb[br],), (rcb,))
                    oi = io % 2
                    io += 1
                    p.tt(ost[oi][:, 0:nq], ps[:, bo, 0:nq], rc[:, 0:nq], ALU.mult, (psb[bo], rcb), (ostb[oi],))
                    r0 = s * 2560 + 1536 + h * 128
                    p.dma(self.Y[r0:r0 + 128, q0:q0 + nq], ost[oi][:, 0:nq], (ostb[oi],), self.tokbufs("Y", s, q0, q0 + nq))

    def run_ssd(self, l):
        p = self.p
        self.new_stage()
        ps, psb = self.ps, self.psb
        cw, cwb = self.load_small("ssd_cw", DEPTH * 4 * 8)
        cbias, cbb = self.load_small("ssd_cb", DEPTH * 8)
        cst, cstb = self.load_small("ssd_consts", 4 * 128)
        if "ssd_rows" not in self.sp_in:
            self.sp_in["ssd_rows"] = self.ext_in("ssd_rows", (1, DEPTH * 552))
        rows = p.tile([128, 552], F32)
        rwb = Buf()
        p.dma(rows, self.sp_in["ssd_rows"][0, l * 552:(l + 1) * 552].partition_broadcast(128), (), (rwb,))
        Abc = p.tile([128, 16], F32)
        p.act(Abc, rows[:, 0:16], AF.Exp, (rwb,), (rwb,))
        p.ts(Abc, Abc, -1.0, None, ALU.mult, None, (rwb,), (rwb,))
        dtb = rows[:, 16:32]
        dsk = rows[:, 32:40]
        nrm = rows[:, 40:552]
        XP = T + 6
        xpall = p.tile([128, 2 * XP], F32)
        xp = [xpall[:, i * XP:(i + 1) * XP] for i in range(2)]
        xpb = bufs(2)
        cv = p.tile([128, T], F32)
        cvb = Buf()
        big = p.tile([128, 18 * 512], F32)
        bigb = Buf()
        xs_fm = big.rearrange("p (j t) -> p j t", j=4)
        z_tm = big.rearrange("p (c f) -> p c f", c=18)
        b_fm = p.tile([128, 2, T], BF16)
        c_fm = p.tile([128, 2, T], BF16)
        bfb, cfb = bufs(2)
        xs_tm = p.tile([128, 18, 512], F32)
        xtb = Buf()
        b_tm = p.tile([128, 18, 256], BF16)
        btb = Buf()
        dtr = p.tile([128, 18, 16], F32)
        dt = p.tile([128, 18, 16], F32)
        dtA = p.tile([128, 18, 16], F32)
        dtt = p.tile([128, 18, 16], F32)
        dtrb, dtbuf = bufs(2)
        yacc = p.tile([128, 18, 512], F32)
        yab = bufs(18)
        yfm = xpall.bitcast(BF16)[:, 0:4 * T].rearrange("p (j t) -> p j t", j=4)
        yfb = Buf()
        D8 = p.tile([128, 8, 128], F32)
        seg = p.tile([128, 8, 128], F32)
        dec = p.tile([128, 8, 128], F32)
        Mt = p.tile([128, 8, 128], BF16)
        xdt = p.tile([128, 512], BF16)
        xw = p.tile([128, 512], BF16)
        tmp = p.tile([128, 512], F32)
        S = p.tile([128, 512], F32)
        Sb = p.tile([128, 512], BF16)
        csc = p.tile([128, 8], F32)
        ecs = p.tile([128, 8], F32)
        w8 = p.tile([128, 8], F32)
        ca = p.tile([128, 8], F32)
        ss = p.tile([128, 2], F32)
        yn = p.tile([128, 512], BF16)
        D8b, segb, decb, Mb, xdtb, xwb, tmpb, Sbuf_, Sbb, cscb, ecsb, w8b, cab, ssb, ynb = bufs(15)
        bank = [0]

        def h8(ap, n=64):
            return ap.rearrange("p (h q) -> p h q", h=8)

        def bc8(ap8, n):
            return ap8.unsqueeze(2).broadcast_to([128, 8, n])

        for s in range(NSEQ):
            for i_ in range(2):
                p.memset(xp[i_], 0.0, (xpb[i_], yfb))
            for j in range(8):
                xi = j % 2
                rx = (s * NFM + PF_SSD + j) * 128
                p.dma(xp[xi][:, 2:2 + LC], self.PF[rx:rx + 128, 0:LC], (self.B("PF", s, PF_SSD + j),), (xpb[xi],))
                p.dma(xp[xi][:, 5 + LC:5 + T], self.PF[rx:rx + 128, LC:T], (self.B("PF", s, PF_SSD + j),), (xpb[xi],))
                for (o0, n, base) in ((0, LC, 0), (LC, LL, LC + 3)):
                    for k in range(4):
                        w = cw[:, (l * 4 + k) * 8 + j:(l * 4 + k) * 8 + j + 1]
                        src = xp[xi][:, base + k:base + k + n]
                        if k == 0:
                            p.ts(cv[:, o0:o0 + n], src, w, cbias[:, l * 8 + j:l * 8 + j + 1], ALU.mult, ALU.add,
                                 (xpb[xi], cwb, cbb), (cvb,))
                        else:
                            p.stt(cv[:, o0:o0 + n], src, w, cv[:, o0:o0 + n], ALU.mult, ALU.add, (xpb[xi], cwb, cvb), (cvb,))
                if j < 4:
                    p.act(xs_fm[:, j, :], cv, AF.Silu, (cvb,), (bigb,))
                elif j < 6:
                    p.act(b_fm[:, j - 4, :], cv, AF.Silu, (cvb,), (bfb,))
                else:
                    p.act(c_fm[:, j - 6, :], cv, AF.Silu, (cvb,), (cfb,))
            for c in range(18):
                bk = bank[0] % 4
                bank[0] += 1
                for j in range(4):
                    p.tr(ps[:, bk, j * 128:(j + 1) * 128], xs_fm[:, j, c * 128:(c + 1) * 128], self.ident_f, (bigb, self.cb), (psb[bk],))
                p.copy(xs_tm[:, c, :], ps[:, bk, :], (psb[bk],), (xtb,), eng=("act" if c % 2 else "dve"))
                bk = 4 + bank[0] % 2
                pv = ps[:, bk, :].bitcast(BF16)
                for g in range(2):
                    p.tr(pv[:, g * 128:(g + 1) * 128], b_fm[:, g, c * 128:(c + 1) * 128], self.ident_b, (bfb, self.cb), (psb[bk],))
                p.copy(b_tm[:, c, :], pv[:, 0:256], (psb[bk],), (btb,), eng=("dve" if c % 2 else "act"))
            PTs = self.PT[s * T:(s + 1) * T, :].rearrange("(c p) f -> p c f", p=128)
            ptbufs = [self.B("PT", s, tt) for tt in range(18)]
            p.dma(dtr, PTs[:, :, TM_DT:TM_DT + 16], ptbufs, (dtrb,))
            p.tt(dtr, dtr, dtb.unsqueeze(1).broadcast_to([128, 18, 16]), ALU.add, (dtrb, rwb), (dtrb,))
            p.ts(dt, dtr, 0.0, None, ALU.max, None, (dtrb,), (dtbuf,))
            p.ts(dtt, dtr, 0.0, None, ALU.min, None, (dtrb,), (dtbuf,))
            p.tt(dtt, dtt, dt, ALU.subtract, (dtbuf,), (dtbuf,))
            p.act(dtt, dtt, AF.Exp, (dtbuf,), (dtbuf,))
            p.act(dtt, dtt, AF.Ln, (dtbuf, self.cb), (dtbuf,), bias=self.one_t)
            p.tt(dt, dt, dtt, ALU.add, (dtbuf,), (dtbuf,))
            p.tt(dtA, dt, Abc.unsqueeze(1).broadcast_to([128, 18, 16]), ALU.mult, (dtbuf, rwb), (dtbuf,))
            for d in range(2):
                order = list(range(18)) if d == 0 else [1, 0] + list(range(17, 1, -1))
                tri = cst[:, d * 128:(d + 1) * 128]
                mneg = cst[:, (2 + d) * 128:(3 + d) * 128]
                LAST = 127 if d == 0 else 0
                p.memset(S, 0.0, (Sbuf_,))
                p.memset(Sb, 0.0, (Sbb,))
                for c in order:
                    cr = slice(c * 128, (c + 1) * 128)
                    dA = dtA[:, c, d * 8:(d + 1) * 8]
                    p.tt(D8, tri.unsqueeze(1).broadcast_to([128, 8, 128]), bc8(dA, 128), ALU.mult, (cstb, dtbuf), (D8b,))
                    D8f = D8.rearrange("p h i -> p (h i)")
                    p.mm(ps[:, 0, :], self.ones_f, D8f[:, 0:512], True, True, (self.cb, D8b), (psb[0],))
                    p.mm(ps[:, 1, :], self.ones_f, D8f[:, 512:1024], True, True, (self.cb, D8b), (psb[1],))
                    csbc = ps[:, 0:2, :].rearrange("p a (h i) -> p (a h) i", h=4)
                    p.mm(ps[:, 2, 0:8], tri, dA, True, True, (cstb, dtbuf), (psb[2],))
                    p.copy(csc, ps[:, 2, 0:8], (psb[2],), (cscb,), eng="act")
                    p.tt(seg, csbc, bc8(csc, 128), ALU.subtract, (psb[0], psb[1], cscb), (segb,))
                    p.tt(seg, seg, mneg.unsqueeze(1).broadcast_to([128, 8, 128]), ALU.add, (segb, cstb), (segb,), eng="pool")
                    p.act(dec, seg, AF.Exp, (segb,), (decb,))
                    for g in range(2):
                        p.mm(ps[:, 3, g * 128:(g + 1) * 128], b_fm[:, g, cr], c_fm[:, g, cr], True, True, (bfb, cfb), (psb[3],))
                    cbt = ps[:, 3, 0:256].rearrange("p (g i) -> p g i", g=2).unsqueeze(2).broadcast_to([128, 2, 4, 128])
                    p.tt(Mt.rearrange("p (g r) i -> p g r i", g=2), dec.rearrange("p (g r) i -> p g r i", g=2), cbt, ALU.mult,
                         (decb, psb[3]), (Mb,))
                    p.tt(h8(xdt), h8(xs_tm[:, c, :]), bc8(dt[:, c, d * 8:(d + 1) * 8], 64), ALU.mult, (xtb, dtbuf), (xdtb,), eng="pool")
                    for h in range(8):
                        p.mm(ps[:, 4, h * 64:(h + 1) * 64], Mt[:, h, :], xdt[:, h * 64:(h + 1) * 64], True, True, (Mb, xdtb), (psb[4],))
                    for g in range(2):
                        p.mm(ps[:, 5, g * 256:(g + 1) * 256], c_fm[:, g, cr], Sb[:, g * 256:(g + 1) * 256], True, True, (cfb, Sbb), (psb[5],))
                    p.act(ecs, csc, AF.Exp, (cscb,), (ecsb,))
                    p.tt(h8(tmp), h8(ps[:, 5, :]), bc8(ecs, 64), ALU.mult, (psb[5], ecsb), (tmpb,))
                    if d == 0:
                        p.tt(yacc[:, c, :], tmp, ps[:, 4, :], ALU.add, (tmpb, psb[4]), (yab[c],))
                    else:
                        p.tt(tmp, tmp, ps[:, 4, :], ALU.add, (tmpb, psb[4]), (tmpb,))
                        p.tt(yacc[:, c, :], yacc[:, c, :], tmp, ALU.add, (tmpb, yab[c]), (yab[c],), eng="pool")
                    p.tt(w8, csbc[:, :, LAST], csc, ALU.subtract, (psb[0], psb[1], cscb), (w8b,))
                    p.act(w8, w8, AF.Exp, (w8b,), (w8b,))
                    p.tt(h8(xw), h8(xdt), bc8(w8, 64), ALU.mult, (xdtb, w8b), (xwb,))
                    for g in range(2):
                        p.mm(ps[:, 6, g * 256:(g + 1) * 256], b_tm[:, c, g * 128:(g + 1) * 128], xw[:, g * 256:(g + 1) * 256], True, True,
                             (btb, xwb), (psb[6],))
                    p.act(ca, csbc[:, :, LAST], AF.Exp, (psb[0], psb[1]), (cab,))
                    p.tt(h8(S), h8(S), bc8(ca, 64), ALU.mult, (Sbuf_, cab), (Sbuf_,))
                    p.tt(S, S, ps[:, 6, :], ALU.add, (Sbuf_, psb[6]), (Sbuf_,))
                    p.copy(Sb, S, (Sbuf_,), (Sbb,), eng="act")
            p.dma(z_tm, PTs[:, :, TM_Z:TM_Z + 512], ptbufs, (bigb,))
            for c in range(18):
                p.tt(h8(tmp), h8(xs_tm[:, c, :]), bc8(dsk, 64), ALU.mult, (xtb, rwb), (tmpb,), eng="pool")
                p.tt(tmp, tmp, yacc[:, c, :], ALU.add, (tmpb, yab[c]), (tmpb,))
                p.act(z_tm[:, c, :], z_tm[:, c, :], AF.Silu, (bigb,), (bigb,))
                p.tt(tmp, tmp, z_tm[:, c, :], ALU.mult, (tmpb, bigb), (tmpb,))
                p.tt(yacc[:, c, :], tmp, tmp, ALU.mult, (tmpb,), (yab[c],), eng="pool")
                p.op("dve", lambda e, c=c: e.tensor_reduce(ss[:, 0:1], yacc[:, c, :], AX.X, ALU.add), (yab[c],), (ssb,))
                p.act(ss[:, 1:2], ss[:, 0:1], AF.Sqrt, (ssb, self.cb), (ssb,), bias=self.eps_t, scale=1.0 / 512)
                p.recip(ss[:, 1:2], ss[:, 1:2], (ssb,), (ssb,))
                p.stt(yn, tmp, ss[:, 1:2], nrm, ALU.mult, ALU.mult, (tmpb, ssb, rwb), (ynb,))
                bk = 4 + c % 2
                pv = ps[:, bk, :].bitcast(BF16)
                for j in range(4):
                    p.tr(pv[:, j * 128:(j + 1) * 128], yn[:, j * 128:(j + 1) * 128], self.ident_b, (ynb, self.cb), (psb[bk],))
                p.copy(yfm[:, :, c * 128:(c + 1) * 128], pv[:, 0:512].rearrange("p (j t) -> p j t", j=4), (psb[bk],), (yfb, xpb[0], xpb[1]),
                       eng=("act" if c % 2 else "dve"))
            Ys = self.Y[s * 2560 + 1024:s * 2560 + 1536, :].rearrange("(j p) t -> p j t", p=128)
            p.dma(Ys, yfm, (yfb,), self.tokbufs("Y", s, 0, T))

    def hy_tables(self, L):
        out = {}
        nch = L // 128
        for kind in ("cf", "sf", "ci", "si"):
            if L == LL:
                Wd, b = self.Wt("dft" + kind)
                out[kind] = (Wd.rearrange("(c p) n -> p c n", p=128), b)
            else:
                ap = self.small("dftc_" + kind, nch * L, BF16)
                out[kind] = (ap.rearrange("p (c n) -> p c n", c=nch), Buf())
        return out

    def run_hy(self, l):
        p = self.p
        ps, psb = self.ps, self.psb
        if not hasattr(self, "KS"):
            self.KS = {L: self.scratch("KS%d" % L, (2 * L, 1024), F32) for L in (LC, LL)}
        if "hy_rows" not in self.sp_in:
            self.sp_in["hy_rows"] = self.ext_in("hy_rows", (1, DEPTH * 3072))
        self.new_stage()
        feat, featb = self.load_small("hy_feat", LC + LL)
        w1, w1b = self.load_small("hy_w1", DEPTH * 64)
        w2, w2b = self.load_small("hy_w2", DEPTH * 64)
        b12, b12b = self.load_small("hy_b12", DEPTH * 4)
        negtn, ntb = self.load_small("hy_negtn", 18)
        w3 = p.tile([128, 2048], F32)
        w3b = Buf()
        p.dma(w3, self.small("hy_w3", DEPTH * 2048)[:, l * 2048:(l + 1) * 2048], (), (w3b,))
        rows = p.tile([128, 2048], F32)
        rwb = Buf()
        p.dma(rows, self.sp_in["hy_rows"][0, l * 3072:l * 3072 + 2048].partition_broadcast(128), (), (rwb,))
        absd = p.tile([128, 2048], F32)
        p.ts(absd, rows, -1.0, None, ALU.mult, None, (rwb,), (rwb,), eng="pool")
        p.tt(absd, absd, rows, ALU.max, (rwb,), (rwb,))
        brow = p.tile([1, 1024], F32)
        p.dma(brow, self.sp_in["hy_rows"][0:1, l * 3072 + 2048:(l + 1) * 3072], (), (rwb,))
        h1 = p.tile([128, LL], F32)
        h2 = p.tile([128, LL], F32)
        win = p.tile([128, 1024], F32)
        filt = p.tile([128, 1024], F32)
        FS = p.tile([128, 16, 2, 512], BF16)
        kst = [p.tile([128, 2, 512], F32) for _ in range(2)]
        tab = [p.tile([128, 16, 256], BF16) for _ in range(4)]
        h1b, h2b, winb, filtb, FSb = bufs(5)
        rred = p.tile([128, 512], F32)
        rredb = Buf()
        kstb = bufs(2)
        tabb = bufs(4)
        bank = 0
        for (L, foff, noff) in ((LC, 0, 0), (LL, LC, 2)):
            nch = L // 128
            N = 2 * L
            tabs = self.hy_tables(L)
            for (src, kdim, wt_, wtb_, bcol, dst, dstb) in ((feat[:, foff:foff + L], 33, w1, w1b, 0, h1, h1b), (h1, 64, w2, w2b, 1, h2, h2b)):
                for c0 in range(0, L, 512):
                    n = min(512, L - c0)
                    bk = bank % 8
                    bank += 1
                    p.mm(ps[0:64, bk, 0:n], wt_[0:kdim, l * 64:(l + 1) * 64], src[0:kdim, c0:c0 + n], True, True,
                         (wtb_, featb, h1b), (psb[bk],))
                    p.ts(dst[0:64, c0:c0 + n], ps[0:64, bk, 0:n], b12[0:64, l * 4 + bcol:l * 4 + bcol + 1],
                         b12[0:64, l * 4 + 2 + bcol:l * 4 + 3 + bcol], ALU.add, ALU.mult, (psb[bk], b12b), (dstb,))
                    rr = rred[0:64, 0:n]
                    p.ts(rr, dst[0:64, c0:c0 + n], 1.0 / (2.0 * PI), 12582912.0, ALU.mult, ALU.add, (dstb,), (rredb,))
                    p.ts(rr, rr, -12582912.0, None, ALU.add, None, (rredb,), (rredb,))
                    p.stt(dst[0:64, c0:c0 + n], rr, -2.0 * PI, dst[0:64, c0:c0 + n], ALU.mult, ALU.add, (rredb, dstb), (dstb,))
                    p.ts(dst[0:64, c0:c0 + n], dst[0:64, c0:c0 + n], 3.1415925, -3.1415925, ALU.min, ALU.max, (dstb,), (dstb,))
                    p.act(dst[0:64, c0:c0 + n], dst[0:64, c0:c0 + n], AF.Sin, (dstb,), (dstb,))
            p.memset(h2[64:65, 0:L], 1.0, (h2b,))
            for o in range(2):
                for tc in range(nch):
                    bk = bank % 4
                    bank += 1
                    for dr in range(2):
                        c0 = (o * 2 + dr) * 512
                        p.mm(ps[:, bk * 2 + dr, :], h2[0:65, tc * 128:(tc + 1) * 128], w3[0:65, c0:c0 + 512], True, True,
                             (h2b, w3b), (psb[bk * 2 + dr],))
                    p.act(win, absd[:, o * 1024:(o + 1) * 1024], AF.Exp, (rwb, ntb), (winb,), scale=negtn[:, noff + tc:noff + tc + 1])
                    for dr in range(2):
                        p.tt(filt[:, dr * 512:(dr + 1) * 512], ps[:, bk * 2 + dr, :], win[:, dr * 512:(dr + 1) * 512], ALU.mult,
                             (psb[bk * 2 + dr], winb), (filtb,))
                    if tc == 0:
                        p.tt(filt[0:1, 0:512], filt[0:1, 0:512], brow[0:1, o * 512:(o + 1) * 512], ALU.add, (filtb, rwb), (filtb,))
                        p.memset(filt[0:1, 512:1024], 0.0, (filtb,))
                    p.tt(FS[:, tc, 0, :], filt[:, 0:512], filt[:, 512:1024], ALU.add, (filtb,), (FSb,))
                    p.tt(FS[:, tc, 1, :], filt[:, 512:1024], filt[:, 0:512], ALU.subtract, (filtb,), (FSb,), eng="pool")
                nfg = max(1, L // 256)
                for fg in range(nfg):
                    fw = min(256, L)
                    for ki, kind in enumerate(("cf", "sf")):
                        tv, tb_ = tabs[kind]
                        ti = (fg % 2) * 2 + ki
                        p.dma(tab[ti][:, 0:nch, 0:fw], tv[:, :, fg * 256:fg * 256 + fw], (tb_,), (tabb[ti],))
                    for fcl in range(fw // 128):
                        fc = fg * 2 + fcl
                        ki_ = fc % 2
                        for ki in range(2):
                            ti = (fg % 2) * 2 + ki
                            bk = 4 + (fc % 2) * 2 + ki
                            for tc in range(nch):
                                p.mm(ps[:, bk, :], tab[ti][:, tc, fcl * 128:(fcl + 1) * 128], FS[:, tc, ki, :], tc == 0, tc == nch - 1,
                                     (tabb[ti], FSb), (psb[bk],))
                            p.act(kst[ki_][:, ki, :], ps[:, bk, :], AF.Copy, (psb[bk],), (kstb[ki_],), scale=2.0 / N)
                        KSv = self.KS[L].rearrange("(r f) c -> f r c", r=2)
                        p.dma(KSv[fc * 128:(fc + 1) * 128, :, o * 512:(o + 1) * 512], kst[ki_], (kstb[ki_],), (self.B("KS", L, o, fc),))
        self.new_stage()
        cw, cwb = self.load_small("hy_cw", DEPTH * 3 * 12)
        cbias, cbb = self.load_small("hy_cb", DEPTH * 12)
        xp = p.tile([128, LL + 2], F32)
        u32 = p.tile([128, LL], F32)
        ub = p.tile([128, LL], BF16)
        xm = p.tile([128, LL], F32)
        z32 = p.tile([128, LL], F32)
        yst = p.tile([128, LL], BF16)
        u_tm = p.tile([128, 16, 512], BF16)
        Yre = p.tile([128, 16, 512], BF16)
        Wim = p.tile([128, 16, 512], BF16)
        kt = [p.tile([128, 2, 512], F32) for _ in range(2)]
        ftab = [p.tile([128, 16, 256], BF16) for _ in range(4)]
        itab = [p.tile([128, 16, 256], BF16) for _ in range(4)]
        tq = [p.tile([128, 512], F32) for _ in range(4)]
        xpb, u32b, ubb, xmb, z32b, ystb, utb, Yb, Wb_ = bufs(9)
        ktb = bufs(2)
        ftabb = bufs(4)
        itabb = bufs(4)
        tqb = bufs(4)

        def short_conv(s, j, t_off, L, dst, dstb):
            rx = (s * NFM + PF_HY + j) * 128
            p.memset(xp[:, 0:1], 0.0, (xpb,), eng="pool")
            p.memset(xp[:, L + 1:L + 2], 0.0, (xpb,), eng="pool")
            p.dma(xp[:, 1:L + 1], self.PF[rx:rx + 128, t_off:t_off + L], (self.B("PF", s, PF_HY + j),), (xpb,))
            for k in range(3):
                w = cw[:, (l * 3 + k) * 12 + j:(l * 3 + k) * 12 + j + 1]
                if k == 0:
                    p.ts(dst[:, 0:L], xp[:, 0:L], w, cbias[:, l * 12 + j:l * 12 + j + 1], ALU.mult, ALU.add, (xpb, cwb, cbb), (dstb,))
                else:
                    p.stt(dst[:, 0:L], xp[:, k:k + L], w, dst[:, 0:L], ALU.mult, ALU.add, (xpb, cwb, dstb), (dstb,))

        def to_tm(src32, srcb, cc, L):
            nch = L // 128
            p.copy(ub[:, 0:L], src32[:, 0:L], (srcb,), (ubb,), eng="act")
            for t8 in range(0, nch, 8):
                nt = min(8, nch - t8)
                bk = self.hbank % 2
                self.hbank += 1
                pv = ps[:, bk, :].bitcast(BF16)
                for q in range(nt):
                    p.tr(pv[:, q * 128:(q + 1) * 128], ub[:, (t8 + q) * 128:(t8 + q + 1) * 128], self.ident_b, (ubb, self.cb), (psb[bk],))
                p.copy(u_tm[:, t8:t8 + nt, cc * 128:(cc + 1) * 128], pv[:, 0:nt * 128].rearrange("p (q c) -> p q c", q=nt),
                       (psb[bk],), (utb,))

        self.hbank = 0
        for s in range(NSEQ):
            for (t_off, L) in ((0, LC), (LC, LL)):
                nch = L // 128
                tabs = self.hy_tables(L)
                KSv = self.KS[L].rearrange("(r f) c -> f r c", r=2)
                for cc in range(4):
                    short_conv(s, cc, t_off, L, u32, u32b)
                    to_tm(u32, u32b, cc, L)
                for o in range(2):
                    nfg = max(1, L // 256)
                    fw = min(256, L)
                    for fg in range(nfg):
                        for ki, kind in enumerate(("cf", "sf")):
                            tv, tb_ = tabs[kind]
                            ti = (fg % 2) * 2 + ki
                            p.dma(ftab[ti][:, 0:nch, 0:fw], tv[:, :, fg * 256:fg * 256 + fw], (tb_,), (ftabb[ti],))
                        for fcl in range(fw // 128):
                            fc = fg * 2 + fcl
                            kq = fc % 2
                            p.dma(kt[kq], KSv[fc * 128:(fc + 1) * 128, :, o * 512:(o + 1) * 512], (self.B("KS", L, o, fc),), (ktb[kq],))
                            bre, bim = 2 + (fc % 2) * 2, 3 + (fc % 2) * 2
                            for ki, bk in ((0, bre), (1, bim)):
                                ti = (fg % 2) * 2 + ki
                                for tc in range(nch):
                                    p.mm(ps[:, bk, :], ftab[ti][:, tc, fcl * 128:(fcl + 1) * 128], u_tm[:, tc, :], tc == 0, tc == nch - 1,
                                         (ftabb[ti], utb), (psb[bk],))
                            p.tt(tq[0], ps[:, bre, :], kt[kq][:, 0, :], ALU.mult, (psb[bre], ktb[kq]), (tqb[0],))
                            p.tt(tq[1], ps[:, bim, :], kt[kq][:, 1, :], ALU.mult, (psb[bim], ktb[kq]), (tqb[1],))
                            p.tt(Yre[:, fc, :], tq[0], tq[1], ALU.add, (tqb[0], tqb[1]), (Yb,), eng="pool")
                            p.tt(tq[2], ps[:, bim, :], kt[kq][:, 0, :], ALU.mult, (psb[bim], ktb[kq]), (tqb[2],))
                            p.tt(tq[3], ps[:, bre, :], kt[kq][:, 1, :], ALU.mult, (psb[bre], ktb[kq]), (tqb[3],))
                            p.tt(Wim[:, fc, :], tq[2], tq[3], ALU.subtract, (tqb[2], tqb[3]), (Wb_,), eng="pool")
                    tbw = min(256, L)
                    it = 0
                    for cc in range(4):
                        short_conv(s, 4 * (o + 1) + cc, t_off, L, xm, xmb)
                        for tb in range(L // tbw):
                            for ki, kind in enumerate(("ci", "si")):
                                tv, tb_ = tabs[kind]
                                ti = (it % 2) * 2 + ki
                                p.dma(itab[ti][:, 0:nch, 0:tbw], tv[:, :, tb * tbw:(tb + 1) * tbw], (tb_,), (itabb[ti],))
                            bk = 6 + it % 2
                            for fc in range(nch):
                                p.mm(ps[:, bk, 0:tbw], Yre[:, fc, cc * 128:(cc + 1) * 128], itab[(it % 2) * 2][:, fc, 0:tbw], fc == 0, False,
                                     (Yb, itabb[(it % 2) * 2]), (psb[bk],))
                                p.mm(ps[:, bk, 0:tbw], Wim[:, fc, cc * 128:(cc + 1) * 128], itab[(it % 2) * 2 + 1][:, fc, 0:tbw], False, fc == nch - 1,
                                     (Wb_, itabb[(it % 2) * 2 + 1]), (psb[bk],))
                            it += 1
                            if o == 0:
                                p.tt(z32[:, tb * tbw:(tb + 1) * tbw], xm[:, tb * tbw:(tb + 1) * tbw], ps[:, bk, 0:tbw], ALU.mult,
                                     (xmb, psb[bk]), (z32b,))
                            else:
                                p.tt(yst[:, tb * tbw:(tb + 1) * tbw], xm[:, tb * tbw:(tb + 1) * tbw], ps[:, bk, 0:tbw], ALU.mult,
                                     (xmb, psb[bk]), (ystb,))
                        if o == 0:
                            to_tm(z32, z32b, cc, L)
                        else:
                            r0 = s * 2560 + cc * 128
                            p.dma(self.Y[r0:r0 + 128, t_off:t_off + L], yst[:, 0:L], (ystb,), self.tokbufs("Y", s, t_off, t_off + L))

    def build(self, upto="all"):
        self.setup()
        if not self.lazy:
            self.declare_weights()
        if self.layers[0] == 0:
            self.stage_input()
        else:
            xin = self.ext_in("X_in", (NSEQ * D, T))
            for s_ in range(NSEQ):
                self.p.dma(self.X[s_ * D:(s_ + 1) * D, :], xin[s_ * D:(s_ + 1) * D, :], (), self.tokbufs("X", s_, 0, T))
        order = ["mod", "norm1", "proj", "hy", "lru", "ssd", "att", "gates", "merge", "out", "norm2", "ffn"]
        for l in self.layers:
            for st in order:
                getattr(self, "run_" + st)(l)
                if upto == (l, st):
                    return self.finish(False)
        return self.finish(self.layers[-1] == DEPTH - 1)

    def run_mod(self, l):
        self.stage_mod(l)

    def run_norm1(self, l):
        self.stage_norm(l, 0)

    def run_gates(self, l):
        pass

    def run_norm2(self, l):
        self.stage_norm(l, 1, router=(l if l % 2 == 1 else None))

    def finish(self, final):
        if final:
            self.stage_final()
        elif not self.debug:
            self.p.barrier()
            xo = self.ext_out("X_out", (NSEQ * D, T))
            for s_ in range(NSEQ):
                self.p.dma(xo[s_ * D:(s_ + 1) * D, :], self.X[s_ * D:(s_ + 1) * D, :], self.tokbufs("X", s_, 0, T), ())
        for name in sorted(self.debug):
            ap, shape, dt = {"X": (self.X, (NSEQ * D, T), F32), "H": (self.H, (NSEQ * D, T), BF16),
                             "PF": (self.PF, (NSEQ * NFM * 128, T), F32), "PT": (self.PT, (NSEQ * T, NTM), F32),
                             "Y": (self.Y, (NSEQ * 2560, T), BF16), "G": (self.G, (NSEQ * 4 * D, T), BF16),
                             "M": (self.M, (NSEQ * D, T), BF16)}[name]
            self.dump(name, ap, shape, dt)
        self.p.emit(self.es)
        self.es.close()
        return self.nc


def _fm(a):
    a = np.asarray(a, np.float32)
    R, C = a.shape
    return np.ascontiguousarray(a.reshape(R, C // 128, 128).transpose(2, 0, 1).reshape(128, R * (C // 128)))


def _shard(a, core, ns):
    rows = a.shape[0]
    rs = rows // ns
    return np.ascontiguousarray(a[core * rs:(core + 1) * rs])


def make_in_map(k, inp, core, ns, extra=None):
    m = {}
    b0 = core * NSEQ
    for name, (shape, dt) in k.inputs.items():
        if extra is not None and name in extra:
            v = extra[name]
        elif name == "x":
            v = inp["x"][b0:b0 + NSEQ].reshape(NSEQ * LL, D)
        elif name == "ctx":
            v = inp["ctx"][b0:b0 + NSEQ].reshape(NSEQ * LC, D)
        elif name == "cT":
            cc = np.concatenate([inp["c"][b0:b0 + NSEQ], inp["c_ctx"][None, :]], 0)
            v = cc.reshape(3, KC, 128).transpose(2, 1, 0).reshape(128, KC * 3)
        elif name == "consts":
            v = np.eye(128, dtype=np.float32)
        elif name == "normg":
            v = _fm(np.concatenate([inp["norm_mix"], inp["norm_ffn"], inp["norm_final"][None, :]], 0))
        elif name == "adab":
            v = _fm(inp["ada_b"])
        elif name.startswith("ada_w"):
            v = _shard(inp["ada_w"][int(name[5:])], core, ns)
        elif name.startswith("w_in"):
            v = _shard(inp["w_in"][int(name[4:])], core, ns)
        elif name.startswith("w_gate"):
            v = _shard(inp["w_gate"][int(name[6:])].reshape(4 * D, D), core, ns)
        elif name.startswith("w_branch"):
            v = _shard(inp["w_branch"][int(name[8:])], core, ns)
        elif name.startswith("w_out"):
            v = _shard(inp["w_out"][int(name[5:])], core, ns)
        elif name.startswith("ffn_w"):
            v = _shard(inp[name[:6]][int(name[7:])], core, ns)
        elif name.startswith("moe_w"):
            _, wn, j, e = name.split("_")
            v = _shard(inp["moe_" + wn][int(j)][int(e)], core, ns)
        elif name.startswith("dft_"):
            v = _shard(_dft_table(name[4:], LL), core, ns)
        else:
            v = host_small(name, inp)
        v = np.ascontiguousarray(v)
        if dt == F32:
            v = v.astype(np.float32, copy=False)
        assert tuple(v.shape) == tuple(shape), (name, v.shape, shape)
        m[name] = v
    return m


_DFT_CACHE = {}


def _dft_table(kind, L):
    key = (kind, L)
    if key not in _DFT_CACHE:
        N = 2 * L
        t = np.arange(L, dtype=np.int64)
        f = np.arange(L, dtype=np.int64)
        ph = ((2 * f[None, :] + 1) * t[:, None]) % (2 * N)
        ang = ph.astype(np.float64) * (np.pi / N)
        tab = np.cos(ang) if kind[0] == "c" else np.sin(ang)
        if kind[1] == "i":
            tab = tab.T
        _DFT_CACHE[key] = np.ascontiguousarray(tab.astype(np.float32).astype(ml_dtypes.bfloat16))
    return _DFT_CACHE[key]


def host_small(name, inp):
    f32 = np.float32
    if name == "bgate":
        return _fm(inp["b_gate"].reshape(DEPTH * 4, D))
    if name == "lru_cw":
        return _fm(inp["lru_conv_w"].reshape(DEPTH * 4, 512))
    if name == "lru_cb":
        return _fm(inp["lru_conv_b"])
    if name in ("lru_ba", "lru_bx"):
        return _fm(inp[name].reshape(DEPTH * 2, 512))
    if name == "lru_lam":
        return _fm(inp["lru_lambda"].reshape(DEPTH * 2, 512))
    if name in ("lru_wa", "lru_wx"):
        return inp[name].reshape(DEPTH * 2 * 8 * 64, 64)
    if name == "att_qn":
        return _fm(inp["att_q_norm"])
    if name == "att_kn":
        return _fm(inp["att_k_norm"])
    if name == "att_rows":
        return np.concatenate([inp["att_q_norm"], inp["att_k_norm"]], 1).reshape(1, DEPTH * 256)
    if name in ("rope_cos", "rope_sin"):
        t = np.arange(LL)
        row = (t // 64).astype(f32)
        col = (t % 64).astype(f32)
        inv = (10000.0 ** (-np.arange(0, 64, 2, dtype=f32) / 64)).astype(f32)
        ang = np.concatenate([row[:, None] * inv, col[:, None] * inv], -1)
        tab = np.cos(ang) if name == "rope_cos" else np.sin(ang)
        return np.ascontiguousarray(np.repeat(tab, 2, axis=1).T.astype(f32))
    if name == "rotm":
        r = np.zeros((128, 128), f32)
        for i in range(64):
            r[2 * i + 1, 2 * i] = -1.0
            r[2 * i, 2 * i + 1] = 1.0
        return r
    if name == "ssd_cw":
        return _fm(inp["ssd_conv_w"].reshape(DEPTH * 4, 1024))
    if name == "ssd_cb":
        return _fm(inp["ssd_conv_b"])
    if name == "ssd_rows":
        return np.concatenate([inp["ssd_a_log"].reshape(DEPTH, 16), inp["ssd_dt_bias"].reshape(DEPTH, 16),
                               inp["ssd_d"], inp["ssd_norm"]], 1).reshape(1, DEPTH * 552)
    if name == "ssd_consts":
        j = np.arange(128)[:, None]
        i = np.arange(128)[None, :]
        tf = (j <= i).astype(f32)
        tb = (j >= i).astype(f32)
        return np.concatenate([tf, tb, (tf - 1.0) * 30000.0, (tb - 1.0) * 30000.0], 1)
    if name == "hy_cw":
        return _fm(inp["hy_conv_w"].reshape(DEPTH * 3, 1536))
    if name == "hy_cb":
        return _fm(inp["hy_conv_b"])
    if name in ("hy_w1", "hy_w2"):
        w = inp[name]
        out = np.zeros((128, DEPTH * 64), f32)
        out[:w.shape[1]] = w.transpose(1, 0, 2).reshape(w.shape[1], DEPTH * 64)
        return out
    if name == "hy_w3":
        out = np.zeros((128, DEPTH * 2048), f32)
        out[:64] = inp["hy_w3"].transpose(1, 0, 2).reshape(64, DEPTH * 2048)
        out[64] = inp["hy_b3"].reshape(DEPTH * 2048)
        return out
    if name == "hy_b12":
        out = np.zeros((128, DEPTH * 4), f32)
        for l in range(DEPTH):
            out[:64, l * 4 + 0] = inp["hy_b1"][l]
            out[:64, l * 4 + 1] = inp["hy_b2"][l]
            out[:64, l * 4 + 2] = inp["hy_freq"][l, 0]
            out[:64, l * 4 + 3] = inp["hy_freq"][l, 1]
        return out
    if name == "hy_rows":
        return np.concatenate([inp["hy_decay"].reshape(DEPTH, 2048), inp["hy_bias"].reshape(DEPTH, 1024)], 1).reshape(1, DEPTH * 3072)
    if name == "hy_feat":
        out = np.zeros((128, LC + LL), f32)
        off = 0
        for L in (LC, LL):
            t = np.arange(L, dtype=np.float64)
            bands = np.linspace(1e-4, 15.0, 16)
            ang = (2.0 * np.pi / L) * t[:, None] * bands[None, :]
            ft = np.concatenate([(t / L)[:, None], np.cos(ang), -np.sin(ang)], -1)
            out[:33, off:off + L] = ft.T
            off += L
        return out
    if name == "hy_negtn":
        out = np.zeros((128, 18), f32)
        pp = np.arange(128)
        for c in range(2):
            out[:, c] = -(c * 128 + pp) / float(LC)
        for c in range(16):
            out[:, 2 + c] = -(c * 128 + pp) / float(LL)
        return out
    if name.startswith("dftc_"):
        tabl = _dft_table(name[5:], LC)
        return np.ascontiguousarray(tabl.reshape(2, 128, LC).transpose(1, 0, 2).reshape(128, 2 * LC))
    if name.startswith("router_b"):
        return inp["moe_router_b"][int(name[8:])].reshape(1, NEXP)
    if name.startswith("router"):
        return _fm(inp["moe_router"][int(name[6:])].T).reshape(128, NEXP, KC).transpose(0, 2, 1).reshape(128, KC * NEXP)
    raise KeyError(name)


_PROG_CACHE = {}
SEGMENTS = [[0], [1], [2], [3]]


def _get_prog(seg):
    key = tuple(seg)
    if key not in _PROG_CACHE:
        k = K(n_layers=DEPTH, n_shards=0, layers=seg)
        nc = k.build()
        _PROG_CACHE[key] = (k, nc)
    return _PROG_CACHE[key]


def _weight_src(name, inp):
    if name.startswith("ada_w"):
        return inp["ada_w"][int(name[5:])]
    if name.startswith("w_in"):
        return inp["w_in"][int(name[4:])]
    if name.startswith("w_gate"):
        return inp["w_gate"][int(name[6:])].reshape(4 * D, D)
    if name.startswith("w_branch"):
        return inp["w_branch"][int(name[8:])]
    if name.startswith("w_out"):
        return inp["w_out"][int(name[5:])]
    if name.startswith("ffn_w"):
        return inp[name[:6]][int(name[7:])]
    if name.startswith("moe_w"):
        _, wn, j, e = name.split("_")
        return inp["moe_" + wn][int(j)][int(e)]
    raise KeyError(name)


def _build_prep(specs):
    nc = bass.Bass("TRN2", target_bir_lowering=False)
    pairs = []
    for name, (rows, cols) in specs.items():
        rs = rows // NCORE
        src = nc.dram_tensor(name, [rs, cols], F32, kind="ExternalInput").ap()
        dst = nc.dram_tensor(name + "_b", [rs, cols], BF16, kind="ExternalOutput").ap()
        pairs.append((src, dst, rs))
    with ExitStack() as es:
        sems = [es.enter_context(nc.semaphore("s%d" % i)) for i in range(8)]
        block = es.enter_context(nc.Block())

        @block.gpsimd
        def _(g):
            cnt = [0] * 8
            i = 0
            for src, dst, rs in pairs:
                for r0 in range(0, rs, 256):
                    r1 = min(rs, r0 + 256)
                    q = i % 8
                    i += 1
                    if cnt[q]:
                        g.wait_ge(sems[q], cnt[q] * 16)
                    g.dma_start(out=dst[r0:r1, :], in_=src[r0:r1, :]).then_inc(sems[q], 16)
                    cnt[q] += 1
            for q in range(8):
                if cnt[q]:
                    g.wait_ge(sems[q], cnt[q] * 16)
    return nc


def kernel(**inputs):
    inp = {k_: np.asarray(v) for k_, v in inputs.items()}
    progs = [_get_prog(seg) for seg in SEGMENTS]
    specs = {}
    for k, _ in progs:
        for name, (rows, cols, cast) in k.wspec.items():
            if cast:
                specs[name] = (rows, cols)
    if "prep" not in _PROG_CACHE:
        _PROG_CACHE["prep"] = _build_prep(specs)
    in_maps = []
    for c in range(NCORE):
        in_maps.append({name: _shard(_weight_src(name, inp), c, NCORE) for name in specs})
    res = run_bass_kernel_spmd(_PROG_CACHE["prep"], in_maps, core_ids=list(range(NCORE)))
    wfull = {name: np.concatenate([np.asarray(res.results[c][name + "_b"]) for c in range(NCORE)], axis=0) for name in specs}
    del in_maps, res
    xs = None
    for (k, nc), seg in zip(progs, SEGMENTS):
        extra_all = {}
        for name, (rows, cols, cast) in k.wspec.items():
            extra_all[name] = wfull[name] if cast else _dft_table(name[4:], LL)
        in_maps = []
        for c in range(NCORE):
            extra = dict(extra_all)
            if xs is not None:
                extra["X_in"] = xs[c]
            in_maps.append(make_in_map(k, inp, c, NCORE, extra=extra))
        res = run_bass_kernel_spmd(nc, in_maps, core_ids=list(range(NCORE)))
        del in_maps
        if seg[-1] == DEPTH - 1:
            outs = [np.asarray(r["out"]).reshape(NSEQ, LL, D) for r in res.results]
            return np.concatenate(outs, axis=0).astype(np.float32, copy=False)
        xs = [np.asarray(r["X_out"]) for r in res.results]
```

```python
import math
from contextlib import ExitStack

import numpy as np
import ml_dtypes

import concourse.bass as bass
import concourse.mybir as mybir
from concourse.bass_utils import run_bass_kernel_spmd

F32 = mybir.dt.float32
BF16 = mybir.dt.bfloat16
U8 = mybir.dt.uint8
ALU = mybir.AluOpType
AF = mybir.ActivationFunctionType
AX = mybir.AxisListType

D = 2048
KC = 16
DEPTH = 4
LC = 256
LL = 2048
T = LC + LL
NSEQ = 2
NCORE = 8
EPS = 1e-6
IN_W = 5648
FF = 5504
NEXP = 8
FFE = 4096
PI = math.pi

OFF_HY, OFF_LRU, OFF_SSD, OFF_ATT = 0, 1536, 2560, 4112
FM_COLS = ([OFF_HY + 128 * i for i in range(12)] + [OFF_LRU + 128 * i for i in range(8)]
           + [OFF_SSD + 512 + 128 * i for i in range(8)] + [OFF_ATT + 128 * i for i in range(10)])
PF_HY, PF_LRU, PF_SSD, PF_ATT = 0, 12, 20, 28
NFM = len(FM_COLS)
TM_Z, TM_DT, TM_V, NTM = 0, 512, 528, 784

SB_BYTES = 206 * 1024


class Buf:
    __slots__ = ("w", "r")

    def __init__(self):
        self.w = None
        self.r = []


def bufs(n):
    return [Buf() for _ in range(n)]


class Prog:
    ENG = ("pe", "dve", "act", "pool", "sp")
    NSLOT = {"sp": 24, "pool": 12, "act": 4}

    def __init__(self, nc):
        self.nc = nc
        self.ops = {e: [] for e in self.ENG}
        self.cnt = {e: 0 for e in self.ENG}
        self.seen = {e: {} for e in self.ENG}
        self.slot_uses = {q: [0] * n for q, n in self.NSLOT.items()}
        self.slot_next = {q: 0 for q in self.NSLOT}
        self.extra_sems = {}
        self.ncoll = 0
        self.sb_off = 0
        self.arena = None
        self.psum = None

    def _deps(self, eng, reads, writes):
        need = {}
        for b in reads:
            if b.w is not None:
                k, v = b.w
                if need.get(k, 0) < v:
                    need[k] = v
        for b in writes:
            if b.w is not None:
                k, v = b.w
                if need.get(k, 0) < v:
                    need[k] = v
            for k, v in b.r:
                if need.get(k, 0) < v:
                    need[k] = v
        waits = []
        seen = self.seen[eng]
        for k, v in need.items():
            if k == "c_pe" and eng == "pe":
                continue
            if seen.get(k, 0) < v:
                waits.append((k, v))
                seen[k] = v
        return waits

    def _mark(self, tok, reads, writes):
        for b in reads:
            b.r.append(tok)
            if len(b.r) > 64:
                m = {}
                for k, v in b.r:
                    if m.get(k, 0) < v:
                        m[k] = v
                b.r = list(m.items())
        for b in writes:
            b.w = tok
            b.r = []

    def op(self, eng, fn, reads=(), writes=(), inc=True):
        waits = self._deps(eng, reads, writes)
        key = "c_" + eng
        if inc:
            self.cnt[eng] += 1
            tok = (key, self.cnt[eng])
            self.ops[eng].append((waits, fn, (key, 1)))
        else:
            tok = (key, self.cnt[eng] + 1)
            self.ops[eng].append((waits, fn, None))
        self._mark(tok, reads, writes)

    def dma(self, out, in_, reads=(), writes=(), q="sp", **kw):
        waits = self._deps(q, reads, writes)
        s = self.slot_next[q]
        self.slot_next[q] = (s + 1) % self.NSLOT[q]
        key = "d_%s_%d" % (q, s)
        prev = self.slot_uses[q][s] * 16
        if prev and self.seen[q].get(key, 0) < prev:
            waits.append((key, prev))
            self.seen[q][key] = prev
        self.slot_uses[q][s] += 1
        tok = (key, prev + 16)
        self.ops[q].append((waits, (lambda e: e.dma_start(out=out, in_=in_, **kw)), (key, 16)))
        self._mark(tok, reads, writes)

    def collective(self, kind, ins, outs, reads, writes):
        waits = self._deps("pool", reads, writes)
        i = self.ncoll
        self.ncoll += 1
        key = "cc"
        prev = i
        if prev and self.seen["pool"].get(key, 0) < prev:
            waits.append((key, prev))
            self.seen["pool"][key] = prev
        self.extra_sems[key] = prev + 1
        tok = (key, prev + 1)
        fn = lambda e: e.collective_compute(kind, ALU.bypass, replica_groups=[list(range(NCORE))],
                                            ins=[ins.opt()], outs=[outs.opt()])
        self.ops["pool"].append((waits, fn, (key, None)))
        self._mark(tok, reads, writes)

    def barrier(self):
        toks = [("c_" + e, self.cnt[e]) for e in self.ENG if self.cnt[e]]
        for q, uses in self.slot_uses.items():
            for s, u in enumerate(uses):
                if u:
                    toks.append(("d_%s_%d" % (q, s), u * 16))
        for k, v in self.extra_sems.items():
            toks.append((k, v))
        for e in self.ENG:
            waits = []
            for k, v in toks:
                if k == "c_" + e:
                    continue
                if self.seen[e].get(k, 0) < v:
                    waits.append((k, v))
                    self.seen[e][k] = v
            if waits:
                self.ops[e].append((waits, None, None))

    def reset_sbuf(self, keep=0):
        self.sb_off = keep

    def tile(self, shape, dtype):
        esz = 4 if dtype == F32 else (2 if dtype == BF16 else 1)
        n = 1
        for s in shape[1:]:
            n *= s
        nbytes = (n * esz + 63) // 64 * 64
        off = self.sb_off
        assert off + nbytes <= SB_BYTES, ("SBUF overflow", off, nbytes)
        self.sb_off = off + nbytes
        ap = self.arena[0:shape[0], off:off + n * esz]
        if dtype != U8:
            ap = ap.bitcast(dtype)
        if len(shape) == 3:
            ap = ap.rearrange("p (a b) -> p a b", a=shape[1])
        elif len(shape) == 4:
            ap = ap.rearrange("p (a b c) -> p a b c", a=shape[1], b=shape[2])
        return ap

    def mm(self, out, lhsT, rhs, start, stop, R, W, inc=None):
        inc = stop if inc is None else inc
        self.op("pe", lambda e: e.matmul(out, lhsT, rhs, start=start, stop=stop), R, W, inc=inc)

    def tr(self, out, in_, ident, R, W):
        self.op("pe", lambda e: e.transpose(out, in_, ident), R, W)

    def act(self, out, in_, func, R, W, bias=None, scale=None, accum_out=None):
        kw = {}
        if bias is not None:
            kw["bias"] = bias
        if scale is not None:
            kw["scale"] = scale
        if accum_out is not None:
            kw["accum_out"] = accum_out
        self.op("act", lambda e: e.activation(out, in_, func, **kw), R, W)

    def ts(self, out, in0, s1, s2, op0, op1, R, W, eng="dve"):
        if s2 is None:
            self.op(eng, lambda e: e.tensor_scalar(out, in0, s1, None, op0), R, W)
        else:
            self.op(eng, lambda e: e.tensor_scalar(out, in0, s1, s2, op0, op1), R, W)

    def tt(self, out, in0, in1, op, R, W, eng="dve"):
        self.op(eng, lambda e: e.tensor_tensor(out, in0, in1, op), R, W)

    def stt(self, out, in0, scalar, in1, op0, op1, R, W):
        self.op("dve", lambda e: e.scalar_tensor_tensor(out, in0, scalar, in1, op0, op1), R, W)

    def copy(self, out, in_, R, W, eng="dve"):
        if eng == "act":
            self.op("act", lambda e: e.activation(out, in_, AF.Copy), R, W)
        else:
            self.op(eng, lambda e: e.tensor_copy(out, in_), R, W)

    def memset(self, ap, val, W, eng="dve"):
        self.op(eng, lambda e: e.memset(ap, val), (), W)

    def recip(self, out, in_, R, W):
        self.op("dve", lambda e: e.reciprocal(out, in_), R, W)

    def emit(self, es):
        nc = self.nc
        keys = ["c_" + e for e in self.ENG]
        for q, uses in self.slot_uses.items():
            for s, u in enumerate(uses):
                if u:
                    keys.append("d_%s_%d" % (q, s))
        keys += list(self.extra_sems)
        sems = {k: es.enter_context(nc.semaphore(k)) for k in keys}
        block = es.enter_context(nc.Block())
        slot_uses = self.slot_uses

        def run(e, name):
            for waits, fn, inc in self.ops[name]:
                for k, v in waits:
                    e.wait_ge(sems[k], v)
                if fn is None:
                    continue
                ins = fn(e)
                if inc is not None:
                    if inc[1] is None:
                        ins.then_inc(sems[inc[0]])
                    else:
                        ins.then_inc(sems[inc[0]], inc[1])
            if name in slot_uses:
                for s, u in enumerate(slot_uses[name]):
                    if u:
                        e.wait_ge(sems["d_%s_%d" % (name, s)], u * 16)

        @block.tensor
        def _(e):
            run(e, "pe")

        @block.vector
        def _(e):
            run(e, "dve")

        @block.scalar
        def _(e):
            run(e, "act")

        @block.gpsimd
        def _(e):
            run(e, "pool")

        @block.sync
        def _(e):
            run(e, "sp")


class K:
    def __init__(self, n_layers=DEPTH, n_shards=NCORE, debug=(), lazy=False, layers=None):
        self.lazy = lazy
        self.layers = list(range(n_layers)) if layers is None else list(layers)
        self.n_layers = n_layers
        self.n_shards = n_shards
        self.debug = set(debug)
        self.nc = bass.Bass("TRN2", target_bir_lowering=False)
        self.p = Prog(self.nc)
        self.inputs = {}
        self.dbuf = {}
        self.es = ExitStack()

    def ext_in(self, name, shape, dtype=F32):
        t = self.nc.dram_tensor(name, list(shape), dtype, kind="ExternalInput").ap()
        self.inputs[name] = (tuple(shape), dtype)
        return t

    def ext_out(self, name, shape, dtype=F32):
        return self.nc.dram_tensor(name, list(shape), dtype, kind="ExternalOutput").ap()

    def scratch(self, name, shape, dtype):
        return self.nc.dram_tensor(name, list(shape), dtype).ap()

    def weight(self, name, rows, cols, cast=True):
        p = self.p
        ns = self.n_shards
        if ns == 0:
            if not hasattr(self, "wspec"):
                self.wspec = {}
            self.wspec[name] = (rows, cols, cast)
            return self.ext_in(name, (rows, cols), BF16), Buf()
        rs = rows // ns
        src = self.ext_in(name, (rs, cols), F32 if cast else BF16)
        full = self.scratch(name + "_g", (rows, cols), BF16)
        b = Buf()
        if ns == 1:
            dst = full
        else:
            dst = self.scratch(name + "_s", (rs, cols), BF16)
        bs = Buf()
        step = 512
        for r0 in range(0, rs, step):
            r1 = min(rs, r0 + step)
            p.dma(dst[r0:r1, :], src[r0:r1, :], (), (bs if ns > 1 else b,), q="pool")
        if ns > 1:
            p.collective("AllGather", dst, full, (bs,), (b,))
        return full, b

    def B(self, name, *idx):
        key = (name,) + idx
        b = self.dbuf.get(key)
        if b is None:
            b = self.dbuf[key] = Buf()
        return b

    def tokbufs(self, name, s, t0, t1):
        return [self.B(name, s, i) for i in range(t0 // 256, (t1 + 255) // 256)]

    def setup(self):
        nc, p, es = self.nc, self.p, self.es
        p.arena = es.enter_context(nc.sbuf_tensor("arena", [128, SB_BYTES], U8))
        ps = es.enter_context(nc.psum_tensor("ps", [128, 8, 512], F32))
        self.ps = ps
        self.psb = bufs(8)
        self.x_in = self.ext_in("x", (NSEQ * LL, D))
        self.ctx_in = self.ext_in("ctx", (NSEQ * LC, D))
        self.cT_in = self.ext_in("cT", (128, KC * 3))
        self.consts_in = self.ext_in("consts", (128, 128))
        self.X = self.scratch("X", (NSEQ * D, T), F32)
        self.H = self.scratch("H", (NSEQ * D, T), BF16)
        self.PF = self.scratch("PF", (NSEQ * NFM * 128, T), F32)
        self.PT = self.scratch("PT", (NSEQ * T, NTM), F32)
        self.Y = self.scratch("Y", (NSEQ * 2560, T), BF16)
        self.G = self.scratch("G", (NSEQ * 4 * D, T), BF16)
        self.M = self.scratch("M", (NSEQ * D, T), BF16)
        self.ident_f = p.tile([128, 128], F32)
        self.ident_b = p.tile([128, 128], BF16)
        self.ones_f = p.tile([128, 128], F32)
        self.ones_b = p.tile([128, 128], BF16)
        self.eps_t = p.tile([128, 1], F32)
        self.one_t = p.tile([128, 1], F32)
        self.negpi_t = p.tile([128, 1], F32)
        self.cb = Buf()
        p.dma(self.ident_f, self.consts_in[:, :], (), (self.cb,))
        p.copy(self.ident_b, self.ident_f, (self.cb,), (self.cb,))
        p.memset(self.ones_f, 1.0, (self.cb,))
        p.memset(self.ones_b, 1.0, (self.cb,))
        p.memset(self.eps_t, EPS, (self.cb,))
        p.memset(self.one_t, 1.0, (self.cb,))
        p.memset(self.negpi_t, -PI, (self.cb,))
        self.sp_in = {}
        self.sT = p.tile([128, KC, 3], BF16)
        self.normg = p.tile([128, 2 * DEPTH * KC + KC], F32)
        self.adab = p.tile([128, DEPTH, 96], F32)
        self.modv = p.tile([128, 96, 3], F32)
        self.A1 = p.tile([128, KC, 3], F32)
        self.A2 = p.tile([128, KC, 3], F32)
        self.modb = Buf()
        normg_in = self.ext_in("normg", (128, 2 * DEPTH * KC + KC))
        adab_in = self.ext_in("adab", (128, DEPTH * 96))
        p.dma(self.normg, normg_in[:, :], (), (self.cb,))
        p.dma(self.adab, adab_in.rearrange("p (l j) -> p l j", l=DEPTH), (), (self.cb,))
        ct = p.tile([128, KC, 3], F32)
        p.dma(ct, self.cT_in.rearrange("p (j b) -> p j b", b=3), (), (self.cb,))
        p.act(self.sT, ct, AF.Silu, (self.cb,), (self.cb,))
        self.keep = p.sb_off

    def new_stage(self):
        self.p.barrier()
        self.p.reset_sbuf(self.keep)

    def stage_input(self):
        p = self.p
        self.new_stage()
        ps = self.ps
        xin = [p.tile([128, D], F32) for _ in range(2)]
        xin_b = bufs(2)
        xT = [p.tile([128, KC, 512], F32) for _ in range(2)]
        xT_b = bufs(2)
        it = 0
        ig = 0
        for s in range(NSEQ):
            groups = [(self.ctx_in, s * LC, 0, 2)] + [(self.x_in, s * LL + 512 * g, LC + 512 * g, 4) for g in range(4)]
            for (src, r0, t0, ntile) in groups:
                xt, xtb = xT[ig % 2], xT_b[ig % 2]
                ig += 1
                for tt in range(ntile):
                    xi, xib = xin[it % 2], xin_b[it % 2]
                    it += 1
                    p.dma(xi, src[r0 + tt * 128:r0 + (tt + 1) * 128, :], (), (xib,))
                    for bq in range(4):
                        bank = (it % 2) * 4 + bq
                        for c in range(4):
                            j = bq * 4 + c
                            p.tr(ps[:, bank, c * 128:(c + 1) * 128], xi[:, j * 128:(j + 1) * 128], self.ident_f,
                                 (xib, self.cb), (self.psb[bank],))
                        dst = xt[:, bq * 4:(bq + 1) * 4, tt * 128:(tt + 1) * 128]
                        srcp = ps[:, bank, :].rearrange("p (c t) -> p c t", c=4)
                        p.copy(dst, srcp, (self.psb[bank],), (xtb,), eng=("act" if bq % 2 else "dve"))
                n = ntile * 128
                Xs = self.X[s * D:(s + 1) * D, :].rearrange("(j p) t -> p j t", p=128)
                p.dma(Xs[:, :, t0:t0 + n], xt[:, :, 0:n], (xtb,), self.tokbufs("X", s, t0, t0 + n))

    def stage_mod(self, l):
        p = self.p
        self.new_stage()
        ps = self.ps
        W, wb = self.Wt("ada", l)
        Wv = W.rearrange("(kc p) n -> p kc n", p=128)
        wt = [p.tile([128, KC, 512], BF16) for _ in range(2)]
        wtb = bufs(2)
        for ti in range(24):
            w_sb, w_b = wt[ti % 2], wtb[ti % 2]
            p.dma(w_sb, Wv[:, :, ti * 512:(ti + 1) * 512], (wb,), (w_b,))
            for c in range(4):
                j = ti * 4 + c
                for k in range(KC):
                    p.mm(ps[:, 0, j * 3:(j + 1) * 3], w_sb[:, k, c * 128:(c + 1) * 128], self.sT[:, k, :],
                         k == 0, k == KC - 1, (w_b, self.cb), (self.psb[0],))
        pv = ps[:, 0, 0:288].rearrange("p (j b) -> p j b", b=3)
        p.tt(self.modv, pv, self.adab[:, l, :].unsqueeze(2).broadcast_to([128, 96, 3]), ALU.add,
             (self.psb[0], self.cb), (self.modb,))
        for (A, g0, m0) in ((self.A1, l * KC, 16), (self.A2, (DEPTH + l) * KC, 64)):
            p.ts(A, self.modv[:, m0:m0 + KC, :], 1.0, None, ALU.add, None, (self.modb,), (self.modb,))
            p.tt(A, A, self.normg[:, g0:g0 + KC].unsqueeze(2).broadcast_to([128, KC, 3]), ALU.mult,
                 (self.modb, self.cb), (self.modb,))

    def seq_blocks(self, s):
        return [(0, LC, 2)] + [(LC + 512 * i, 512, s) for i in range(4)]

    def stage_norm(self, l, which, router=None):
        p = self.p
        self.new_stage()
        ps = self.ps
        A = self.A1 if which == 0 else self.A2
        m0 = 0 if which == 0 else 48
        x_sbs = [p.tile([128, KC, 512], F32) for _ in range(2)]
        xbs_ = bufs(2)
        sq_sb = p.tile([128, KC, 512], F32)
        h_sb = p.tile([128, KC, 512], BF16)
        rs_sb = p.tile([128, 512], F32)
        sqb, hb, rsb = bufs(3)
        if router is not None:
            f_sb = p.tile([128, KC, 512], F32)
            fb = Buf()
            self.router_setup(router)
        blocks = [(s, t0, n, col) for s in range(NSEQ) for (t0, n, col) in self.seq_blocks(s)]

        def load_x(i):
            s_, t0_, n_, _c = blocks[i]
            Xv = self.X[s_ * D:(s_ + 1) * D, :].rearrange("(j p) t -> p j t", p=128)
            p.dma(x_sbs[i % 2][:, :, 0:n_], Xv[:, :, t0_:t0_ + n_], self.tokbufs("X", s_, t0_, t0_ + n_), (xbs_[i % 2],))

        load_x(0)
        for bi_, (s, t0, n, col) in enumerate(blocks):
            if bi_ + 1 < len(blocks):
                load_x(bi_ + 1)
            x_sb, xb = x_sbs[bi_ % 2], xbs_[bi_ % 2]
            Hs = self.H[s * D:(s + 1) * D, :].rearrange("(j p) t -> p j t", p=128)
            if True:
                p.act(sq_sb[:, :, 0:n], x_sb[:, :, 0:n], AF.Square, (xb,), (sqb,))
                for j in range(KC):
                    p.mm(ps[:, 0, 0:n], self.ones_f, sq_sb[:, j, 0:n], j == 0, j == KC - 1, (sqb, self.cb), (self.psb[0],))
                p.act(rs_sb[:, 0:n], ps[:, 0, 0:n], AF.Sqrt, (self.psb[0], self.cb), (rsb,), bias=self.eps_t, scale=1.0 / D)
                p.recip(rs_sb[:, 0:n], rs_sb[:, 0:n], (rsb,), (rsb,))
                p.tt(sq_sb[:, :, 0:n], x_sb[:, :, 0:n], rs_sb[:, 0:n].unsqueeze(1).broadcast_to([128, KC, n]), ALU.mult,
                     (xb, rsb), (sqb,))
                for j in range(KC):
                    if router is None:
                        p.act(h_sb[:, j, 0:n], sq_sb[:, j, 0:n], AF.Identity, (sqb, self.modb), (hb,),
                              bias=self.modv[:, m0 + j, col:col + 1], scale=A[:, j, col:col + 1])
                    else:
                        p.act(f_sb[:, j, 0:n], sq_sb[:, j, 0:n], AF.Identity, (sqb, self.modb), (fb,),
                              bias=self.modv[:, m0 + j, col:col + 1], scale=A[:, j, col:col + 1])
                if router is not None:
                    p.copy(h_sb[:, :, 0:n], f_sb[:, :, 0:n], (fb,), (hb,))
                    self.router_block(router, s, t0, n, f_sb, fb)
                p.dma(Hs[:, :, t0:t0 + n], h_sb[:, :, 0:n], (hb,), self.tokbufs("H", s, t0, t0 + n))

    WSPEC = {"ada": ("ada_w%d", D, 6 * D), "in": ("w_in%d", D, IN_W), "gate": ("w_gate%d", 4 * D, D),
             "branch": ("w_branch%d", 2560, D), "out": ("w_out%d", D, D)}

    def Wt(self, kind, l=0, e=0):
        key = (kind, l, e)
        if not hasattr(self, "_w"):
            self._w = {}
        if key in self._w:
            return self._w[key]
        j = l // 2
        if kind in self.WSPEC:
            nm, r, c = self.WSPEC[kind]
            v = self.weight(nm % l, r, c)
        elif kind in ("f1", "f3"):
            v = self.weight("ffn_w%s_%d" % (kind[1], j), D, FF)
        elif kind == "f2":
            v = self.weight("ffn_w2_%d" % j, FF, D)
        elif kind in ("m1", "m3"):
            v = self.weight("moe_w%s_%d_%d" % (kind[1], j, e), D, FFE)
        elif kind == "m2":
            v = self.weight("moe_w2_%d_%d" % (j, e), FFE, D)
        elif kind.startswith("dft"):
            v = self.weight("dft_" + kind[3:], LL, LL, cast=False)
        else:
            raise KeyError(kind)
        self._w[key] = v
        return v

    def declare_weights(self):
        for l in self.layers:
            self.Wt("ada", l)
            self.Wt("in", l)
            if l == self.layers[0]:
                for nm in ("cf", "sf", "ci", "si"):
                    self.Wt("dft" + nm)
            self.Wt("gate", l)
            self.Wt("branch", l)
            self.Wt("out", l)
            if l % 2 == 0:
                for kd in ("f1", "f3", "f2"):
                    self.Wt(kd, l)
            else:
                for e in range(NEXP):
                    for kd in ("m1", "m3", "m2"):
                        self.Wt(kd, l, e)

    def dump(self, name, ap, shape, dtype):
        o = self.ext_out("dbg_" + name, shape, dtype)
        self.p.barrier()
        self.p.dma(o, ap, (), ())

    def small(self, name, ncols, dtype=F32):
        if name not in self.sp_in:
            self.sp_in[name] = self.ext_in(name, (128, ncols), dtype)
        return self.sp_in[name]

    def load_small(self, name, ncols, shape=None):
        p = self.p
        t = p.tile([128, ncols], F32)
        b = Buf()
        p.dma(t, self.small(name, ncols)[:, :], (), (b,))
        return t, b

    def load_hseq(self, src, s, nch, t_sb, blkbufs, name):
        p = self.p
        v = src.rearrange("(j p) t -> p j t", p=128)
        for bi, (t0, n, col) in enumerate(self.seq_blocks(s)):
            p.dma(t_sb[:, :, t0:t0 + n], v[:, :, t0:t0 + n], self.tokbufs(name, s, t0, t0 + n), (blkbufs[bi],))

    PROJ_TILES = [(0, 512, 0), (512, 512, 4), (1024, 512, 8), (1536, 512, 12), (2048, 512, 16),
                  (3072, 512, 20), (3584, 512, 24), (4112, 512, 28), (4624, 512, 32), (5136, 256, 36)]

    def run_proj(self, l):
        p = self.p
        self.new_stage()
        ps, psb = self.ps, self.psb
        Win, winb = self.Wt("in", l)
        Wg, wgb = self.Wt("gate", l)
        Winv = Win.rearrange("(kc p) n -> p kc n", p=128)
        Wgv = Wg.rearrange("(i kc p) n -> p i kc n", i=4, p=128)
        bg, bgb = self.load_small("bgate", DEPTH * 4 * KC)
        h_sb = p.tile([128, KC, T], BF16)
        hblk = bufs(5)
        wt = [p.tile([128, KC, 512], BF16) for _ in range(2)]
        wtb = bufs(2)
        st32 = [p.tile([128, T], F32) for _ in range(2)]
        st32b = bufs(2)
        st16 = [p.tile([128, T], BF16) for _ in range(2)]
        st16b = bufs(2)
        wz = p.tile([128, KC, 512], BF16)
        wdt = p.tile([128, KC, 16], BF16)
        wv = p.tile([128, KC, 256], BF16)
        wtmb = Buf()
        pt = [p.tile([128, NTM], F32) for _ in range(2)]
        ptb = bufs(2)
        p.dma(wz, Winv[:, :, OFF_SSD:OFF_SSD + 512], (winb,), (wtmb,))
        p.dma(wdt, Winv[:, :, OFF_ATT - 16:OFF_ATT], (winb,), (wtmb,))
        p.dma(wv, Winv[:, :, IN_W - 256:IN_W], (winb,), (wtmb,))
        tiles = [("in",) + t for t in self.PROJ_TILES] + [("gate", i, c0) for i in range(4) for c0 in range(0, D, 512)]
        bank = [0]

        def load(ti):
            tl = tiles[ti]
            w_sb, w_b = wt[ti % 2], wtb[ti % 2]
            if tl[0] == "in":
                p.dma(w_sb[:, :, 0:tl[2]], Winv[:, :, tl[1]:tl[1] + tl[2]], (winb,), (w_b,))
            else:
                p.dma(w_sb, Wgv[:, tl[1], :, tl[2]:tl[2] + 512], (wgb,), (w_b,))

        nst = [0]
        for s in range(NSEQ):
            self.load_hseq(self.H[s * D:(s + 1) * D, :], s, KC, h_sb, hblk, "H")
            load(0)
            for ti, tl in enumerate(tiles):
                if ti + 1 < len(tiles):
                    load(ti + 1)
                w_sb, w_b = wt[ti % 2], wtb[ti % 2]
                nch = (tl[2] if tl[0] == "in" else 512) // 128
                for c in range(nch):
                    si = nst[0] % 2
                    nst[0] += 1
                    for bi, (t0, n, col) in enumerate(self.seq_blocks(s)):
                        bk = bank[0] % 6
                        bank[0] += 1
                        for k in range(KC):
                            p.mm(ps[:, bk, 0:n], w_sb[:, k, c * 128:(c + 1) * 128], h_sb[:, k, t0:t0 + n],
                                 k == 0, k == KC - 1, (w_b, hblk[bi]), (psb[bk],))
                        if tl[0] == "in":
                            p.copy(st32[si][:, t0:t0 + n], ps[:, bk, 0:n], (psb[bk],), (st32b[si],),
                                   eng=("act" if bi % 2 else "dve"))
                        else:
                            dc = tl[2] // 128 + c
                            o = ((l * 4 + tl[1]) * KC + dc)
                            p.act(st16[si][:, t0:t0 + n], ps[:, bk, 0:n], AF.Sigmoid, (psb[bk], bgb), (st16b[si],),
                                  bias=bg[:, o:o + 1])
                    if tl[0] == "in":
                        ch = tl[3] + c
                        r0 = (s * NFM + ch) * 128
                        p.dma(self.PF[r0:r0 + 128, :], st32[si], (st32b[si],), (self.B("PF", s, ch),))
                    else:
                        dc = tl[2] // 128 + c
                        r0 = ((s * 4 + tl[1]) * KC + dc) * 128
                        p.dma(self.G[r0:r0 + 128, :], st16[si], (st16b[si],), (self.B("G", s, tl[1], dc),))
            for tt in range(T // 128):
                bi = 0 if tt < 2 else 1 + (tt - 2) // 4
                pi = tt % 2
                for k in range(KC):
                    p.mm(ps[:, 6, :], h_sb[:, k, tt * 128:(tt + 1) * 128], wz[:, k, :], k == 0, k == KC - 1,
                         (hblk[bi], wtmb), (psb[6],))
                for k in range(KC):
                    p.mm(ps[:, 7, 0:16], h_sb[:, k, tt * 128:(tt + 1) * 128], wdt[:, k, :], k == 0, k == KC - 1,
                         (hblk[bi], wtmb), (psb[7],))
                for k in range(KC):
                    p.mm(ps[:, 7, 16:272], h_sb[:, k, tt * 128:(tt + 1) * 128], wv[:, k, :], k == 0, k == KC - 1,
                         (hblk[bi], wtmb), (psb[7],))
                p.copy(pt[pi][:, 0:512], ps[:, 6, :], (psb[6],), (ptb[pi],), eng="act")
                p.copy(pt[pi][:, 512:NTM], ps[:, 7, 0:272], (psb[7],), (ptb[pi],))
                r0 = s * T + tt * 128
                p.dma(self.PT[r0:r0 + 128, :], pt[pi], (ptb[pi],), (self.B("PT", s, tt),))

    BR_K = [(0, 4), (4, 8), (8, 12), (12, 20)]

    def run_merge(self, l):
        p = self.p
        self.new_stage()
        ps, psb = self.ps, self.psb
        Wb, wbb = self.Wt("branch", l)
        Wbv = Wb.rearrange("(kc p) n -> p kc n", p=128)
        y_sb = p.tile([128, 20, T], BF16)
        yblk = bufs(5)
        wt = [p.tile([128, 20, 512], BF16) for _ in range(2)]
        wtb = bufs(2)
        g_sb = [p.tile([128, 4, 512], BF16) for _ in range(2)]
        gb = bufs(2)
        m_sb = [p.tile([128, 512], F32) for _ in range(2)]
        mb = bufs(2)
        tmp = [p.tile([128, 512], F32) for _ in range(3)]
        tmpb = bufs(3)
        st16 = [p.tile([128, T], BF16) for _ in range(2)]
        st16b = bufs(2)
        it = 0
        for s in range(NSEQ):
            self.load_hseq(self.Y[s * 2560:(s + 1) * 2560, :], s, 20, y_sb, yblk, "Y")
            Gs = self.G[s * 4 * D:(s + 1) * 4 * D, :].rearrange("(i c p) t -> p i c t", i=4, c=KC)
            p.dma(wt[0], Wbv[:, :, 0:512], (wbb,), (wtb[0],))
            for ti in range(4):
                if ti + 1 < 4:
                    p.dma(wt[(ti + 1) % 2], Wbv[:, :, (ti + 1) * 512:(ti + 2) * 512], (wbb,), (wtb[(ti + 1) % 2],))
                w_sb, w_b = wt[ti % 2], wtb[ti % 2]
                for c in range(4):
                    dc = ti * 4 + c
                    si = dc % 2
                    for bi, (t0, n, col) in enumerate(self.seq_blocks(s)):
                        gi = it % 2
                        it += 1
                        p.dma(g_sb[gi][:, :, 0:n], Gs[:, :, dc, t0:t0 + n], [self.B("G", s, i, dc) for i in range(4)], (gb[gi],))
                        for i, (k0, k1) in enumerate(self.BR_K):
                            bk = (it % 2) * 4 + i
                            for k in range(k0, k1):
                                p.mm(ps[:, bk, 0:n], w_sb[:, k, c * 128:(c + 1) * 128], y_sb[:, k, t0:t0 + n],
                                     k == k0, k == k1 - 1, (w_b, yblk[bi]), (psb[bk],))
                        m = m_sb[gi]
                        bk0 = (it % 2) * 4
                        p.tt(m[:, 0:n], g_sb[gi][:, 0, 0:n], ps[:, bk0, 0:n], ALU.mult, (gb[gi], psb[bk0]), (mb[gi],))
                        for i in range(1, 4):
                            p.tt(tmp[i - 1][:, 0:n], g_sb[gi][:, i, 0:n], ps[:, bk0 + i, 0:n], ALU.mult,
                                 (gb[gi], psb[bk0 + i]), (tmpb[i - 1],))
                        p.tt(m[:, 0:n], m[:, 0:n], tmp[0][:, 0:n], ALU.add, (mb[gi], tmpb[0]), (mb[gi],), eng="pool")
                        p.tt(tmp[1][:, 0:n], tmp[1][:, 0:n], tmp[2][:, 0:n], ALU.add, (tmpb[1], tmpb[2]), (tmpb[1],), eng="pool")
                        p.tt(st16[si][:, t0:t0 + n], m[:, 0:n], tmp[1][:, 0:n], ALU.add, (mb[gi], tmpb[1]), (st16b[si],), eng="pool")
                    r0 = s * D + dc * 128
                    p.dma(self.M[r0:r0 + 128, :], st16[si], (st16b[si],), (self.B("M", s, dc),))

    def run_out(self, l):
        p = self.p
        self.new_stage()
        ps, psb = self.ps, self.psb
        Wo, wob = self.Wt("out", l)
        Wov = Wo.rearrange("(kc p) n -> p kc n", p=128)
        m_sb = p.tile([128, KC, T], BF16)
        mblk = bufs(5)
        wt = [p.tile([128, KC, 512], BF16) for _ in range(2)]
        wtb = bufs(2)
        xr = [p.tile([128, T], F32) for _ in range(2)]
        xrb = bufs(2)
        bank = 0
        for s in range(NSEQ):
            v = self.M[s * D:(s + 1) * D, :].rearrange("(j p) t -> p j t", p=128)
            for bi, (t0, n, col) in enumerate(self.seq_blocks(s)):
                p.dma(m_sb[:, :, t0:t0 + n], v[:, :, t0:t0 + n], [self.B("M", s, dc) for dc in range(KC)], (mblk[bi],))
            allx = self.tokbufs("X", s, 0, T)
            p.dma(wt[0], Wov[:, :, 0:512], (wob,), (wtb[0],))
            for ti in range(4):
                if ti + 1 < 4:
                    p.dma(wt[(ti + 1) % 2], Wov[:, :, (ti + 1) * 512:(ti + 2) * 512], (wob,), (wtb[(ti + 1) % 2],))
                w_sb, w_b = wt[ti % 2], wtb[ti % 2]
                for c in range(4):
                    dc = ti * 4 + c
                    xi = dc % 2
                    r0 = s * D + dc * 128
                    p.dma(xr[xi], self.X[r0:r0 + 128, :], allx, (xrb[xi],))
                    for bi, (t0, n, col) in enumerate(self.seq_blocks(s)):
                        bk = bank % 8
                        bank += 1
                        for k in range(KC):
                            p.mm(ps[:, bk, 0:n], w_sb[:, k, c * 128:(c + 1) * 128], m_sb[:, k, t0:t0 + n],
                                 k == 0, k == KC - 1, (w_b, mblk[bi]), (psb[bk],))
                        p.stt(xr[xi][:, t0:t0 + n], ps[:, bk, 0:n], self.modv[:, 32 + dc, col:col + 1], xr[xi][:, t0:t0 + n],
                              ALU.mult, ALU.add, (psb[bk], self.modb, xrb[xi]), (xrb[xi],))
                    p.dma(self.X[r0:r0 + 128, :], xr[xi], (xrb[xi],), allx)

    def ffn_groups(self, s):
        out = []
        for g0, n in ((0, 512), (512, 512), (1024, 512), (1536, 512), (2048, 256)):
            subs = [(0, 256, 2), (256, 256, s)] if g0 == 0 else [(0, n, s)]
            out.append((g0, n, subs))
        return out

    def run_ffn(self, l):
        if l % 2 == 1:
            return self.run_moe(l)
        p = self.p
        self.new_stage()
        ps, psb = self.ps, self.psb
        W1, w1b = self.Wt("f1", l)
        W3, w3b = self.Wt("f3", l)
        W2, w2b = self.Wt("f2", l)
        W1v = W1.rearrange("(kc p) n -> p kc n", p=128)
        W3v = W3.rearrange("(kc p) n -> p kc n", p=128)
        W2v = W2.rearrange("(kc p) n -> p kc n", p=128)
        NFC = FF // 128
        f_sb = p.tile([128, KC, 512], BF16)
        fbuf = Buf()
        w1t = [p.tile([128, KC, 256], BF16) for _ in range(2)]
        w3t = [p.tile([128, KC, 256], BF16) for _ in range(2)]
        w13b = bufs(2)
        a_sb = p.tile([128, NFC, 512], BF16)
        ab = bufs(NFC)
        w2t = [p.tile([128, NFC, 256], BF16) for _ in range(2)]
        w2tb = bufs(2)
        tmp = [p.tile([128, 512], F32) for _ in range(2)]
        tmpb = bufs(2)
        xt = [p.tile([128, 512], F32) for _ in range(2)]
        xtb = bufs(2)
        t1 = [(c0, min(256, FF - c0)) for c0 in range(0, FF, 256)]
        it = 0
        for s in range(NSEQ):
            Hs = self.H[s * D:(s + 1) * D, :].rearrange("(j p) t -> p j t", p=128)
            for (g0, n, subs) in self.ffn_groups(s):
                p.dma(f_sb[:, :, 0:n], Hs[:, :, g0:g0 + n], self.tokbufs("H", s, g0, g0 + n), (fbuf,))

                def ld1(ti):
                    c0, nc_ = t1[ti]
                    p.dma(w1t[ti % 2][:, :, 0:nc_], W1v[:, :, c0:c0 + nc_], (w1b,), (w13b[ti % 2],))
                    p.dma(w3t[ti % 2][:, :, 0:nc_], W3v[:, :, c0:c0 + nc_], (w3b,), (w13b[ti % 2],))

                def ld2(ti):
                    p.dma(w2t[ti % 2], W2v[:, :, ti * 256:(ti + 1) * 256], (w2b,), (w2tb[ti % 2],))

                ld1(0)
                for ti, (c0, nc_) in enumerate(t1):
                    if ti + 1 < len(t1):
                        ld1(ti + 1)
                    else:
                        ld2(0)
                    for c in range(nc_ // 128):
                        ffc = c0 // 128 + c
                        ba, bb = (it % 2) * 2, (it % 2) * 2 + 1
                        it += 1
                        for k in range(KC):
                            p.mm(ps[:, ba, 0:n], w1t[ti % 2][:, k, c * 128:(c + 1) * 128], f_sb[:, k, 0:n], k == 0, k == KC - 1,
                                 (w13b[ti % 2], fbuf), (psb[ba],))
                        for k in range(KC):
                            p.mm(ps[:, bb, 0:n], w3t[ti % 2][:, k, c * 128:(c + 1) * 128], f_sb[:, k, 0:n], k == 0, k == KC - 1,
                                 (w13b[ti % 2], fbuf), (psb[bb],))
                        tb = it % 2
                        p.act(tmp[tb][:, 0:n], ps[:, ba, 0:n], AF.Silu, (psb[ba],), (tmpb[tb],))
                        p.tt(a_sb[:, ffc, 0:n], tmp[tb][:, 0:n], ps[:, bb, 0:n], ALU.mult, (tmpb[tb], psb[bb]), (ab[ffc],))
                for ti in range(8):
                    if ti + 1 < 8:
                        ld2(ti + 1)
                    for c in range(2):
                        dc = ti * 2 + c
                        bk = 4 + dc % 4
                        xi = dc % 2
                        r0 = s * D + dc * 128
                        xbs = self.tokbufs("X", s, g0, g0 + n)
                        p.dma(xt[xi][:, 0:n], self.X[r0:r0 + 128, g0:g0 + n], xbs, (xtb[xi],))
                        for k in range(NFC):
                            p.mm(ps[:, bk, 0:n], w2t[ti % 2][:, k, c * 128:(c + 1) * 128], a_sb[:, k, 0:n], k == 0, k == NFC - 1,
                                 (w2tb[ti % 2], ab[k]), (psb[bk],))
                        for (o, m, col) in subs:
                            p.stt(xt[xi][:, o:o + m], ps[:, bk, o:o + m], self.modv[:, 80 + dc, col:col + 1], xt[xi][:, o:o + m],
                                  ALU.mult, ALU.add, (psb[bk], self.modb, xtb[xi]), (xtb[xi],))
                        p.dma(self.X[r0:r0 + 128, g0:g0 + n], xt[xi][:, 0:n], (xtb[xi],), xbs)

    def run_moe(self, l):
        p = self.p
        self.new_stage()
        ps, psb = self.ps, self.psb
        NFC = FFE // 128
        f_sb = p.tile([128, KC, 512], BF16)
        fbuf = Buf()
        w1t = [p.tile([128, KC, 256], BF16) for _ in range(2)]
        w3t = [p.tile([128, KC, 256], BF16) for _ in range(2)]
        w13b = bufs(2)
        a_sb = p.tile([128, NFC, 512], BF16)
        ab = bufs(NFC)
        w2t = [p.tile([128, NFC, 256], BF16) for _ in range(2)]
        w2tb = bufs(2)
        acc = p.tile([128, KC, 512], F32)
        accb = bufs(KC)
        tmp = [p.tile([128, 512], F32) for _ in range(2)]
        tmpb = bufs(2)
        tmp2 = [p.tile([128, 512], F32) for _ in range(2)]
        tmp2b = bufs(2)
        gt = [p.tile([128, 512], F32) for _ in range(2)]
        gtb = bufs(2)
        xt = [p.tile([128, 512], F32) for _ in range(2)]
        xtb = bufs(2)
        GF = self.GATES[l]
        it = 0
        for s in range(NSEQ):
            Hs = self.H[s * D:(s + 1) * D, :].rearrange("(j p) t -> p j t", p=128)
            for (g0, n, subs) in self.ffn_groups(s):
                p.dma(f_sb[:, :, 0:n], Hs[:, :, g0:g0 + n], self.tokbufs("H", s, g0, g0 + n), (fbuf,))
                for e in range(NEXP):
                    W1, w1b = self.Wt("m1", l, e)
                    W3, w3b = self.Wt("m3", l, e)
                    W2, w2b = self.Wt("m2", l, e)
                    W1v = W1.rearrange("(kc p) n -> p kc n", p=128)
                    W3v = W3.rearrange("(kc p) n -> p kc n", p=128)
                    W2v = W2.rearrange("(kc p) n -> p kc n", p=128)
                    gi = e % 2
                    p.dma(gt[gi][:, 0:n], GF[e, s * T + g0:s * T + g0 + n].partition_broadcast(128),
                          self.tokbufs("GATES%d" % l, s, g0, g0 + n), (gtb[gi],))

                    def ld1(ti):
                        p.dma(w1t[ti % 2], W1v[:, :, ti * 256:(ti + 1) * 256], (w1b,), (w13b[ti % 2],))
                        p.dma(w3t[ti % 2], W3v[:, :, ti * 256:(ti + 1) * 256], (w3b,), (w13b[ti % 2],))

                    def ld2(ti):
                        p.dma(w2t[ti % 2], W2v[:, :, ti * 256:(ti + 1) * 256], (w2b,), (w2tb[ti % 2],))

                    ld1(0)
                    for ti in range(16):
                        if ti + 1 < 16:
                            ld1(ti + 1)
                        else:
                            ld2(0)
                        for c in range(2):
                            ffc = ti * 2 + c
                            ba, bb = (it % 2) * 2, (it % 2) * 2 + 1
                            it += 1
                            for k in range(KC):
                                p.mm(ps[:, ba, 0:n], w1t[ti % 2][:, k, c * 128:(c + 1) * 128], f_sb[:, k, 0:n], k == 0, k == KC - 1,
                                     (w13b[ti % 2], fbuf), (psb[ba],))
                            for k in range(KC):
                                p.mm(ps[:, bb, 0:n], w3t[ti % 2][:, k, c * 128:(c + 1) * 128], f_sb[:, k, 0:n], k == 0, k == KC - 1,
                                     (w13b[ti % 2], fbuf), (psb[bb],))
                            tb = it % 2
                            p.act(tmp[tb][:, 0:n], ps[:, ba, 0:n], AF.Silu, (psb[ba],), (tmpb[tb],))
                            p.tt(tmp2[tb][:, 0:n], tmp[tb][:, 0:n], ps[:, bb, 0:n], ALU.mult, (tmpb[tb], psb[bb]), (tmp2b[tb],))
                            p.tt(a_sb[:, ffc, 0:n], tmp2[tb][:, 0:n], gt[gi][:, 0:n], ALU.mult, (tmp2b[tb], gtb[gi]), (ab[ffc],), eng="pool")
                    for ti in range(8):
                        if ti + 1 < 8:
                            ld2(ti + 1)
                        for c in range(2):
                            dc = ti * 2 + c
                            bk = 4 + dc % 4
                            for k in range(NFC):
                                p.mm(ps[:, bk, 0:n], w2t[ti % 2][:, k, c * 128:(c + 1) * 128], a_sb[:, k, 0:n], k == 0, k == NFC - 1,
                                     (w2tb[ti % 2], ab[k]), (psb[bk],))
                            if e == 0:
                                p.copy(acc[:, dc, 0:n], ps[:, bk, 0:n], (psb[bk],), (accb[dc],))
                            else:
                                p.tt(acc[:, dc, 0:n], acc[:, dc, 0:n], ps[:, bk, 0:n], ALU.add, (accb[dc], psb[bk]), (accb[dc],))
                xbs = self.tokbufs("X", s, g0, g0 + n)
                for dc in range(KC):
                    xi = dc % 2
                    r0 = s * D + dc * 128
                    p.dma(xt[xi][:, 0:n], self.X[r0:r0 + 128, g0:g0 + n], xbs, (xtb[xi],))
                    for (o, m, col) in subs:
                        p.stt(xt[xi][:, o:o + m], acc[:, dc, o:o + m], self.modv[:, 80 + dc, col:col + 1], xt[xi][:, o:o + m],
                              ALU.mult, ALU.add, (accb[dc], self.modb, xtb[xi]), (xtb[xi],))
                    p.dma(self.X[r0:r0 + 128, g0:g0 + n], xt[xi][:, 0:n], (xtb[xi],), xbs)

    def router_setup(self, l):
        p = self.p
        j = l // 2
        if not hasattr(self, "GATES"):
            self.GATES = {}
        self.GATES[l] = self.scratch("GATES%d" % l, (NEXP, NSEQ * T), F32)
        self.wr, self.wrb = self.load_small("router%d" % j, KC * NEXP)
        self.rb = p.tile([128, NEXP], F32)
        rb_in = self.ext_in("router_b%d" % j, (1, NEXP))
        p.dma(self.rb, rb_in[0, :].partition_broadcast(128), (), (self.wrb,))
        self.r_t = [p.tile([128, NEXP], F32) for _ in range(6)]
        self.r_s = [p.tile([128, 1], F32) for _ in range(5)]
        self.r_b = Buf()
        self.gT = p.tile([NEXP, 512], F32)
        self.gTb = Buf()

    def router_block(self, l, s, t0, n, f_sb, fb):
        p = self.p
        ps, psb = self.ps, self.psb
        wr = self.wr.rearrange("p (k e) -> p k e", e=NEXP)
        lg, eq, l2, sel, ex, w = self.r_t
        m1, m2, nm1, sm, rs = self.r_s
        rb = self.r_b
        for tt in range(n // 128):
            for k in range(KC):
                p.mm(ps[:, 1, 0:NEXP], f_sb[:, k, tt * 128:(tt + 1) * 128], wr[:, k, :], k == 0, k == KC - 1,
                     (fb, self.wrb), (psb[1],))
            p.tt(lg, ps[:, 1, 0:NEXP], self.rb, ALU.add, (psb[1], self.wrb), (rb,))
            p.op("dve", lambda e: e.tensor_reduce(m1, lg, AX.X, ALU.max), (rb,), (rb,))
            p.ts(eq, lg, m1[:, 0:1], None, ALU.is_equal, None, (rb,), (rb,))
            p.stt(l2, eq, -1e30, lg, ALU.mult, ALU.add, (rb,), (rb,))
            p.op("dve", lambda e: e.tensor_reduce(m2, l2, AX.X, ALU.max), (rb,), (rb,))
            p.ts(sel, lg, m2[:, 0:1], None, ALU.is_ge, None, (rb,), (rb,))
            p.ts(nm1, m1, -1.0, None, ALU.mult, None, (rb,), (rb,))
            p.act(ex, lg, AF.Exp, (rb,), (rb,), bias=nm1[:, 0:1])
            p.tt(w, ex, sel, ALU.mult, (rb,), (rb,))
            p.op("dve", lambda e: e.tensor_reduce(sm, w, AX.X, ALU.add), (rb,), (rb,))
            p.recip(rs, sm, (rb,), (rb,))
            p.ts(w, w, rs[:, 0:1], None, ALU.mult, None, (rb,), (rb,))
            p.tr(ps[0:NEXP, 2, 0:128], w, self.ident_f, (rb, self.cb), (psb[2],))
            p.copy(self.gT[:, tt * 128:(tt + 1) * 128], ps[0:NEXP, 2, 0:128], (psb[2],), (self.gTb,))
        p.dma(self.GATES[l][:, s * T + t0:s * T + t0 + n], self.gT[:, 0:n], (self.gTb,),
              self.tokbufs("GATES%d" % l, s, t0, t0 + n))

    def stage_final(self):
        p = self.p
        self.new_stage()
        ps, psb = self.ps, self.psb
        out = self.ext_out("out", (NSEQ * LL, D))
        x_sb = p.tile([128, KC, 512], F32)
        sq_sb = p.tile([128, KC, 512], F32)
        rs_sb = p.tile([128, 512], F32)
        o_sb = [p.tile([128, D], F32) for _ in range(2)]
        xb, sqb, rsb = bufs(3)
        ob = bufs(2)
        gf0 = 2 * DEPTH * KC
        it = 0
        for s in range(NSEQ):
            Xs = self.X[s * D:(s + 1) * D, :].rearrange("(j p) t -> p j t", p=128)
            for g in range(4):
                t0 = LC + g * 512
                p.dma(x_sb, Xs[:, :, t0:t0 + 512], self.tokbufs("X", s, t0, t0 + 512), (xb,))
                p.act(sq_sb, x_sb, AF.Square, (xb,), (sqb,))
                for j in range(KC):
                    p.mm(ps[:, 0, :], self.ones_f, sq_sb[:, j, :], j == 0, j == KC - 1, (sqb, self.cb), (psb[0],))
                p.act(rs_sb, ps[:, 0, :], AF.Sqrt, (psb[0], self.cb), (rsb,), bias=self.eps_t, scale=1.0 / D)
                p.recip(rs_sb, rs_sb, (rsb,), (rsb,))
                for j in range(KC):
                    p.stt(sq_sb[:, j, :], x_sb[:, j, :], self.normg[:, gf0 + j:gf0 + j + 1], rs_sb, ALU.mult, ALU.mult,
                          (xb, rsb, self.cb), (sqb,))
                for tt in range(4):
                    oi = it % 2
                    it += 1
                    for bq in range(4):
                        bank = 1 + (it % 2) * 3 + (bq % 3) if False else 4 + bq
                        for c in range(4):
                            j = bq * 4 + c
                            p.tr(ps[:, bank, c * 128:(c + 1) * 128], sq_sb[:, j, tt * 128:(tt + 1) * 128], self.ident_f,
                                 (sqb, self.cb), (psb[bank],))
                        p.copy(o_sb[oi][:, bq * 512:(bq + 1) * 512], ps[:, bank, :], (psb[bank],), (ob[oi],),
                               eng=("act" if bq % 2 else "dve"))
                    r0 = s * LL + g * 512 + tt * 128
                    p.dma(out[r0:r0 + 128, :], o_sb[oi], (ob[oi],), ())

    def run_lru(self, l):
        p = self.p
        self.new_stage()
        ps, psb = self.ps, self.psb
        cw, cwb = self.load_small("lru_cw", DEPTH * 4 * 4)
        cbias, cbb = self.load_small("lru_cb", DEPTH * 4)
        ba, bab = self.load_small("lru_ba", DEPTH * 2 * 4)
        bx, bxb = self.load_small("lru_bx", DEPTH * 2 * 4)
        lam, lamb = self.load_small("lru_lam", DEPTH * 2 * 4)
        if "lru_wa" not in self.sp_in:
            self.sp_in["lru_wa"] = self.ext_in("lru_wa", (DEPTH * 2 * 8 * 64, 64))
            self.sp_in["lru_wx"] = self.ext_in("lru_wx", (DEPTH * 2 * 8 * 64, 64))
        stg = p.tile([128, 16, 128], F32)
        bd = p.tile([128, 16, 128], BF16)
        bdb = Buf()
        sgb = bufs(16)
        for i in range(16):
            p.memset(stg[:, i, :], 0.0, (sgb[i],), eng="pool")
        for d in range(2):
            for mi, nm in enumerate(("lru_wa", "lru_wx")):
                for j in range(4):
                    idx = (d * 2 + mi) * 4 + j
                    for half in range(2):
                        r0 = ((l * 2 + d) * 8 + 2 * j + half) * 64
                        p.dma(stg[half * 64:(half + 1) * 64, idx, half * 64:(half + 1) * 64], self.sp_in[nm][r0:r0 + 64, :],
                              (), (sgb[idx],))
        p.copy(bd, stg, sgb, (bdb,))
        nl = p.tile([128, 8], F32)
        nl2 = p.tile([128, 8], F32)
        p.act(nl, lam[:, l * 8:(l + 1) * 8], AF.Exp, (lamb,), (bdb,), scale=-1.0)
        p.act(nl, nl, AF.Ln, (bdb,), (bdb,), bias=self.one_t)
        p.ts(nl2, nl, -16.0, None, ALU.mult, None, (bdb,), (bdb,))
        p.ts(nl, nl, -8.0, None, ALU.mult, None, (bdb,), (bdb,))
        XP = T + 6
        gate = p.tile([128, T], F32)
        xp = p.tile([128, XP], F32)
        xc = p.tile([128, T], F32)
        xcb = p.tile([128, T], BF16)
        r_sb = p.tile([128, T], F32)
        i_sb = p.tile([128, T], F32)
        a_sb = p.tile([128, T], F32)
        t_sb = p.tile([128, T], F32)
        hf = p.tile([128, T], F32)
        hb = p.tile([128, T], F32)
        u_sb = p.tile([128, T], F32)
        y_sb = p.tile([128, T], BF16)
        gb_, xpb, xcbuf, xcbb, rb_, ib_, ab_, tb_, hfb, hbb, ub, yb = bufs(12)
        p.memset(xp, 0.0, (xpb,))
        bank = 0
        for s in range(NSEQ):
            for j in range(4):
                rg = (s * NFM + PF_LRU + j) * 128
                rx = (s * NFM + PF_LRU + 4 + j) * 128
                p.dma(gate, self.PF[rg:rg + 128, :], (self.B("PF", s, PF_LRU + j),), (gb_,))
                p.dma(xp[:, 2:2 + LC], self.PF[rx:rx + 128, 0:LC], (self.B("PF", s, PF_LRU + 4 + j),), (xpb,))
                p.dma(xp[:, 5 + LC:5 + T], self.PF[rx:rx + 128, LC:T], (self.B("PF", s, PF_LRU + 4 + j),), (xpb,))
                for (o0, n, base) in ((0, LC, 0), (LC, LL, LC + 3)):
                    for k in range(4):
                        w = cw[:, (l * 4 + k) * 4 + j:(l * 4 + k) * 4 + j + 1]
                        src = xp[:, base + k:base + k + n]
                        if k == 0:
                            p.ts(xc[:, o0:o0 + n], src, w, cbias[:, l * 4 + j:l * 4 + j + 1], ALU.mult, ALU.add,
                                 (xpb, cwb, cbb), (xcbuf,))
                        else:
                            p.stt(xc[:, o0:o0 + n], src, w, xc[:, o0:o0 + n], ALU.mult, ALU.add, (xpb, cwb, xcbuf), (xcbuf,))
                p.copy(xcb, xc, (xcbuf,), (xcbb,), eng="act")
                for d in range(2):
                    for (t0, n, col) in self.seq_blocks(s):
                        for mi, (dst, dstb, bias_t, bias_b) in enumerate(((r_sb, rb_, ba, bab), (i_sb, ib_, bx, bxb))):
                            bk = bank % 8
                            bank += 1
                            idx = (d * 2 + mi) * 4 + j
                            p.mm(ps[:, bk, 0:n], bd[:, idx, :], xcb[:, t0:t0 + n], True, True, (bdb, xcbb), (psb[bk],))
                            o = (l * 2 + d) * 4 + j
                            p.act(dst[:, t0:t0 + n], ps[:, bk, 0:n], AF.Sigmoid, (psb[bk], bias_b), (dstb,), bias=bias_t[:, o:o + 1])
                    p.act(a_sb, r_sb, AF.Exp, (rb_, bdb), (ab_,), scale=nl[:, d * 4 + j:d * 4 + j + 1])
                    p.act(t_sb, r_sb, AF.Exp, (rb_, bdb), (tb_,), scale=nl2[:, d * 4 + j:d * 4 + j + 1])
                    p.act(t_sb, t_sb, AF.Sqrt, (tb_, self.cb), (tb_,), bias=self.one_t, scale=-1.0)
                    p.tt(t_sb, t_sb, i_sb, ALU.mult, (tb_, ib_), (tb_,))
                    p.tt(t_sb, t_sb, xc, ALU.mult, (tb_, xcbuf), (tb_,), eng="pool")
                    if d == 0:
                        p.op("dve", lambda e: e.tensor_tensor_scan(hf, a_sb, t_sb, 0.0, ALU.mult, ALU.add), (ab_, tb_), (hfb,))
                    else:
                        p.op("dve", lambda e: e.tensor_tensor_scan(hb[:, 0:LC][:, ::-1], a_sb[:, 0:LC][:, ::-1], t_sb[:, 0:LC][:, ::-1],
                                                                  0.0, ALU.mult, ALU.add), (ab_, tb_), (hbb,))
                        p.op("dve", lambda e: e.tensor_tensor_scan(hb[:, LC:T][:, ::-1], a_sb[:, LC:T][:, ::-1], t_sb[:, LC:T][:, ::-1],
                                                                  hb[:, 0:1], ALU.mult, ALU.add), (ab_, tb_, hbb), (hbb,))
                p.tt(u_sb, gate, gate, ALU.mult, (gb_,), (ub,), eng="pool")
                p.ts(u_sb, u_sb, 0.044715, 1.0, ALU.mult, ALU.add, (ub,), (ub,), eng="pool")
                p.tt(u_sb, u_sb, gate, ALU.mult, (ub, gb_), (ub,), eng="pool")
                p.act(u_sb, u_sb, AF.Sigmoid, (ub,), (ub,), scale=1.5957691216057308)
                p.tt(u_sb, u_sb, gate, ALU.mult, (ub, gb_), (ub,), eng="pool")
                p.tt(hf, hf, hb, ALU.add, (hfb, hbb), (hfb,))
                p.tt(y_sb, u_sb, hf, ALU.mult, (ub, hfb), (yb,))
                r0 = s * 2560 + 512 + j * 128
                p.dma(self.Y[r0:r0 + 128, :], y_sb, (yb,), self.tokbufs("Y", s, 0, T))

    def run_att(self, l):
        p = self.p
        self.new_stage()
        ps, psb = self.ps, self.psb
        SC = 128.0 ** -0.5
        qn, qnb_ = self.load_small("att_qn", DEPTH)
        kn, knb_ = self.load_small("att_kn", DEPTH)
        cos_t, cosb = self.load_small("rope_cos", LL)
        sin_t, sinb = self.load_small("rope_sin", LL)
        rot32, rotb = self.load_small("rotm", 128)
        if "att_rows" not in self.sp_in:
            self.sp_in["att_rows"] = self.ext_in("att_rows", (1, DEPTH * 256))
        rot = p.tile([128, 128], BF16)
        p.copy(rot, rot32, (rotb,), (rotb,))
        gqs = p.tile([128, 1], F32)
        p.ts(gqs, qn[:, l:l + 1], SC, None, ALU.mult, None, (qnb_,), (qnb_,))
        rows = p.tile([1, 256], F32)
        mx = p.tile([1, 4], F32)
        nb = p.tile([128, 1], F32)
        nbb = Buf()
        p.dma(rows, self.sp_in["att_rows"][0:1, l * 256:(l + 1) * 256], (), (nbb,))
        p.op("dve", lambda e: e.tensor_reduce(mx[:, 0:2], rows.rearrange("p (a b) -> p a b", a=2), AX.X, ALU.max,
                                              apply_absolute_value=True), (nbb,), (nbb,))
        p.tt(mx[:, 2:3], mx[:, 0:1], mx[:, 1:2], ALU.mult, (nbb,), (nbb,))
        p.ts(mx[:, 3:4], mx[:, 2:3], -(128.0 ** 0.5), None, ALU.mult, None, (nbb,), (nbb,))
        p.mm(ps[:, 0, 0:1], self.ones_f[0:1, :], mx[:, 3:4], True, True, (nbb, self.cb), (psb[0],))
        p.copy(nb, ps[:, 0, 0:1], (psb[0],), (nbb,))
        x32 = p.tile([128, T], F32)
        sq = p.tile([128, T], F32)
        rs = p.tile([128, 512], F32)
        qn32 = p.tile([128, T], F32)
        qnb = p.tile([128, LL], BF16)
        t1 = p.tile([128, 512], F32)
        t2 = p.tile([128, 512], F32)
        qk = p.tile([128, 10, T], BF16)
        qkb = bufs(10)
        v32 = p.tile([128, 18, 256], F32)
        v_sb = p.tile([128, 18, 256], BF16)
        pT = [p.tile([128, 512], BF16) for _ in range(3)]
        pTb = bufs(3)
        rc = p.tile([128, 512], F32)
        ost = [p.tile([128, 512], BF16) for _ in range(2)]
        ostb = bufs(2)
        x32b, sqb, rsb, qn32b, qnbb, t1b, t2b, v32b, vb, rcb = bufs(10)
        bank = 0
        io = 0
        for s in range(NSEQ):
            for hh in range(10):
                r0 = (s * NFM + PF_ATT + hh) * 128
                g = gqs if hh < 8 else kn[:, l:l + 1]
                p.dma(x32, self.PF[r0:r0 + 128, :], (self.B("PF", s, PF_ATT + hh),), (x32b,))
                p.act(sq, x32, AF.Square, (x32b,), (sqb,))
                for (t0, n, col) in self.seq_blocks(s):
                    bk = bank % 4
                    bank += 1
                    p.mm(ps[:, bk, 0:n], self.ones_f, sq[:, t0:t0 + n], True, True, (sqb, self.cb), (psb[bk],))
                    p.act(rs[:, 0:n], ps[:, bk, 0:n], AF.Sqrt, (psb[bk], self.cb), (rsb,), bias=self.eps_t, scale=1.0 / 128)
                    p.recip(rs[:, 0:n], rs[:, 0:n], (rsb,), (rsb,))
                    p.stt(qn32[:, t0:t0 + n], x32[:, t0:t0 + n], g, rs[:, 0:n], ALU.mult, ALU.mult,
                          (x32b, rsb, qnb_, knb_), (qn32b,))
                p.copy(qk[:, hh, 0:LC], qn32[:, 0:LC], (qn32b,), (qkb[hh],), eng="act")
                p.copy(qnb, qn32[:, LC:T], (qn32b,), (qnbb,), eng="act")
                for gblk in range(4):
                    c0 = gblk * 512
                    bk = bank % 4
                    bank += 1
                    p.mm(ps[:, bk, :], rot, qnb[:, c0:c0 + 512], True, True, (rotb, qnbb), (psb[bk],))
                    p.tt(t1, qn32[:, LC + c0:LC + c0 + 512], cos_t[:, c0:c0 + 512], ALU.mult, (qn32b, cosb), (t1b,), eng="pool")
                    p.tt(t2, ps[:, bk, :], sin_t[:, c0:c0 + 512], ALU.mult, (psb[bk], sinb), (t2b,))
                    p.tt(qk[:, hh, LC + c0:LC + c0 + 512], t1, t2, ALU.add, (t1b, t2b), (qkb[hh],))
            PTs = self.PT[s * T:(s + 1) * T, :].rearrange("(c p) f -> p c f", p=128)
            p.dma(v32, PTs[:, :, TM_V:TM_V + 256], [self.B("PT", s, tt) for tt in range(18)], (v32b,))
            p.copy(v_sb, v32, (v32b,), (vb,))
            for h in range(8):
                g = h // 4
                for (q0, nq, nsc) in ((0, LC, 2), (LC, 512, 18), (LC + 512, 512, 18), (LC + 1024, 512, 18), (LC + 1536, 512, 18)):
                    bo, br = 4 + (io % 2) * 2, 5 + (io % 2) * 2
                    for sc in range(nsc):
                        bk = bank % 4
                        bank += 1
                        pi = bank % 3
                        p.mm(ps[:, bk, 0:nq], qk[:, 8 + g, sc * 128:(sc + 1) * 128], qk[:, h, q0:q0 + nq], True, True,
                             (qkb[8 + g], qkb[h]), (psb[bk],))
                        p.act(pT[pi][:, 0:nq], ps[:, bk, 0:nq], AF.Exp, (psb[bk], nbb), (pTb[pi],), bias=nb[:, 0:1])
                        p.mm(ps[:, bo, 0:nq], v_sb[:, sc, g * 128:(g + 1) * 128], pT[pi][:, 0:nq], sc == 0, sc == nsc - 1,
                             (vb, pTb[pi]), (psb[bo],))
                        p.mm(ps[:, br, 0:nq], self.ones_b, pT[pi][:, 0:nq], sc == 0, sc == nsc - 1,
                             (self.cb, pTb[pi]), (psb[br],))
                    p.recip(rc[:, 0:nq], ps[:, br, 0:nq], (psb[br],), (rcb,))
                    oi = io % 2
                    io += 1
                    p.tt(ost[oi][:, 0:nq], ps[:, bo, 0:nq], rc[:, 0:nq], ALU.mult, (psb[bo], rcb), (ostb[oi],))
                    r0 = s * 2560 + 1536 + h * 128
                    p.dma(self.Y[r0:r0 + 128, q0:q0 + nq], ost[oi][:, 0:nq], (ostb[oi],), self.tokbufs("Y", s, q0, q0 + nq))

    def run_ssd(self, l):
        p = self.p
        self.new_stage()
        ps, psb = self.ps, self.psb
        cw, cwb = self.load_small("ssd_cw", DEPTH * 4 * 8)
        cbias, cbb = self.load_small("ssd_cb", DEPTH * 8)
        cst, cstb = self.load_small("ssd_consts", 4 * 128)
        if "ssd_rows" not in self.sp_in:
            self.sp_in["ssd_rows"] = self.ext_in("ssd_rows", (1, DEPTH * 552))
        rows = p.tile([128, 552], F32)
        rwb = Buf()
        p.dma(rows, self.sp_in["ssd_rows"][0, l * 552:(l + 1) * 552].partition_broadcast(128), (), (rwb,))
        Abc = p.tile([128, 16], F32)
        p.act(Abc, rows[:, 0:16], AF.Exp, (rwb,), (rwb,))
        p.ts(Abc, Abc, -1.0, None, ALU.mult, None, (rwb,), (rwb,))
        dtb = rows[:, 16:32]
        dsk = rows[:, 32:40]
        nrm = rows[:, 40:552]
        XP = T + 6
        xpall = p.tile([128, 2 * XP], F32)
        xp = [xpall[:, i * XP:(i + 1) * XP] for i in range(2)]
        xpb = bufs(2)
        cv = p.tile([128, T], F32)
        cvb = Buf()
        big = p.tile([128, 18 * 512], F32)
        bigb = Buf()
        xs_fm = big.rearrange("p (j t) -> p j t", j=4)
        z_tm = big.rearrange("p (c f) -> p c f", c=18)
        b_fm = p.tile([128, 2, T], BF16)
        c_fm = p.tile([128, 2, T], BF16)
        bfb, cfb = bufs(2)
        xs_tm = p.tile([128, 18, 512], F32)
        xtb = Buf()
        b_tm = p.tile([128, 18, 256], BF16)
        btb = Buf()
        dtr = p.tile([128, 18, 16], F32)
        dt = p.tile([128, 18, 16], F32)
        dtA = p.tile([128, 18, 16], F32)
        dtt = p.tile([128, 18, 16], F32)
        dtrb, dtbuf = bufs(2)
        yacc = p.tile([128, 18, 512], F32)
        yab = bufs(18)
        yfm = xpall.bitcast(BF16)[:, 0:4 * T].rearrange("p (j t) -> p j t", j=4)
        yfb = Buf()
        D8 = p.tile([128, 8, 128], F32)
        seg = p.tile([128, 8, 128], F32)
        dec = p.tile([128, 8, 128], F32)
        Mt = p.tile([128, 8, 128], BF16)
        xdt = p.tile([128, 512], BF16)
        xw = p.tile([128, 512], BF16)
        tmp = p.tile([128, 512], F32)
        S = p.tile([128, 512], F32)
        Sb = p.tile([128, 512], BF16)
        csc = p.tile([128, 8], F32)
        ecs = p.tile([128, 8], F32)
        w8 = p.tile([128, 8], F32)
        ca = p.tile([128, 8], F32)
        ss = p.tile([128, 2], F32)
        yn = p.tile([128, 512], BF16)
        D8b, segb, decb, Mb, xdtb, xwb, tmpb, Sbuf_, Sbb, cscb, ecsb, w8b, cab, ssb, ynb = bufs(15)
        bank = [0]

        def h8(ap, n=64):
            return ap.rearrange("p (h q) -> p h q", h=8)

        def bc8(ap8, n):
            return ap8.unsqueeze(2).broadcast_to([128, 8, n])

        for s in range(NSEQ):
            for i_ in range(2):
                p.memset(xp[i_], 0.0, (xpb[i_], yfb))
            for j in range(8):
                xi = j % 2
                rx = (s * NFM + PF_SSD + j) * 128
                p.dma(xp[xi][:, 2:2 + LC], self.PF[rx:rx + 128, 0:LC], (self.B("PF", s, PF_SSD + j),), (xpb[xi],))
                p.dma(xp[xi][:, 5 + LC:5 + T], self.PF[rx:rx + 128, LC:T], (self.B("PF", s, PF_SSD + j),), (xpb[xi],))
                for (o0, n, base) in ((0, LC, 0), (LC, LL, LC + 3)):
                    for k in range(4):
                        w = cw[:, (l * 4 + k) * 8 + j:(l * 4 + k) * 8 + j + 1]
                        src = xp[xi][:, base + k:base + k + n]
                        if k == 0:
                            p.ts(cv[:, o0:o0 + n], src, w, cbias[:, l * 8 + j:l * 8 + j + 1], ALU.mult, ALU.add,
                                 (xpb[xi], cwb, cbb), (cvb,))
                        else:
                            p.stt(cv[:, o0:o0 + n], src, w, cv[:, o0:o0 + n], ALU.mult, ALU.add, (xpb[xi], cwb, cvb), (cvb,))
                if j < 4:
                    p.act(xs_fm[:, j, :], cv, AF.Silu, (cvb,), (bigb,))
                elif j < 6:
                    p.act(b_fm[:, j - 4, :], cv, AF.Silu, (cvb,), (bfb,))
                else:
                    p.act(c_fm[:, j - 6, :], cv, AF.Silu, (cvb,), (cfb,))
            for c in range(18):
                bk = bank[0] % 4
                bank[0] += 1
                for j in range(4):
                    p.tr(ps[:, bk, j * 128:(j + 1) * 128], xs_fm[:, j, c * 128:(c + 1) * 128], self.ident_f, (bigb, self.cb), (psb[bk],))
                p.copy(xs_tm[:, c, :], ps[:, bk, :], (psb[bk],), (xtb,), eng=("act" if c % 2 else "dve"))
                bk = 4 + bank[0] % 2
                pv = ps[:, bk, :].bitcast(BF16)
                for g in range(2):
                    p.tr(pv[:, g * 128:(g + 1) * 128], b_fm[:, g, c * 128:(c + 1) * 128], self.ident_b, (bfb, self.cb), (psb[bk],))
                p.copy(b_tm[:, c, :], pv[:, 0:256], (psb[bk],), (btb,), eng=("dve" if c % 2 else "act"))
            PTs = self.PT[s * T:(s + 1) * T, :].rearrange("(c p) f -> p c f", p=128)
            ptbufs = [self.B("PT", s, tt) for tt in range(18)]
            p.dma(dtr, PTs[:, :, TM_DT:TM_DT + 16], ptbufs, (dtrb,))
            p.tt(dtr, dtr, dtb.unsqueeze(1).broadcast_to([128, 18, 16]), ALU.add, (dtrb, rwb), (dtrb,))
            p.ts(dt, dtr, 0.0, None, ALU.max, None, (dtrb,), (dtbuf,))
            p.ts(dtt, dtr, 0.0, None, ALU.min, None, (dtrb,), (dtbuf,))
            p.tt(dtt, dtt, dt, ALU.subtract, (dtbuf,), (dtbuf,))
            p.act(dtt, dtt, AF.Exp, (dtbuf,), (dtbuf,))
            p.act(dtt, dtt, AF.Ln, (dtbuf, self.cb), (dtbuf,), bias=self.one_t)
            p.tt(dt, dt, dtt, ALU.add, (dtbuf,), (dtbuf,))
            p.tt(dtA, dt, Abc.unsqueeze(1).broadcast_to([128, 18, 16]), ALU.mult, (dtbuf, rwb), (dtbuf,))
            for d in range(2):
                order = list(range(18)) if d == 0 else [1, 0] + list(range(17, 1, -1))
                tri = cst[:, d * 128:(d + 1) * 128]
                mneg = cst[:, (2 + d) * 128:(3 + d) * 128]
                LAST = 127 if d == 0 else 0
                p.memset(S, 0.0, (Sbuf_,))
                p.memset(Sb, 0.0, (Sbb,))
                for c in order:
                    cr = slice(c * 128, (c + 1) * 128)
                    dA = dtA[:, c, d * 8:(d + 1) * 8]
                    p.tt(D8, tri.unsqueeze(1).broadcast_to([128, 8, 128]), bc8(dA, 128), ALU.mult, (cstb, dtbuf), (D8b,))
                    D8f = D8.rearrange("p h i -> p (h i)")
                    p.mm(ps[:, 0, :], self.ones_f, D8f[:, 0:512], True, True, (self.cb, D8b), (psb[0],))
                    p.mm(ps[:, 1, :], self.ones_f, D8f[:, 512:1024], True, True, (self.cb, D8b), (psb[1],))
                    csbc = ps[:, 0:2, :].rearrange("p a (h i) -> p (a h) i", h=4)
                    p.mm(ps[:, 2, 0:8], tri, dA, True, True, (cstb, dtbuf), (psb[2],))
                    p.copy(csc, ps[:, 2, 0:8], (psb[2],), (cscb,), eng="act")
                    p.tt(seg, csbc, bc8(csc, 128), ALU.subtract, (psb[0], psb[1], cscb), (segb,))
                    p.tt(seg, seg, mneg.unsqueeze(1).broadcast_to([128, 8, 128]), ALU.add, (segb, cstb), (segb,), eng="pool")
                    p.act(dec, seg, AF.Exp, (segb,), (decb,))
                    for g in range(2):
                        p.mm(ps[:, 3, g * 128:(g + 1) * 128], b_fm[:, g, cr], c_fm[:, g, cr], True, True, (bfb, cfb), (psb[3],))
                    cbt = ps[:, 3, 0:256].rearrange("p (g i) -> p g i", g=2).unsqueeze(2).broadcast_to([128, 2, 4, 128])
                    p.tt(Mt.rearrange("p (g r) i -> p g r i", g=2), dec.rearrange("p (g r) i -> p g r i", g=2), cbt, ALU.mult,
                         (decb, psb[3]), (Mb,))
                    p.tt(h8(xdt), h8(xs_tm[:, c, :]), bc8(dt[:, c, d * 8:(d + 1) * 8], 64), ALU.mult, (xtb, dtbuf), (xdtb,), eng="pool")
                    for h in range(8):
                        p.mm(ps[:, 4, h * 64:(h + 1) * 64], Mt[:, h, :], xdt[:, h * 64:(h + 1) * 64], True, True, (Mb, xdtb), (psb[4],))
                    for g in range(2):
                        p.mm(ps[:, 5, g * 256:(g + 1) * 256], c_fm[:, g, cr], Sb[:, g * 256:(g + 1) * 256], True, True, (cfb, Sbb), (psb[5],))
                    p.act(ecs, csc, AF.Exp, (cscb,), (ecsb,))
                    p.tt(h8(tmp), h8(ps[:, 5, :]), bc8(ecs, 64), ALU.mult, (psb[5], ecsb), (tmpb,))
                    if d == 0:
                        p.tt(yacc[:, c, :], tmp, ps[:, 4, :], ALU.add, (tmpb, psb[4]), (yab[c],))
                    else:
                        p.tt(tmp, tmp, ps[:, 4, :], ALU.add, (tmpb, psb[4]), (tmpb,))
                        p.tt(yacc[:, c, :], yacc[:, c, :], tmp, ALU.add, (tmpb, yab[c]), (yab[c],), eng="pool")
                    p.tt(w8, csbc[:, :, LAST], csc, ALU.subtract, (psb[0], psb[1], cscb), (w8b,))
                    p.act(w8, w8, AF.Exp, (w8b,), (w8b,))
                    p.tt(h8(xw), h8(xdt), bc8(w8, 64), ALU.mult, (xdtb, w8b), (xwb,))
                    for g in range(2):
                        p.mm(ps[:, 6, g * 256:(g + 1) * 256], b_tm[:, c, g * 128:(g + 1) * 128], xw[:, g * 256:(g + 1) * 256], True, True,
                             (btb, xwb), (psb[6],))
                    p.act(ca, csbc[:, :, LAST], AF.Exp, (psb[0], psb[1]), (cab,))
                    p.tt(h8(S), h8(S), bc8(ca, 64), ALU.mult, (Sbuf_, cab), (Sbuf_,))
                    p.tt(S, S, ps[:, 6, :], ALU.add, (Sbuf_, psb[6]), (Sbuf_,))
                    p.copy(Sb, S, (Sbuf_,), (Sbb,), eng="act")
            p.dma(z_tm, PTs[:, :, TM_Z:TM_Z + 512], ptbufs, (bigb,))
            for c in range(18):
                p.tt(h8(tmp), h8(xs_tm[:, c, :]), bc8(dsk, 64), ALU.mult, (xtb, rwb), (tmpb,), eng="pool")
                p.tt(tmp, tmp, yacc[:, c, :], ALU.add, (tmpb, yab[c]), (tmpb,))
                p.act(z_tm[:, c, :], z_tm[:, c, :], AF.Silu, (bigb,), (bigb,))
                p.tt(tmp, tmp, z_tm[:, c, :], ALU.mult, (tmpb, bigb), (tmpb,))
                p.tt(yacc[:, c, :], tmp, tmp, ALU.mult, (tmpb,), (yab[c],), eng="pool")
                p.op("dve", lambda e, c=c: e.tensor_reduce(ss[:, 0:1], yacc[:, c, :], AX.X, ALU.add), (yab[c],), (ssb,))
                p.act(ss[:, 1:2], ss[:, 0:1], AF.Sqrt, (ssb, self.cb), (ssb,), bias=self.eps_t, scale=1.0 / 512)
                p.recip(ss[:, 1:2], ss[:, 1:2], (ssb,), (ssb,))
                p.stt(yn, tmp, ss[:, 1:2], nrm, ALU.mult, ALU.mult, (tmpb, ssb, rwb), (ynb,))
                bk = 4 + c % 2
                pv = ps[:, bk, :].bitcast(BF16)
                for j in range(4):
                    p.tr(pv[:, j * 128:(j + 1) * 128], yn[:, j * 128:(j + 1) * 128], self.ident_b, (ynb, self.cb), (psb[bk],))
                p.copy(yfm[:, :, c * 128:(c + 1) * 128], pv[:, 0:512].rearrange("p (j t) -> p j t", j=4), (psb[bk],), (yfb, xpb[0], xpb[1]),
                       eng=("act" if c % 2 else "dve"))
            Ys = self.Y[s * 2560 + 1024:s * 2560 + 1536, :].rearrange("(j p) t -> p j t", p=128)
            p.dma(Ys, yfm, (yfb,), self.tokbufs("Y", s, 0, T))

    def hy_tables(self, L):
        out = {}
        nch = L // 128
        for kind in ("cf", "sf", "ci", "si"):
            if L == LL:
                Wd, b = self.Wt("dft" + kind)
                out[kind] = (Wd.rearrange("(c p) n -> p c n", p=128), b)
            else:
                ap = self.small("dftc_" + kind, nch * L, BF16)
                out[kind] = (ap.rearrange("p (c n) -> p c n", c=nch), Buf())
        return out

    def run_hy(self, l):
        p = self.p
        ps, psb = self.ps, self.psb
        if not hasattr(self, "KS"):
            self.KS = {L: self.scratch("KS%d" % L, (2 * L, 1024), F32) for L in (LC, LL)}
        if "hy_rows" not in self.sp_in:
            self.sp_in["hy_rows"] = self.ext_in("hy_rows", (1, DEPTH * 3072))
        self.new_stage()
        feat, featb = self.load_small("hy_feat", LC + LL)
        w1, w1b = self.load_small("hy_w1", DEPTH * 64)
        w2, w2b = self.load_small("hy_w2", DEPTH * 64)
        b12, b12b = self.load_small("hy_b12", DEPTH * 4)
        negtn, ntb = self.load_small("hy_negtn", 18)
        w3 = p.tile([128, 2048], F32)
        w3b = Buf()
        p.dma(w3, self.small("hy_w3", DEPTH * 2048)[:, l * 2048:(l + 1) * 2048], (), (w3b,))
        rows = p.tile([128, 2048], F32)
        rwb = Buf()
        p.dma(rows, self.sp_in["hy_rows"][0, l * 3072:l * 3072 + 2048].partition_broadcast(128), (), (rwb,))
        absd = p.tile([128, 2048], F32)
        p.ts(absd, rows, -1.0, None, ALU.mult, None, (rwb,), (rwb,), eng="pool")
        p.tt(absd, absd, rows, ALU.max, (rwb,), (rwb,))
        brow = p.tile([1, 1024], F32)
        p.dma(brow, self.sp_in["hy_rows"][0:1, l * 3072 + 2048:(l + 1) * 3072], (), (rwb,))
        h1 = p.tile([128, LL], F32)
        h2 = p.tile([128, LL], F32)
        win = p.tile([128, 1024], F32)
        filt = p.tile([128, 1024], F32)
        FS = p.tile([128, 16, 2, 512], BF16)
        kst = [p.tile([128, 2, 512], F32) for _ in range(2)]
        tab = [p.tile([128, 16, 256], BF16) for _ in range(4)]
        h1b, h2b, winb, filtb, FSb = bufs(5)
        rred = p.tile([128, 512], F32)
        rredb = Buf()
        kstb = bufs(2)
        tabb = bufs(4)
        bank = 0
        for (L, foff, noff) in ((LC, 0, 0), (LL, LC, 2)):
            nch = L // 128
            N = 2 * L
            tabs = self.hy_tables(L)
            for (src, kdim, wt_, wtb_, bcol, dst, dstb) in ((feat[:, foff:foff + L], 33, w1, w1b, 0, h1, h1b), (h1, 64, w2, w2b, 1, h2, h2b)):
                for c0 in range(0, L, 512):
                    n = min(512, L - c0)
                    bk = bank % 8
                    bank += 1
                    p.mm(ps[0:64, bk, 0:n], wt_[0:kdim, l * 64:(l + 1) * 64], src[0:kdim, c0:c0 + n], True, True,
                         (wtb_, featb, h1b), (psb[bk],))
                    p.ts(dst[0:64, c0:c0 + n], ps[0:64, bk, 0:n], b12[0:64, l * 4 + bcol:l * 4 + bcol + 1],
                         b12[0:64, l * 4 + 2 + bcol:l * 4 + 3 + bcol], ALU.add, ALU.mult, (psb[bk], b12b), (dstb,))
                    rr = rred[0:64, 0:n]
                    p.ts(rr, dst[0:64, c0:c0 + n], 1.0 / (2.0 * PI), 12582912.0, ALU.mult, ALU.add, (dstb,), (rredb,))
                    p.ts(rr, rr, -12582912.0, None, ALU.add, None, (rredb,), (rredb,))
                    p.stt(dst[0:64, c0:c0 + n], rr, -2.0 * PI, dst[0:64, c0:c0 + n], ALU.mult, ALU.add, (rredb, dstb), (dstb,))
                    p.ts(dst[0:64, c0:c0 + n], dst[0:64, c0:c0 + n], 3.1415925, -3.1415925, ALU.min, ALU.max, (dstb,), (dstb,))
                    p.act(dst[0:64, c0:c0 + n], dst[0:64, c0:c0 + n], AF.Sin, (dstb,), (dstb,))
            p.memset(h2[64:65, 0:L], 1.0, (h2b,))
            for o in range(2):
                for tc in range(nch):
                    bk = bank % 4
                    bank += 1
                    for dr in range(2):
                        c0 = (o * 2 + dr) * 512
                        p.mm(ps[:, bk * 2 + dr, :], h2[0:65, tc * 128:(tc + 1) * 128], w3[0:65, c0:c0 + 512], True, True,
                             (h2b, w3b), (psb[bk * 2 + dr],))
                    p.act(win, absd[:, o * 1024:(o + 1) * 1024], AF.Exp, (rwb, ntb), (winb,), scale=negtn[:, noff + tc:noff + tc + 1])
                    for dr in range(2):
                        p.tt(filt[:, dr * 512:(dr + 1) * 512], ps[:, bk * 2 + dr, :], win[:, dr * 512:(dr + 1) * 512], ALU.mult,
                             (psb[bk * 2 + dr], winb), (filtb,))
                    if tc == 0:
                        p.tt(filt[0:1, 0:512], filt[0:1, 0:512], brow[0:1, o * 512:(o + 1) * 512], ALU.add, (filtb, rwb), (filtb,))
                        p.memset(filt[0:1, 512:1024], 0.0, (filtb,))
                    p.tt(FS[:, tc, 0, :], filt[:, 0:512], filt[:, 512:1024], ALU.add, (filtb,), (FSb,))
                    p.tt(FS[:, tc, 1, :], filt[:, 512:1024], filt[:, 0:512], ALU.subtract, (filtb,), (FSb,), eng="pool")
                nfg = max(1, L // 256)
                for fg in range(nfg):
                    fw = min(256, L)
                    for ki, kind in enumerate(("cf", "sf")):
                        tv, tb_ = tabs[kind]
                        ti = (fg % 2) * 2 + ki
                        p.dma(tab[ti][:, 0:nch, 0:fw], tv[:, :, fg * 256:fg * 256 + fw], (tb_,), (tabb[ti],))
                    for fcl in range(fw // 128):
                        fc = fg * 2 + fcl
                        ki_ = fc % 2
                        for ki in range(2):
                            ti = (fg % 2) * 2 + ki
                            bk = 4 + (fc % 2) * 2 + ki
                            for tc in range(nch):
                                p.mm(ps[:, bk, :], tab[ti][:, tc, fcl * 128:(fcl + 1) * 128], FS[:, tc, ki, :], tc == 0, tc == nch - 1,
                                     (tabb[ti], FSb), (psb[bk],))
                            p.act(kst[ki_][:, ki, :], ps[:, bk, :], AF.Copy, (psb[bk],), (kstb[ki_],), scale=2.0 / N)
                        KSv = self.KS[L].rearrange("(r f) c -> f r c", r=2)
                        p.dma(KSv[fc * 128:(fc + 1) * 128, :, o * 512:(o + 1) * 512], kst[ki_], (kstb[ki_],), (self.B("KS", L, o, fc),))
        self.new_stage()
        cw, cwb = self.load_small("hy_cw", DEPTH * 3 * 12)
        cbias, cbb = self.load_small("hy_cb", DEPTH * 12)
        xp = p.tile([128, LL + 2], F32)
        u32 = p.tile([128, LL], F32)
        ub = p.tile([128, LL], BF16)
        xm = p.tile([128, LL], F32)
        z32 = p.tile([128, LL], F32)
        yst = p.tile([128, LL], BF16)
        u_tm = p.tile([128, 16, 512], BF16)
        Yre = p.tile([128, 16, 512], BF16)
        Wim = p.tile([128, 16, 512], BF16)
        kt = [p.tile([128, 2, 512], F32) for _ in range(2)]
        ftab = [p.tile([128, 16, 256], BF16) for _ in range(4)]
        itab = [p.tile([128, 16, 256], BF16) for _ in range(4)]
        tq = [p.tile([128, 512], F32) for _ in range(4)]
        xpb, u32b, ubb, xmb, z32b, ystb, utb, Yb, Wb_ = bufs(9)
        ktb = bufs(2)
        ftabb = bufs(4)
        itabb = bufs(4)
        tqb = bufs(4)

        def short_conv(s, j, t_off, L, dst, dstb):
            rx = (s * NFM + PF_HY + j) * 128
            p.memset(xp[:, 0:1], 0.0, (xpb,), eng="pool")
            p.memset(xp[:, L + 1:L + 2], 0.0, (xpb,), eng="pool")
            p.dma(xp[:, 1:L + 1], self.PF[rx:rx + 128, t_off:t_off + L], (self.B("PF", s, PF_HY + j),), (xpb,))
            for k in range(3):
                w = cw[:, (l * 3 + k) * 12 + j:(l * 3 + k) * 12 + j + 1]
                if k == 0:
                    p.ts(dst[:, 0:L], xp[:, 0:L], w, cbias[:, l * 12 + j:l * 12 + j + 1], ALU.mult, ALU.add, (xpb, cwb, cbb), (dstb,))
                else:
                    p.stt(dst[:, 0:L], xp[:, k:k + L], w, dst[:, 0:L], ALU.mult, ALU.add, (xpb, cwb, dstb), (dstb,))

        def to_tm(src32, srcb, cc, L):
            nch = L // 128
            p.copy(ub[:, 0:L], src32[:, 0:L], (srcb,), (ubb,), eng="act")
            for t8 in range(0, nch, 8):
                nt = min(8, nch - t8)
                bk = self.hbank % 2
                self.hbank += 1
                pv = ps[:, bk, :].bitcast(BF16)
                for q in range(nt):
                    p.tr(pv[:, q * 128:(q + 1) * 128], ub[:, (t8 + q) * 128:(t8 + q + 1) * 128], self.ident_b, (ubb, self.cb), (psb[bk],))
                p.copy(u_tm[:, t8:t8 + nt, cc * 128:(cc + 1) * 128], pv[:, 0:nt * 128].rearrange("p (q c) -> p q c", q=nt),
                       (psb[bk],), (utb,))

        self.hbank = 0
        for s in range(NSEQ):
            for (t_off, L) in ((0, LC), (LC, LL)):
                nch = L // 128
                tabs = self.hy_tables(L)
                KSv = self.KS[L].rearrange("(r f) c -> f r c", r=2)
                for cc in range(4):
                    short_conv(s, cc, t_off, L, u32, u32b)
                    to_tm(u32, u32b, cc, L)
                for o in range(2):
                    nfg = max(1, L // 256)
                    fw = min(256, L)
                    for fg in range(nfg):
                        for ki, kind in enumerate(("cf", "sf")):
                            tv, tb_ = tabs[kind]
                            ti = (fg % 2) * 2 + ki
                            p.dma(ftab[ti][:, 0:nch, 0:fw], tv[:, :, fg * 256:fg * 256 + fw], (tb_,), (ftabb[ti],))
                        for fcl in range(fw // 128):
                            fc = fg * 2 + fcl
                            kq = fc % 2
                            p.dma(kt[kq], KSv[fc * 128:(fc + 1) * 128, :, o * 512:(o + 1) * 512], (self.B("KS", L, o, fc),), (ktb[kq],))
                            bre, bim = 2 + (fc % 2) * 2, 3 + (fc % 2) * 2
                            for ki, bk in ((0, bre), (1, bim)):
                                ti = (fg % 2) * 2 + ki
                                for tc in range(nch):
                                    p.mm(ps[:, bk, :], ftab[ti][:, tc, fcl * 128:(fcl + 1) * 128], u_tm[:, tc, :], tc == 0, tc == nch - 1,
                                         (ftabb[ti], utb), (psb[bk],))
                            p.tt(tq[0], ps[:, bre, :], kt[kq][:, 0, :], ALU.mult, (psb[bre], ktb[kq]), (tqb[0],))
                            p.tt(tq[1], ps[:, bim, :], kt[kq][:, 1, :], ALU.mult, (psb[bim], ktb[kq]), (tqb[1],))
                            p.tt(Yre[:, fc, :], tq[0], tq[1], ALU.add, (tqb[0], tqb[1]), (Yb,), eng="pool")
                            p.tt(tq[2], ps[:, bim, :], kt[kq][:, 0, :], ALU.mult, (psb[bim], ktb[kq]), (tqb[2],))
                            p.tt(tq[3], ps[:, bre, :], kt[kq][:, 1, :], ALU.mult, (psb[bre], ktb[kq]), (tqb[3],))
                            p.tt(Wim[:, fc, :], tq[2], tq[3], ALU.subtract, (tqb[2], tqb[3]), (Wb_,), eng="pool")
                    tbw = min(256, L)
                    it = 0
                    for cc in range(4):
                        short_conv(s, 4 * (o + 1) + cc, t_off, L, xm, xmb)
                        for tb in range(L // tbw):
                            for ki, kind in enumerate(("ci", "si")):
                                tv, tb_ = tabs[kind]
                                ti = (it % 2) * 2 + ki
                                p.dma(itab[ti][:, 0:nch, 0:tbw], tv[:, :, tb * tbw:(tb + 1) * tbw], (tb_,), (itabb[ti],))
                            bk = 6 + it % 2
                            for fc in range(nch):
                                p.mm(ps[:, bk, 0:tbw], Yre[:, fc, cc * 128:(cc + 1) * 128], itab[(it % 2) * 2][:, fc, 0:tbw], fc == 0, False,
                                     (Yb, itabb[(it % 2) * 2]), (psb[bk],))
                                p.mm(ps[:, bk, 0:tbw], Wim[:, fc, cc * 128:(cc + 1) * 128], itab[(it % 2) * 2 + 1][:, fc, 0:tbw], False, fc == nch - 1,
                                     (Wb_, itabb[(it % 2) * 2 + 1]), (psb[bk],))
                            it += 1
                            if o == 0:
                                p.tt(z32[:, tb * tbw:(tb + 1) * tbw], xm[:, tb * tbw:(tb + 1) * tbw], ps[:, bk, 0:tbw], ALU.mult,
                                     (xmb, psb[bk]), (z32b,))
                            else:
                                p.tt(yst[:, tb * tbw:(tb + 1) * tbw], xm[:, tb * tbw:(tb + 1) * tbw], ps[:, bk, 0:tbw], ALU.mult,
                                     (xmb, psb[bk]), (ystb,))
                        if o == 0:
                            to_tm(z32, z32b, cc, L)
                        else:
                            r0 = s * 2560 + cc * 128
                            p.dma(self.Y[r0:r0 + 128, t_off:t_off + L], yst[:, 0:L], (ystb,), self.tokbufs("Y", s, t_off, t_off + L))

    def build(self, upto="all"):
        self.setup()
        if not self.lazy:
            self.declare_weights()
        if self.layers[0] == 0:
            self.stage_input()
        else:
            xin = self.ext_in("X_in", (NSEQ * D, T))
            for s_ in range(NSEQ):
                self.p.dma(self.X[s_ * D:(s_ + 1) * D, :], xin[s_ * D:(s_ + 1) * D, :], (), self.tokbufs("X", s_, 0, T))
        order = ["mod", "norm1", "proj", "hy", "lru", "ssd", "att", "gates", "merge", "out", "norm2", "ffn"]
        for l in self.layers:
            for st in order:
                getattr(self, "run_" + st)(l)
                if upto == (l, st):
                    return self.finish(False)
        return self.finish(self.layers[-1] == DEPTH - 1)

    def run_mod(self, l):
        self.stage_mod(l)

    def run_norm1(self, l):
        self.stage_norm(l, 0)

    def run_gates(self, l):
        pass

    def run_norm2(self, l):
        self.stage_norm(l, 1, router=(l if l % 2 == 1 else None))

    def finish(self, final):
        if final:
            self.stage_final()
        elif not self.debug:
            self.p.barrier()
            xo = self.ext_out("X_out", (NSEQ * D, T))
            for s_ in range(NSEQ):
                self.p.dma(xo[s_ * D:(s_ + 1) * D, :], self.X[s_ * D:(s_ + 1) * D, :], self.tokbufs("X", s_, 0, T), ())
        for name in sorted(self.debug):
            ap, shape, dt = {"X": (self.X, (NSEQ * D, T), F32), "H": (self.H, (NSEQ * D, T), BF16),
                             "PF": (self.PF, (NSEQ * NFM * 128, T), F32), "PT": (self.PT, (NSEQ * T, NTM), F32),
                             "Y": (self.Y, (NSEQ * 2560, T), BF16), "G": (self.G, (NSEQ * 4 * D, T), BF16),
                             "M": (self.M, (NSEQ * D, T), BF16)}[name]
            self.dump(name, ap, shape, dt)
        self.p.emit(self.es)
        self.es.close()
        return self.nc


def _fm(a):
    a = np.asarray(a, np.float32)
    R, C = a.shape
    return np.ascontiguousarray(a.reshape(R, C // 128, 128).transpose(2, 0, 1).reshape(128, R * (C // 128)))


def _shard(a, core, ns):
    rows = a.shape[0]
    rs = rows // ns
    return np.ascontiguousarray(a[core * rs:(core + 1) * rs])


def make_in_map(k, inp, core, ns, extra=None):
    m = {}
    b0 = core * NSEQ
    for name, (shape, dt) in k.inputs.items():
        if extra is not None and name in extra:
            v = extra[name]
        elif name == "x":
            v = inp["x"][b0:b0 + NSEQ].reshape(NSEQ * LL, D)
        elif name == "ctx":
            v = inp["ctx"][b0:b0 + NSEQ].reshape(NSEQ * LC, D)
        elif name == "cT":
            cc = np.concatenate([inp["c"][b0:b0 + NSEQ], inp["c_ctx"][None, :]], 0)
            v = cc.reshape(3, KC, 128).transpose(2, 1, 0).reshape(128, KC * 3)
        elif name == "consts":
            v = np.eye(128, dtype=np.float32)
        elif name == "normg":
            v = _fm(np.concatenate([inp["norm_mix"], inp["norm_ffn"], inp["norm_final"][None, :]], 0))
        elif name == "adab":
            v = _fm(inp["ada_b"])
        elif name.startswith("ada_w"):
            v = _shard(inp["ada_w"][int(name[5:])], core, ns)
        elif name.startswith("w_in"):
            v = _shard(inp["w_in"][int(name[4:])], core, ns)
        elif name.startswith("w_gate"):
            v = _shard(inp["w_gate"][int(name[6:])].reshape(4 * D, D), core, ns)
        elif name.startswith("w_branch"):
            v = _shard(inp["w_branch"][int(name[8:])], core, ns)
        elif name.startswith("w_out"):
            v = _shard(inp["w_out"][int(name[5:])], core, ns)
        elif name.startswith("ffn_w"):
            v = _shard(inp[name[:6]][int(name[7:])], core, ns)
        elif name.startswith("moe_w"):
            _, wn, j, e = name.split("_")
            v = _shard(inp["moe_" + wn][int(j)][int(e)], core, ns)
        elif name.startswith("dft_"):
            v = _shard(_dft_table(name[4:], LL), core, ns)
        else:
            v = host_small(name, inp)
        v = np.ascontiguousarray(v)
        if dt == F32:
            v = v.astype(np.float32, copy=False)
        assert tuple(v.shape) == tuple(shape), (name, v.shape, shape)
        m[name] = v
    return m


_DFT_CACHE = {}


def _dft_table(kind, L):
    key = (kind, L)
    if key not in _DFT_CACHE:
        N = 2 * L
        t = np.arange(L, dtype=np.int64)
        f = np.arange(L, dtype=np.int64)
        ph = ((2 * f[None, :] + 1) * t[:, None]) % (2 * N)
        ang = ph.astype(np.float64) * (np.pi / N)
        tab = np.cos(ang) if kind[0] == "c" else np.sin(ang)
        if kind[1] == "i":
            tab = tab.T
        _DFT_CACHE[key] = np.ascontiguousarray(tab.astype(np.float32).astype(ml_dtypes.bfloat16))
    return _DFT_CACHE[key]


def host_small(name, inp):
    f32 = np.float32
    if name == "bgate":
        return _fm(inp["b_gate"].reshape(DEPTH * 4, D))
    if name == "lru_cw":
        return _fm(inp["lru_conv_w"].reshape(DEPTH * 4, 512))
    if name == "lru_cb":
        return _fm(inp["lru_conv_b"])
    if name in ("lru_ba", "lru_bx"):
        return _fm(inp[name].reshape(DEPTH * 2, 512))
    if name == "lru_lam":
        return _fm(inp["lru_lambda"].reshape(DEPTH * 2, 512))
    if name in ("lru_wa", "lru_wx"):
        return inp[name].reshape(DEPTH * 2 * 8 * 64, 64)
    if name == "att_qn":
        return _fm(inp["att_q_norm"])
    if name == "att_kn":
        return _fm(inp["att_k_norm"])
    if name == "att_rows":
        return np.concatenate([inp["att_q_norm"], inp["att_k_norm"]], 1).reshape(1, DEPTH * 256)
    if name in ("rope_cos", "rope_sin"):
        t = np.arange(LL)
        row = (t // 64).astype(f32)
        col = (t % 64).astype(f32)
        inv = (10000.0 ** (-np.arange(0, 64, 2, dtype=f32) / 64)).astype(f32)
        ang = np.concatenate([row[:, None] * inv, col[:, None] * inv], -1)
        tab = np.cos(ang) if name == "rope_cos" else np.sin(ang)
        return np.ascontiguousarray(np.repeat(tab, 2, axis=1).T.astype(f32))
    if name == "rotm":
        r = np.zeros((128, 128), f32)
        for i in range(64):
            r[2 * i + 1, 2 * i] = -1.0
            r[2 * i, 2 * i + 1] = 1.0
        return r
    if name == "ssd_cw":
        return _fm(inp["ssd_conv_w"].reshape(DEPTH * 4, 1024))
    if name == "ssd_cb":
        return _fm(inp["ssd_conv_b"])
    if name == "ssd_rows":
        return np.concatenate([inp["ssd_a_log"].reshape(DEPTH, 16), inp["ssd_dt_bias"].reshape(DEPTH, 16),
                               inp["ssd_d"], inp["ssd_norm"]], 1).reshape(1, DEPTH * 552)
    if name == "ssd_consts":
        j = np.arange(128)[:, None]
        i = np.arange(128)[None, :]
        tf = (j <= i).astype(f32)
        tb = (j >= i).astype(f32)
        return np.concatenate([tf, tb, (tf - 1.0) * 30000.0, (tb - 1.0) * 30000.0], 1)
    if name == "hy_cw":
        return _fm(inp["hy_conv_w"].reshape(DEPTH * 3, 1536))
    if name == "hy_cb":
        return _fm(inp["hy_conv_b"])
    if name in ("hy_w1", "hy_w2"):
        w = inp[name]
        out = np.zeros((128, DEPTH * 64), f32)
        out[:w.shape[1]] = w.transpose(1, 0, 2).reshape(w.shape[1], DEPTH * 64)
        return out
    if name == "hy_w3":
        out = np.zeros((128, DEPTH * 2048), f32)
        out[:64] = inp["hy_w3"].transpose(1, 0, 2).reshape(64, DEPTH * 2048)
        out[64] = inp["hy_b3"].reshape(DEPTH * 2048)
        return out
    if name == "hy_b12":
        out = np.zeros((128, DEPTH * 4), f32)
        for l in range(DEPTH):
            out[:64, l * 4 + 0] = inp["hy_b1"][l]
            out[:64, l * 4 + 1] = inp["hy_b2"][l]
            out[:64, l * 4 + 2] = inp["hy_freq"][l, 0]
            out[:64, l * 4 + 3] = inp["hy_freq"][l, 1]
        return out
    if name == "hy_rows":
        return np.concatenate([inp["hy_decay"].reshape(DEPTH, 2048), inp["hy_bias"].reshape(DEPTH, 1024)], 1).reshape(1, DEPTH * 3072)
    if name == "hy_feat":
        out = np.zeros((128, LC + LL), f32)
        off = 0
        for L in (LC, LL):
            t = np.arange(L, dtype=np.float64)
            bands = np.linspace(1e-4, 15.0, 16)
            ang = (2.0 * np.pi / L) * t[:, None] * bands[None, :]
            ft = np.concatenate([(t / L)[:, None], np.cos(ang), -np.sin(ang)], -1)
            out[:33, off:off + L] = ft.T
            off += L
        return out
    if name == "hy_negtn":
        out = np.zeros((128, 18), f32)
        pp = np.arange(128)
        for c in range(2):
            out[:, c] = -(c * 128 + pp) / float(LC)
        for c in range(16):
            out[:, 2 + c] = -(c * 128 + pp) / float(LL)
        return out
    if name.startswith("dftc_"):
        tabl = _dft_table(name[5:], LC)
        return np.ascontiguousarray(tabl.reshape(2, 128, LC).transpose(1, 0, 2).reshape(128, 2 * LC))
    if name.startswith("router_b"):
        return inp["moe_router_b"][int(name[8:])].reshape(1, NEXP)
    if name.startswith("router"):
        return _fm(inp["moe_router"][int(name[6:])].T).reshape(128, NEXP, KC).transpose(0, 2, 1).reshape(128, KC * NEXP)
    raise KeyError(name)


_PROG_CACHE = {}
SEGMENTS = [[0], [1], [2], [3]]


def _get_prog(seg):
    key = tuple(seg)
    if key not in _PROG_CACHE:
        k = K(n_layers=DEPTH, n_shards=0, layers=seg)
        nc = k.build()
        _PROG_CACHE[key] = (k, nc)
    return _PROG_CACHE[key]


def _weight_src(name, inp):
    if name.startswith("ada_w"):
        return inp["ada_w"][int(name[5:])]
    if name.startswith("w_in"):
        return inp["w_in"][int(name[4:])]
    if name.startswith("w_gate"):
        return inp["w_gate"][int(name[6:])].reshape(4 * D, D)
    if name.startswith("w_branch"):
        return inp["w_branch"][int(name[8:])]
    if name.startswith("w_out"):
        return inp["w_out"][int(name[5:])]
    if name.startswith("ffn_w"):
        src = {"ffn_w1": inp["ffn_w1"], "ffn_w3": inp["ffn_w3"], "ffn_w2": inp["ffn_w2"]}[name[:6]]
        return src[int(name[7:])]
    if name.startswith("moe_w"):
        _, wn, j, e = name.split("_")
        src = {"w1": inp["moe_w1"], "w3": inp["moe_w3"], "w2": inp["moe_w2"]}[wn]
        return src[int(j)][int(e)]
    raise KeyError(name)


def _build_prep(specs):
    nc = bass.Bass("TRN2", target_bir_lowering=False)
    pairs = []
    for name, (rows, cols) in specs.items():
        rs = rows // NCORE
        src = nc.dram_tensor(name, [rs, cols], F32, kind="ExternalInput").ap()
        dst = nc.dram_tensor(name + "_b", [rs, cols], BF16, kind="ExternalOutput").ap()
        pairs.append((src, dst, rs))
    with ExitStack() as es:
        sems = [es.enter_context(nc.semaphore("s%d" % i)) for i in range(8)]
        block = es.enter_context(nc.Block())

        @block.gpsimd
        def _(g):
            cnt = [0] * 8
            i = 0
            for src, dst, rs in pairs:
                for r0 in range(0, rs, 256):
                    r1 = min(rs, r0 + 256)
                    q = i % 8
                    i += 1
                    if cnt[q]:
                        g.wait_ge(sems[q], cnt[q] * 16)
                    g.dma_start(out=dst[r0:r1, :], in_=src[r0:r1, :]).then_inc(sems[q], 16)
                    cnt[q] += 1
            for q in range(8):
                if cnt[q]:
                    g.wait_ge(sems[q], cnt[q] * 16)
    return nc


def kernel(**inputs):
    inp = {k_: np.asarray(v) for k_, v in inputs.items()}
    progs = [_get_prog(seg) for seg in SEGMENTS]
    specs = {}
    for k, _ in progs:
        for name, (rows, cols, cast) in k.wspec.items():
            if cast:
                specs[name] = (rows, cols)
    if "prep" not in _PROG_CACHE:
        _PROG_CACHE["prep"] = _build_prep(specs)
    in_maps = []
    for c in range(NCORE):
        in_maps.append({name: _shard(_weight_src(name, inp), c, NCORE) for name in specs})
    res = run_bass_kernel_spmd(_PROG_CACHE["prep"], in_maps, core_ids=list(range(NCORE)))
    wfull = {name: np.concatenate([np.asarray(res.results[c][name + "_b"]) for c in range(NCORE)], axis=0) for name in specs}
    del in_maps, res
    xs = None
    for (k, nc), seg in zip(progs, SEGMENTS):
        extra_all = {}
        for name, (rows, cols, cast) in k.wspec.items():
            extra_all[name] = wfull[name] if cast else _dft_table(name[4:], LL)
        in_maps = []
        for c in range(NCORE):
            extra = dict(extra_all)
            if xs is not None:
                extra["X_in"] = xs[c]
            in_maps.append(make_in_map(k, inp, c, NCORE, extra=extra))
        res = run_bass_kernel_spmd(nc, in_maps, core_ids=list(range(NCORE)))
        del in_maps
        if seg[-1] == DEPTH - 1:
            outs = [np.asarray(r["out"]).reshape(NSEQ, LL, D) for r in res.results]
            return np.concatenate(outs, axis=0).astype(np.float32, copy=False)
        xs = [np.asarray(r["X_out"]) for r in res.results]
```
